# Optimizing a Trainium2 kernel written in Bass

```python
import math
import jax, jax.numpy as jnp
from jax import lax
import numpy as np

D_MODEL = 1024
BATCH = 8
SEQ = 2048
DEPTH = 2

CTX_LEN = 256
GRID_W = 64
D_S5 = D_MODEL // 2
S5_GROUP = 16
S5_GROUPS = D_S5 // S5_GROUP
S5_STATE = 64
D_SC = D_MODEL // 4
SC_WIDTH = 3
D_CF = D_MODEL // 4
CF_WIDTH = 31
SPLITS = (D_S5, D_S5 + D_SC, D_S5 + 2 * D_SC, D_S5 + 3 * D_SC, D_S5 + 3 * D_SC + D_CF)
D_IN = D_S5 + 3 * D_SC + 2 * D_CF
N_GROUPS = 4
EXP_PER_GROUP = 4
N_EXPERTS = N_GROUPS * EXP_PER_GROUP
D_EXPERT = D_MODEL // 4
TOP_K = 2
DN_ALPHA = (2 * DEPTH) ** 0.25
DN_BETA = (8 * DEPTH) ** -0.25
LN_EPS = 1e-5

kernel_name = 'hybrid_s5_conv_conformer_hmoe_diffusion'


def layer_norm(x, g, b):
    xf = x.astype(jnp.float32)
    mu = jnp.mean(xf, -1, keepdims=True)
    var = jnp.mean(jnp.square(xf - mu), -1, keepdims=True)
    return ((xf - mu) * lax.rsqrt(var + LN_EPS) * g.astype(jnp.float32) + b.astype(jnp.float32)).astype(x.dtype)


def modulate(x, shift, scale):
    return x * (1 + scale) + shift


def dwconv_seq(x, w):
    k = w.shape[0]
    p = (k - 1) // 2
    return lax.conv_general_dilated(x, w[:, None, :].astype(x.dtype), (1,), [(p, p)],
                                    dimension_numbers=('NWC', 'WIO', 'NWC'),
                                    feature_group_count=x.shape[-1])


def dwconv_grid(x, w):
    kh, kw = w.shape[0], w.shape[1]
    return lax.conv_general_dilated(x, w[:, :, None, :].astype(x.dtype), (1, 1),
                                    [((kh - 1) // 2, (kh - 1) // 2), ((kw - 1) // 2, (kw - 1) // 2)],
                                    dimension_numbers=('NHWC', 'HWIO', 'NHWC'),
                                    feature_group_count=x.shape[-1])


def _linear_combine(e1, e2):
    a1, b1 = e1
    a2, b2 = e2
    return a2 * a1, a2 * b1 + b2


def linear_scan(lam_bar, bu):
    a = jnp.broadcast_to(lam_bar, (1,) + bu.shape[1:])
    _, h = lax.associative_scan(_linear_combine, (a, bu), axis=1)
    return h


def zoh(a_re, a_im, log_dt, b_re, b_im):
    f32 = jnp.float32
    lam = lax.complex(a_re.astype(f32), a_im.astype(f32))
    dt = jnp.exp(log_dt.astype(f32))[:, None]
    lam_bar = jnp.exp(lam * dt)
    b = lax.complex(b_re.astype(f32), b_im.astype(f32))
    b_bar = ((lam_bar - 1.0) / lam)[..., None] * b
    return lam_bar, b_bar


def s5_mixer(u, uc, a_re, a_im, log_dt, b_re, b_im, c_re, c_im, d_skip, w_glu, b_glu, ctx_out):
    f32 = jnp.float32
    bsz, seq, _ = u.shape
    n_ctx = uc.shape[1]
    ug = u.astype(f32).reshape(bsz, seq, S5_GROUPS, S5_GROUP)
    ucg = uc.astype(f32).reshape(bsz, n_ctx, S5_GROUPS, S5_GROUP)
    d = d_skip.astype(f32)
    y = d * u.astype(f32)
    yc = d * uc.astype(f32) if ctx_out else None
    for direction in range(2):
        lam_bar, b_bar = zoh(a_re[direction], a_im[direction], log_dt[direction],
                             b_re[direction], b_im[direction])
        c_mat = lax.complex(c_re[direction].astype(f32), c_im[direction].astype(f32))
        bu_c = jnp.einsum('bsgn,gpn->bsgp', ucg, b_bar)
        bu = jnp.einsum('bsgn,gpn->bsgp', ug, b_bar)
        if direction == 1:
            bu_c = jnp.flip(bu_c, 1)
            bu = jnp.flip(bu, 1)
        h_c = linear_scan(lam_bar, bu_c)
        bu = bu.at[:, 0].add(lam_bar * h_c[:, -1])
        h = linear_scan(lam_bar, bu)
        if direction == 1:
            h = jnp.flip(h, 1)
        y = y + jnp.real(jnp.einsum('bsgp,gnp->bsgn', h, c_mat)).reshape(bsz, seq, D_S5)
        if ctx_out:
            if direction == 1:
                h_c = jnp.flip(h_c, 1)
            yc = yc + jnp.real(jnp.einsum('bsgp,gnp->bsgn', h_c, c_mat)).reshape(bsz, n_ctx, D_S5)
    wg = w_glu.astype(f32)
    bg = b_glu.astype(f32)

    def glu(t):
        t = jax.nn.gelu(t)
        return t * jax.nn.sigmoid(t @ wg + bg)

    y = glu(y).astype(u.dtype)
    yc = glu(yc).astype(uc.dtype) if ctx_out else None
    return y, yc


def mixer_sublayer(h, hc, rows, w_in, s5_a_re, s5_a_im, s5_log_dt, s5_b_re, s5_b_im, s5_c_re, s5_c_im,
                   s5_d, w_glu, b_glu, w_sc, w_dw, b_dw, ln_cf_g, ln_cf_b, w_o, ctx_out):
    bsz, seq, _ = h.shape
    z = h @ w_in
    u, b_g, c_g, v, g_a, g_b = jnp.split(z, list(SPLITS), axis=-1)
    zc = hc @ (w_in if ctx_out else w_in[:, :D_S5])
    y_s5, yc_s5 = s5_mixer(u, zc[..., :D_S5], s5_a_re, s5_a_im, s5_log_dt, s5_b_re, s5_b_im,
                           s5_c_re, s5_c_im, s5_d, w_glu, b_glu, ctx_out)
    y_sc = b_g * dwconv_grid((c_g * v).reshape(bsz, rows, GRID_W, D_SC), w_sc[None]).reshape(bsz, seq, D_SC)
    t = dwconv_grid((g_a * jax.nn.sigmoid(g_b)).reshape(bsz, rows, GRID_W, D_CF),
                    w_dw[:, None]).reshape(bsz, seq, D_CF) + b_dw
    y_cf = jax.nn.silu(layer_norm(t, ln_cf_g, ln_cf_b))
    y = jnp.concatenate([y_s5, y_sc, y_cf], axis=-1) @ w_o
    if not ctx_out:
        return y, None
    _, bc_g, cc_g, vc, gc_a, gc_b = jnp.split(zc, list(SPLITS), axis=-1)
    yc_sc = bc_g * dwconv_seq(cc_g * vc, w_sc)
    tc = dwconv_seq(gc_a * jax.nn.sigmoid(gc_b), w_dw) + b_dw
    yc_cf = jax.nn.silu(layer_norm(tc, ln_cf_g, ln_cf_b))
    yc = jnp.concatenate([yc_s5, yc_sc, yc_cf], axis=-1) @ w_o
    return y, yc


def hier_moe(h, w_rg, b_rg, w_rexp, b_rexp, w_gate, w_up, w_down):
    f32 = jnp.float32
    hf = h.astype(f32)
    g_logits = hf @ w_rg.astype(f32) + b_rg.astype(f32)
    g_idx = jnp.argmax(g_logits, axis=-1)
    g_w = jnp.take_along_axis(jax.nn.softmax(g_logits, axis=-1), g_idx[..., None], axis=-1)
    e_logits = (hf @ w_rexp.astype(f32) + b_rexp.astype(f32)).reshape(
        h.shape[:-1] + (N_GROUPS, EXP_PER_GROUP))
    e_logits = jnp.take_along_axis(e_logits, g_idx[..., None, None], axis=-2)[..., 0, :]
    top_v, top_i = lax.top_k(e_logits, TOP_K)
    comb = g_w * jax.nn.softmax(top_v, axis=-1)
    expert = g_idx[..., None] * EXP_PER_GROUP + top_i
    weights = jnp.sum(jax.nn.one_hot(expert, N_EXPERTS, dtype=f32) * comb[..., None], axis=-2)
    gate = jnp.einsum('bsd,edf->bsef', h, w_gate)
    up = jnp.einsum('bsd,edf->bsef', h, w_up)
    act = jax.nn.silu(gate) * up * weights[..., None].astype(h.dtype)
    return jnp.einsum('bsef,efd->bsd', act, w_down)


def setup_inputs(seed: int = 0) -> dict:
    key = jax.random.key(seed)
    keys = jax.random.split(key, 40)
    counter = iter(range(40))
    f32 = jnp.float32

    def nrm(shape, scale):
        return scale * jax.random.normal(keys[next(counter)], shape, f32)

    L, D = DEPTH, D_MODEL
    G, P, N = S5_GROUPS, S5_STATE, S5_GROUP
    a_im0 = jnp.pi * jnp.arange(P, dtype=f32)
    return {
        'x': nrm((BATCH, SEQ, D), 1.0),
        'c': nrm((BATCH, D), 1.0),
        'ctx': nrm((BATCH, CTX_LEN, D), 1.0),
        'c_ctx': nrm((D,), 1.0),
        'w_mod': nrm((L, D, 6 * D), 0.5 * D ** -0.5),
        'b_mod': nrm((L, 6 * D), 0.02),
        'w_in': nrm((L, D, D_IN), D ** -0.5),
        's5_a_re': -0.5 * (1.0 + nrm((L, 2, G, P), 0.02)),
        's5_a_im': a_im0 + nrm((L, 2, G, P), 0.01),
        's5_log_dt': jax.random.uniform(keys[next(counter)], (L, 2, G), f32,
                                        math.log(1e-3), math.log(1e-1)),
        's5_b_re': nrm((L, 2, G, P, N), (2 * N) ** -0.5),
        's5_b_im': nrm((L, 2, G, P, N), (2 * N) ** -0.5),
        's5_c_re': nrm((L, 2, G, N, P), 0.5),
        's5_c_im': nrm((L, 2, G, N, P), 0.5),
        's5_d': nrm((L, D_S5), 1.0),
        'w_glu': nrm((L, D_S5, D_S5), D_S5 ** -0.5),
        'b_glu': nrm((L, D_S5), 0.02),
        'w_sc': nrm((L, SC_WIDTH, D_SC), SC_WIDTH ** -0.5),
        'w_dw': nrm((L, CF_WIDTH, D_CF), CF_WIDTH ** -0.5),
        'b_dw': nrm((L, D_CF), 0.02),
        'ln_cf_g': 1.0 + nrm((L, D_CF), 0.02),
        'ln_cf_b': nrm((L, D_CF), 0.02),
        'w_o': nrm((L, D, D), DN_BETA * D ** -0.5),
        'ln1_g': 1.0 + nrm((L, D), 0.02),
        'ln1_b': nrm((L, D), 0.02),
        'w_rg': nrm((L, D, N_GROUPS), D ** -0.5),
        'b_rg': nrm((L, N_GROUPS), 0.01),
        'w_rexp': nrm((L, D, N_EXPERTS), D ** -0.5),
        'b_rexp': nrm((L, N_EXPERTS), 0.01),
        'w_gate': nrm((L, N_EXPERTS, D, D_EXPERT), D ** -0.5),
        'w_up': nrm((L, N_EXPERTS, D, D_EXPERT), D ** -0.5),
        'w_down': nrm((L, N_EXPERTS, D_EXPERT, D), DN_BETA * D_EXPERT ** -0.5),
        'ln2_g': 1.0 + nrm((L, D), 0.02),
        'ln2_b': nrm((L, D), 0.02),
    }


def reference(x, c, ctx, c_ctx, w_mod, b_mod, w_in, s5_a_re, s5_a_im, s5_log_dt, s5_b_re, s5_b_im,
              s5_c_re, s5_c_im, s5_d, w_glu, b_glu, w_sc, w_dw, b_dw, ln_cf_g, ln_cf_b, w_o,
              ln1_g, ln1_b, w_rg, b_rg, w_rexp, b_rexp, w_gate, w_up, w_down, ln2_g, ln2_b):
    rows = x.shape[1] // GRID_W
    x_lat, x_ctx = x, ctx
    for l in range(DEPTH):
        last = l == DEPTH - 1
        mod = (jax.nn.silu(c) @ w_mod[l] + b_mod[l])[:, None, :]
        mod_c = jax.nn.silu(c_ctx) @ w_mod[l] + b_mod[l]
        sh1, sc1, g1, sh2, sc2, g2 = jnp.split(mod, 6, axis=-1)
        sh1c, sc1c, g1c, sh2c, sc2c, g2c = jnp.split(mod_c, 6, axis=-1)
        y, yc = mixer_sublayer(modulate(x_lat, sh1, sc1), modulate(x_ctx, sh1c, sc1c), rows, w_in[l],
                               s5_a_re[l], s5_a_im[l], s5_log_dt[l], s5_b_re[l], s5_b_im[l],
                               s5_c_re[l], s5_c_im[l], s5_d[l], w_glu[l], b_glu[l], w_sc[l], w_dw[l],
                               b_dw[l], ln_cf_g[l], ln_cf_b[l], w_o[l], not last)
        x_lat = layer_norm(DN_ALPHA * x_lat + g1 * y, ln1_g[l], ln1_b[l])
        if not last:
            x_ctx = layer_norm(DN_ALPHA * x_ctx + g1c * yc, ln1_g[l], ln1_b[l])
            f_ctx = hier_moe(modulate(x_ctx, sh2c, sc2c), w_rg[l], b_rg[l], w_rexp[l], b_rexp[l],
                             w_gate[l], w_up[l], w_down[l])
            x_ctx = layer_norm(DN_ALPHA * x_ctx + g2c * f_ctx, ln2_g[l], ln2_b[l])
        f = hier_moe(modulate(x_lat, sh2, sc2), w_rg[l], b_rg[l], w_rexp[l], b_rexp[l],
                     w_gate[l], w_up[l], w_down[l])
        x_lat = layer_norm(DN_ALPHA * x_lat + g2 * f, ln2_g[l], ln2_b[l])
    return x_lat
```

```python
import math
from contextlib import ExitStack
import numpy as np
import concourse.bass as bass
import concourse.mybir as mybir
from concourse.bass_utils import run_bass_kernel_spmd

F32 = mybir.dt.float32
BF16 = mybir.dt.bfloat16
I32 = mybir.dt.int32
AF = mybir.ActivationFunctionType
ALU = mybir.AluOpType
AX = mybir.AxisListType

D = 1024
DEPTH = 2
NTOK = 2304
NCTX = 256
NLAT = 2048
D_IN = 1792
NE = 16
DN_ALPHA = (2 * DEPTH) ** 0.25
LN_EPS = 1e-5
NCH = 288
BLOCKS = [(0, 256)] + [(256 + 512 * i, 512) for i in range(4)]
TWO_PI = 2.0 * math.pi
C1 = 6.28125
C2 = TWO_PI - C1


class Sched:
    ENG = ('pe', 'dve', 'act', 'pool', 'sp')

    def __init__(self, nc, stack):
        self.nc = nc
        self.ops = {e: [] for e in self.ENG}
        self.sem = {}
        for e in ('pe', 'dve', 'act', 'pool'):
            self.sem['c_' + e] = stack.enter_context(nc.semaphore('c_' + e))
        self.NDMA = 12
        self.rr = {}
        for q in ('sp', 'act', 'pool'):
            self.rr[q] = 0
            for i in range(self.NDMA):
                self.sem[f'd_{q}{i}'] = stack.enter_context(nc.semaphore(f'd_{q}{i}'))
        self.cnt = {k: 0 for k in self.sem}
        self.lastw = {}
        self.readers = {}
        self.waited = {e: {} for e in self.ENG}
        self.nops = 0
        self.debug = False

    def _deps(self, eng, reads, writes, extra=()):
        deps = {}
        for s, v in extra:
            deps[s] = v

        def add(s, v):
            if deps.get(s, 0) < v:
                deps[s] = v
        for k in reads:
            if k in self.lastw:
                add(*self.lastw[k])
        for k in writes:
            if k in self.lastw:
                add(*self.lastw[k])
            for s, v in self.readers.get(k, {}).items():
                add(s, v)
        waits = []
        w = self.waited[eng]
        for s, v in deps.items():
            if eng == 'pe' and s == 'c_pe':
                continue
            if w.get(s, 0) < v:
                w[s] = v
                waits.append((s, v))
        return waits

    def _mark(self, me, reads, writes):
        s, v = me
        for k in reads:
            self.readers.setdefault(k, {})[s] = v
        for k in writes:
            self.lastw[k] = me
            self.readers[k] = {}

    def op(self, eng, fn, reads=(), writes=()):
        if self.debug:
            import sys as _s
            fr = _s._getframe(1)
            ln = []
            while fr is not None and len(ln) < 3:
                ln.append(fr.f_lineno)
                fr = fr.f_back
            fn0 = fn
            fn = lambda e, fn0=fn0, ln=tuple(ln): fn0(e).annotate(f"L{ln}")
        waits = self._deps(eng, reads, writes)
        s = 'c_' + eng
        self.cnt[s] += 1
        self.ops[eng].append((waits, fn, s, 1))
        self._mark((s, self.cnt[s]), reads, writes)
        self.nops += 1

    def dma(self, q, out, in_, reads=(), writes=(), **kw):
        i = self.rr[q]
        self.rr[q] = (i + 1) % self.NDMA
        s = f'd_{q}{i}'
        extra = [(s, self.cnt[s])] if self.cnt[s] > 0 else []
        waits = self._deps(q, reads, writes, extra)
        self.cnt[s] += 16
        self.ops[q].append((waits, lambda e: e.dma_start(out=out, in_=in_, **kw), s, 16))
        self._mark((s, self.cnt[s]), reads, writes)
        self.nops += 1

    def barrier(self):
        for e in self.ENG:
            waits = []
            w = self.waited[e]
            for s, v in self.cnt.items():
                if v > 0 and w.get(s, 0) < v:
                    w[s] = v
                    waits.append((s, v))
            if waits:
                self.ops[e].append((waits, None, None, 0))
        self.lastw = {}
        self.readers = {}

    def emit(self):
        nc = self.nc
        with nc.Block() as block:
            decos = {'pe': block.tensor, 'dve': block.vector, 'act': block.scalar,
                     'pool': block.gpsimd, 'sp': block.sync}
            for e in self.ENG:
                ops = self.ops[e]

                def body(engine, ops=ops):
                    for waits, fn, s, inc in ops:
                        for (ws, wv) in waits:
                            engine.wait_ge(self.sem[ws], wv)
                        if fn is not None:
                            fn(engine).then_inc(self.sem[s], inc)
                decos[e](body)


def build_nc(dbg=False, stop=None):
    nc = bass.Bass("TRN2", target_bir_lowering=False)
    di = {}

    def inp(name, shape):
        di[name] = nc.dram_tensor(name, list(shape), F32, kind="ExternalInput").ap()
        return di[name]
    L = DEPTH
    xin = inp('xin', (NTOK, D))
    cT = inp('cT', (128, 8, 2))
    ident_d = inp('ident', (128, 128))
    w_mod = inp('w_mod', (L, D, 6 * D)); b_mod = inp('b_mod', (L, 6 * D))
    w_in = inp('w_in', (L, D, D_IN))
    a_re = inp('s5_a_re', (L, 2, 32, 64)); a_im = inp('s5_a_im', (L, 2, 32, 64)); log_dt = inp('s5_log_dt', (L, 2, 32))
    b_re = inp('s5_b_re', (L, 2, 32, 64, 16)); b_im = inp('s5_b_im', (L, 2, 32, 64, 16))
    c_re = inp('s5_c_re', (L, 2, 32, 16, 64)); c_im = inp('s5_c_im', (L, 2, 32, 16, 64))
    s5_d = inp('s5_d', (L, 512)); w_glu = inp('w_glu', (L, 512, 512)); b_glu = inp('b_glu', (L, 512))
    w_sc = inp('w_sc', (L, 3, 256)); w_dw = inp('w_dw', (L, 31, 256)); b_dw = inp('b_dw', (L, 256))
    ln_cf_g = inp('ln_cf_g', (L, 256)); ln_cf_b = inp('ln_cf_b', (L, 256))
    w_o = inp('w_o', (L, D, D)); ln1_g = inp('ln1_g', (L, D)); ln1_b = inp('ln1_b', (L, D))
    w_rg = inp('w_rg', (L, D, 4)); b_rg = inp('b_rg', (L, 4)); w_rexp = inp('w_rexp', (L, D, 16)); b_rexp = inp('b_rexp', (L, 16))
    w_gate = inp('w_gate', (L, NE, D, 256)); w_up = inp('w_up', (L, NE, D, 256)); w_down = inp('w_down', (L, NE, 256, D))
    ln2_g = inp('ln2_g', (L, D)); ln2_b = inp('ln2_b', (L, D))
    out = nc.dram_tensor('out', [NLAT, D], F32, kind="ExternalOutput").ap()
    xs = nc.dram_tensor('xs', [NTOK, D], F32).ap()
    modv = nc.dram_tensor('modv', [L, 2, 6 * D], F32).ap()
    dbg_out = None
    if dbg:
        dbg_out = nc.dram_tensor('dbg', [3, NTOK, 9216], F32, kind="ExternalOutput").ap()

    with ExitStack() as st:
        S = Sched(nc, st)
        S.debug = dbg
        ARENA_COLS = 50000
        arena = st.enter_context(nc.sbuf_tensor("arena", [128, ARENA_COLS], F32))[:]
        PS = [st.enter_context(nc.psum_tensor(f"ps{i}", [128, 512], F32)) for i in range(8)]
        PS = [p_[:] for p_ in PS]
        bump = [0]
        limit = [ARENA_COLS]
        YCAT_COLS = 8 * NTOK // 2

        def alloc(cols, dt=F32, shape=None):
            c32 = cols if dt == F32 else (cols + 1) // 2
            c32 = (c32 + 7) // 8 * 8
            o = bump[0]
            bump[0] += c32
            assert bump[0] <= limit[0], (bump[0], limit[0])
            v = arena[:, o:o + c32]
            if dt != F32:
                v = v.bitcast(dt)
            v = v[:, 0:cols]
            return v

        def r3(v, b):
            return v.rearrange("p (a b) -> p a b", b=b)

        def r4(v, b, c):
            return v.rearrange("p (a b c) -> p a b c", b=b, c=c)

        IDENT = alloc(128)
        EPS = alloc(8)
        S.dma('sp', IDENT, ident_d, writes=['IDENT'])
        S.op('pool', lambda e: e.memset(EPS, LN_EPS), writes=['EPS'])
        CT = alloc(16)
        SCT = alloc(16)
        persist_mark = bump[0]

        def TT(eng, out_, a, b, op, reads, writes):
            S.op(eng, lambda e: e.tensor_tensor(out=out_, in0=a, in1=b, op=op), reads, writes)

        def TS(eng, out_, a, s1, s2, op0, op1, reads, writes):
            if op1 is None:
                S.op(eng, lambda e: e.tensor_scalar(out=out_, in0=a, scalar1=s1, scalar2=None, op0=op0), reads, writes)
            else:
                S.op(eng, lambda e: e.tensor_scalar(out=out_, in0=a, scalar1=s1, scalar2=s2, op0=op0, op1=op1), reads, writes)

        def STT(out_, a, sc, b, op0, op1, reads, writes):
            S.op('dve', lambda e: e.scalar_tensor_tensor(out=out_, in0=a, scalar=sc, in1=b, op0=op0, op1=op1), reads, writes)

        def ACT(out_, in_, func, reads, writes, scale=None, bias=None, accum_out=None):
            kw = {}
            if scale is not None:
                kw['scale'] = scale
            if bias is not None:
                kw['bias'] = bias
            if accum_out is not None:
                kw['accum_out'] = accum_out
            S.op('act', lambda e: e.activation(out=out_, in_=in_, func=func, **kw), reads, writes)

        def CP(eng, out_, in_, reads, writes):
            if eng == 'act':
                S.op('act', lambda e: e.copy(out=out_, in_=in_), reads, writes)
            else:
                S.op(eng, lambda e: e.tensor_copy(out=out_, in_=in_), reads, writes)

        def MM(out_, lhsT, rhs, start, stop, reads, writes):
            S.op('pe', lambda e: e.matmul(out_, lhsT=lhsT, rhs=rhs, start=start, stop=stop), reads, writes)

        def TR(out_, in_, reads, writes):
            n = in_.shape[0]
            S.op('pe', lambda e: e.transpose(out=out_, in_=in_, identity=IDENT[0:n, 0:n]), list(reads) + ['IDENT'], writes)

        def OP(eng, meth, reads, writes, **kw):
            S.op(eng, lambda e: getattr(e, meth)(**kw), reads, writes)

        def MEMSET(eng, ap, val, writes):
            S.op(eng, lambda e: e.memset(ap, val), (), writes)

        S.dma('sp', xs[0:1152], xin[0:1152], writes=['xs'])
        S.dma('pool', xs[1152:2304], xin[1152:2304], writes=['xs2'])
        S.dma('sp', CT, cT.rearrange("k a r -> k (a r)"), writes=['CT'])
        ACT(SCT, CT, AF.Silu, ['CT'], ['SCT'])
        SCT3 = r3(SCT, 2)
        def emit_mod(l_, WM, MODSB, BM, sfx):
            for half in range(2):
                S.dma('pool', BM[0:2, :], b_mod[l_:l_ + 1, half * 3072:(half + 1) * 3072].partition_broadcast(2), writes=['BM' + sfx])
                for kc in range(8):
                    wb = WM[kc % 2]
                    S.dma('sp' if kc % 2 == 0 else 'act', wb, w_mod[l_, kc * 128:(kc + 1) * 128, half * 3072:(half + 1) * 3072],
                          writes=[f'WM{kc % 2}' + sfx])
                    for n in range(6):
                        MM(PS[n][0:2, :], SCT3[:, kc, :], wb[:, n * 512:(n + 1) * 512], kc == 0, kc == 7,
                           ['SCT', f'WM{kc % 2}' + sfx], [f'ps{n}'])
                for n in range(6):
                    CP('act', MODSB[0:2, n * 512:(n + 1) * 512], PS[n][0:2, :], [f'ps{n}'], ['MODSB' + sfx])
                TT('pool', MODSB[0:2, :], MODSB[0:2, :], BM[0:2, :], ALU.add, ['MODSB' + sfx, 'BM' + sfx], ['MODSB' + sfx])
                S.dma('sp', modv[l_, :, half * 3072:(half + 1) * 3072], MODSB[0:2, :], reads=['MODSB' + sfx], writes=['modv'])

        WM = [alloc(3072), alloc(3072)]
        MODSB = alloc(3072)
        BM = alloc(3072)
        emit_mod(0, WM, MODSB, BM, '')
        S.barrier()

        for l in range(L):
            if stop == 'p0':
                break
            last = (l == L - 1)
            bump[0] = persist_mark

            def load_bc(dst, src_row, key):
                S.dma('sp', dst, src_row.partition_broadcast(128), reads=['modv'], writes=[key])

            def modtile(j, key):
                ts = []
                for kind in range(2):
                    t = alloc(1024)
                    load_bc(t, modv[l, kind:kind + 1, j * 1024:(j + 1) * 1024], f'{key}{kind}')
                    ts.append(t)
                return ts

            def kind_of(ti):
                return 1 if ti < 2 else 0

            YCAT = arena[:, ARENA_COLS - YCAT_COLS:ARENA_COLS].bitcast(BF16); YCAT3 = r3(YCAT, NTOK)
            lay_mark = bump[0]
            limit[0] = ARENA_COLS

            UT = alloc(4 * NTOK, BF16); UT3 = r3(UT, NTOK)
            YACC = alloc(4 * NTOK); YACC3 = r3(YACC, NTOK)
            s5_mark = bump[0]
            SH1 = modtile(0, 'SH1'); SC1 = modtile(1, 'SC1')
            for kind in range(2):
                TS('dve', SC1[kind], SC1[kind], 1.0, None, ALU.add, None, [f'SC1{kind}'], [f'SC1{kind}'])
            if stop == 'b1a':
                break
            WST = alloc(8 * 512)
            WST3 = r3(WST, 512)
            WINU = alloc(8 * 512, BF16); WINU3 = r3(WINU, 512)
            S.dma('sp', WST3, w_in[l, :, 0:512].rearrange("(kc k) f -> k kc f", k=128), writes=['WST'])
            CP('act', WINU, WST, ['WST'], ['WINU'])
            DSK = alloc(4)
            S.dma('sp', DSK, s5_d[l].rearrange("(c p) -> p c", p=128), writes=['DSK'], allow_slow_non_contiguous=True)
            if stop == 'b1b':
                break
            XT = [alloc(1024), alloc(1024)]
            HTs3 = [r3(alloc(8 * 512, BF16), 512), r3(alloc(8 * 512, BF16), 512)]

            def make_hT(tok0, ntok, sh, sc, shk, sck, hpar=0):
                HT3 = HTs3[hpar]
                for i in range(ntok // 128):
                    ti = (tok0 // 128) + i
                    kd = kind_of(ti)
                    xt = XT[ti % 2]
                    xk = f'XT{ti % 2}'
                    S.dma('sp', xt, xs[ti * 128:(ti + 1) * 128, :], reads=['xs', 'xs2'], writes=[xk])
                    TT('dve', xt, xt, sc[kd], ALU.mult, [xk, f'{sck}{kd}'], [xk])
                    TT('pool', xt, xt, sh[kd], ALU.add, [xk, f'{shk}{kd}'], [xk])
                    for hh in range(2):
                        pb = 6 + hh
                        for k4 in range(4):
                            kc = hh * 4 + k4
                            TR(PS[pb][:, k4 * 128:(k4 + 1) * 128], xt[:, kc * 128:(kc + 1) * 128], [xk], [f'ps{pb}'])
                        CP('act', HT3[:, hh * 4:(hh + 1) * 4, i * 128:(i + 1) * 128],
                           r3(PS[pb], 128), [f'ps{pb}'], [f'HT{hpar}'])

            make_hT(BLOCKS[0][0], BLOCKS[0][1], SH1, SC1, 'SH1', 'SC1', 0)
            for bix, (tok0, ntok) in enumerate(BLOCKS):
                hpar = bix % 2
                HT3 = HTs3[hpar]
                if bix + 1 < len(BLOCKS):
                    make_hT(BLOCKS[bix + 1][0], BLOCKS[bix + 1][1], SH1, SC1, 'SH1', 'SC1', (bix + 1) % 2)
                if stop == 'b1c':
                    break
                for fc in range(4):
                    pb = fc % 2
                    for kc in range(8):
                        MM(PS[pb][:, 0:ntok], WINU3[:, kc, fc * 128:(fc + 1) * 128], HT3[:, kc, 0:ntok], kc == 0, kc == 7,
                           ['WINU', f'HT{hpar}'], [f'ps{pb}'])
                    CP('act', UT3[:, fc, tok0:tok0 + ntok], PS[pb][:, 0:ntok], [f'ps{pb}'], ['UT'])
                    if stop == 'b1d':
                        continue
                    ACT(YACC3[:, fc, tok0:tok0 + ntok], PS[pb][:, 0:ntok], AF.Copy, [f'ps{pb}', 'DSK'], ['YACC'], scale=DSK[:, fc:fc + 1])
                if stop in ('b1d', 'b1e'):
                    break
            S.barrier()
            bump[0] = s5_mark
            if stop in ('b1', 'b1c', 'b1d', 'b1e'):
                break

            def T32():
                return alloc(32)
            NAT = alloc(128)
            NAT2 = alloc(8)
            ARE = T32(); AIM = T32(); DT = T32()

            def load_T(dst, src2d, key):
                S.dma('sp', NAT[0:32, :], src2d, writes=['NAT'])
                TR(PS[0][:, 0:32], NAT[0:32, :], ['NAT'], ['ps0'])
                CP('act', dst, PS[0][:, 0:32], ['ps0'], [key])
            load_T(ARE, a_re[l].rearrange("d (pr g2) p -> (d pr) (g2 p)", g2=2), 'ARE')
            load_T(AIM, a_im[l].rearrange("d (pr g2) p -> (d pr) (g2 p)", g2=2), 'AIM')
            S.dma('sp', NAT2[0:32, 0:2], log_dt[l].rearrange("d (pr g2) -> (d pr) g2", g2=2), writes=['NAT2'])
            CP('dve', r3(NAT[0:32, :], 64), NAT2[0:32, 0:2].unsqueeze(2).to_broadcast([32, 2, 64]), ['NAT2', 'NAT'], ['NAT'])
            TR(PS[0][:, 0:32], NAT[0:32, :], ['NAT'], ['ps0'])
            ACT(DT, PS[0][:, 0:32], AF.Exp, ['ps0'], ['DT'])
            RHO = T32(); TH = T32(); KF = T32(); KI = alloc(32).bitcast(I32); TMP = T32(); TMP2 = T32()
            TT('dve', RHO, ARE, DT, ALU.mult, ['ARE', 'DT'], ['RHO'])
            TT('dve', TH, AIM, DT, ALU.mult, ['AIM', 'DT'], ['TH'])
            TS('dve', TMP, TH, 1.0 / TWO_PI, None, ALU.mult, None, ['TH'], ['TMP'])
            CP('dve', KI, TMP, ['TMP'], ['KI'])
            CP('dve', KF, KI, ['KI'], ['KF'])
            STT(TMP, KF, -C1, TH, ALU.mult, ALU.add, ['KF', 'TH'], ['TMP'])
            STT(TMP2, KF, -C2, TMP, ALU.mult, ALU.add, ['KF', 'TMP'], ['TMP2'])
            SHh = T32(); CHh = T32(); HPI = alloc(8)
            MEMSET('pool', HPI, math.pi / 2, ['HPI'])
            ACT(SHh, TMP2, AF.Sin, ['TMP2'], ['SHh'], scale=0.5)
            ACT(CHh, TMP2, AF.Sin, ['TMP2', 'HPI'], ['CHh'], scale=-0.5, bias=HPI[:, 0:1])
            C1T = T32(); S1T = T32(); R1 = T32()
            TT('dve', TMP, CHh, CHh, ALU.mult, ['CHh'], ['TMP'])
            TT('dve', TMP2, SHh, SHh, ALU.mult, ['SHh'], ['TMP2'])
            TT('dve', C1T, TMP, TMP2, ALU.subtract, ['TMP', 'TMP2'], ['C1T'])
            TT('dve', TMP, SHh, CHh, ALU.mult, ['SHh', 'CHh'], ['TMP'])
            TS('dve', S1T, TMP, 2.0, None, ALU.mult, None, ['TMP'], ['S1T'])
            ACT(R1, RHO, AF.Exp, ['RHO'], ['R1'])
            ER = alloc(16 * 32); EI = alloc(16 * 32); ER3 = r3(ER, 32); EI3 = r3(EI, 32)
            MEMSET('pool', ER3[:, 0, :], 1.0, ['ER'])
            MEMSET('pool', EI3[:, 0, :], 0.0, ['EI'])
            for s in range(15):
                TT('dve', TMP, ER3[:, s, :], C1T, ALU.mult, ['ER', 'C1T'], ['TMP'])
                TT('dve', TMP2, EI3[:, s, :], S1T, ALU.mult, ['EI', 'S1T'], ['TMP2'])
                TT('dve', ER3[:, s + 1, :], TMP, TMP2, ALU.subtract, ['TMP', 'TMP2'], ['ER'])
                TT('dve', TMP, ER3[:, s, :], S1T, ALU.mult, ['ER', 'S1T'], ['TMP'])
                TT('dve', TMP2, EI3[:, s, :], C1T, ALU.mult, ['EI', 'C1T'], ['TMP2'])
                TT('dve', EI3[:, s + 1, :], TMP, TMP2, ALU.add, ['TMP', 'TMP2'], ['EI'])
            RP = alloc(8 * 32); RP3 = r3(RP, 32)
            for k in range(1, 9):
                ACT(RP3[:, k - 1, :], RHO, AF.Exp, ['RHO'], ['RP'], scale=float(k))
            QR = T32(); QI = T32(); NR = T32(); NI = T32(); DEN = T32()
            TT('dve', TMP, R1, C1T, ALU.mult, ['R1', 'C1T'], ['TMP'])
            TS('dve', NR, TMP, -1.0, None, ALU.add, None, ['TMP'], ['NR'])
            TT('dve', NI, R1, S1T, ALU.mult, ['R1', 'S1T'], ['NI'])
            TT('dve', TMP, ARE, ARE, ALU.mult, ['ARE'], ['TMP'])
            TT('dve', TMP2, AIM, AIM, ALU.mult, ['AIM'], ['TMP2'])
            TT('dve', DEN, TMP, TMP2, ALU.add, ['TMP', 'TMP2'], ['DEN'])
            OP('dve', 'reciprocal', ['DEN'], ['DEN'], out=DEN, in_=DEN)
            TT('dve', TMP, NR, ARE, ALU.mult, ['NR', 'ARE'], ['TMP'])
            TT('dve', TMP2, NI, AIM, ALU.mult, ['NI', 'AIM'], ['TMP2'])
            TT('dve', TMP, TMP, TMP2, ALU.add, ['TMP', 'TMP2'], ['TMP'])
            TT('dve', QR, TMP, DEN, ALU.mult, ['TMP', 'DEN'], ['QR'])
            TT('dve', TMP, NI, ARE, ALU.mult, ['NI', 'ARE'], ['TMP'])
            TT('dve', TMP2, NR, AIM, ALU.mult, ['NR', 'AIM'], ['TMP2'])
            TT('dve', TMP, TMP, TMP2, ALU.subtract, ['TMP', 'TMP2'], ['TMP'])
            TT('dve', QI, TMP, DEN, ALU.mult, ['TMP', 'DEN'], ['QI'])
            FR = alloc(256); FI = alloc(256); ELR = alloc(256); ELI = alloc(256); KR = alloc(256); KIm = alloc(256)
            FR3 = r3(FR, 8); FI3 = r3(FI, 8); ELR3 = r3(ELR, 8); ELI3 = r3(ELI, 8); KR3 = r3(KR, 8); KI3 = r3(KIm, 8)
            for s in range(8):
                TT('dve', TMP, ER3[:, s, :], QR, ALU.mult, ['ER', 'QR'], ['TMP'])
                TT('dve', TMP2, EI3[:, s, :], QI, ALU.mult, ['EI', 'QI'], ['TMP2'])
                TT('dve', FR3[:, :, s], TMP, TMP2, ALU.add, ['TMP', 'TMP2'], ['FR'])
                TT('dve', TMP, ER3[:, s, :], QI, ALU.mult, ['ER', 'QI'], ['TMP'])
                TT('dve', TMP2, EI3[:, s, :], QR, ALU.mult, ['EI', 'QR'], ['TMP2'])
                TT('dve', FI3[:, :, s], TMP, TMP2, ALU.subtract, ['TMP', 'TMP2'], ['FI'])
                CP('pool', ELR3[:, :, s], ER3[:, s, :], ['ER'], ['ELR'])
                CP('pool', ELI3[:, :, s], EI3[:, s, :], ['EI'], ['ELI'])
                TT('dve', KR3[:, :, s], ER3[:, s + 8, :], RP3[:, s, :], ALU.mult, ['ER', 'RP'], ['KR'])
                TT('dve', KI3[:, :, s], EI3[:, s + 8, :], RP3[:, s, :], ALU.mult, ['EI', 'RP'], ['KI_'])
            LRR = alloc(64); LII = alloc(64); LRR3 = r3(LRR, 2); LII3 = r3(LII, 2)
            for c in range(2):
                TT('dve', LRR3[:, :, c], ER3[:, 8, :], RP3[:, 7, :], ALU.mult, ['ER', 'RP'], ['LRR'])
                TT('dve', LII3[:, :, c], EI3[:, 8, :], RP3[:, 7, :], ALU.mult, ['EI', 'RP'], ['LII'])
            BRE = alloc(1024); BIM = alloc(1024); CTR = alloc(1024); CTI = alloc(1024)
            BRE3 = r3(BRE, 32); BIM3 = r3(BIM, 32); CTR3 = r3(CTR, 32); CTI3 = r3(CTI, 32)
            CN = alloc(32 * 128); CN3 = r3(CN, 128)
            for (dst3, src, key) in ((BRE3, b_re, 'BRE'), (BIM3, b_im, 'BIM')):
                MEMSET('pool', dst3, 0.0, [key])
                v = src[l].rearrange("d (pr g2) p n -> g2 p (d pr) n", g2=2)
                for g2 in range(2):
                    S.dma('sp', dst3[g2 * 64:(g2 + 1) * 64, :, g2 * 16:(g2 + 1) * 16], v[g2], writes=[key])
            for (dst3, src, key) in ((CTR3, c_re, 'CTR'), (CTI3, c_im, 'CTI')):
                MEMSET('pool', CN3[0:32], 0.0, ['CN'])
                v = src[l].rearrange("d (pr g2) n p -> g2 n (d pr) p", g2=2)
                for g2 in range(2):
                    S.dma('sp', CN3[g2 * 16:(g2 + 1) * 16, :, g2 * 64:(g2 + 1) * 64], v[g2], writes=['CN'])
                for ub in range(2):
                    for uu in range(16):
                        u = ub * 16 + uu
                        TR(PS[ub][:, uu * 32:(uu + 1) * 32], CN3[0:32, u, :], ['CN'], [f'ps{ub}'])
                    CP('act', dst3[:, ub * 16:(ub + 1) * 16, :], r3(PS[ub], 32), [f'ps{ub}'], [key])
            if stop == 'tab':
                break
            XZ = alloc(32 * 289 * 2, BF16); XZ4 = r4(XZ, 289, 2)
            MEMSET('pool', XZ, 0.0, ['XZ'])
            WB = [alloc(8 * 128), alloc(8 * 128)]
            T1 = alloc(256); T2 = alloc(256)
            LBm = {}
            LCm = {}
            for d in range(2):
                for ri in range(2):
                    LBm[d, ri] = alloc(8 * 128, BF16)
                    LCm[d, ri] = alloc(8 * 128, BF16)
            DEC = [alloc(512), alloc(512)]

            def bc_s(tab3, u):
                return tab3[:, u, :].unsqueeze(2).to_broadcast([128, 8, 32])

            def bc_m(mat3, u):
                return mat3[:, u, :].unsqueeze(1).to_broadcast([128, 8, 32])

            def cplx_outer(eng, out_re, out_im, Ar, Ai, Br, Bi, u, keys_r, neg_im, okeys):
                t1 = r3(T1, 32); t2 = r3(T2, 32)
                TT(eng, t1, bc_s(Ar, u), bc_m(Br, u), ALU.mult, keys_r, ['T1'])
                TT(eng, t2, bc_s(Ai, u), bc_m(Bi, u), ALU.mult, keys_r, ['T2'])
                TT(eng, out_re, t1, t2, ALU.subtract, ['T1', 'T2'], [okeys[0]])
                TT(eng, t1, bc_s(Ar, u), bc_m(Bi, u), ALU.mult, keys_r, ['T1'])
                TT(eng, t2, bc_s(Ai, u), bc_m(Br, u), ALU.mult, keys_r, ['T2'])
                if neg_im:
                    S.op('dve', lambda e: e.scalar_tensor_tensor(out=out_im, in0=t1, scalar=-1.0, in1=t2, op0=ALU.mult, op1=ALU.subtract),
                         ['T1', 'T2'], [okeys[1]])
                else:
                    TT(eng, out_im, t1, t2, ALU.add, ['T1', 'T2'], [okeys[1]])

            tabkeys = ['FR', 'FI', 'ELR', 'ELI', 'KR', 'KI_', 'BRE', 'BIM', 'CTR', 'CTI']

            def gen_unit(d, pair, carry):
                u = d * 16 + pair
                q = pair % 4
                win = slice(32 * q, 32 * q + 32)
                if not carry:
                    WBr3 = r3(WB[0], 128); WBi3 = r3(WB[1], 128)
                    cplx_outer('dve', WBr3[:, :, win], WBi3[:, :, win], FR3, FI3, BRE3, BIM3, u, tabkeys, False, ['WB0', 'WB1'])
                    for ri in range(2):
                        w3 = r3(WB[ri], 128)
                        for hb in range(2):
                            pb = 6 + hb
                            for s4 in range(4):
                                s = hb * 4 + s4
                                TR(PS[pb][:, s4 * 128:(s4 + 1) * 128], w3[:, s, :], [f'WB{ri}'], [f'ps{pb}'])
                            CP('act', LBm[d, ri][:, hb * 512:(hb + 1) * 512], PS[pb], [f'ps{pb}'], [f'LB{d}{ri}'])
                    lc_r = r3(LCm[d, 0], 128); lc_i = r3(LCm[d, 1], 128)
                    cplx_outer('dve', lc_r[:, :, win], lc_i[:, :, win], ELR3, ELI3, CTR3, CTI3, u, tabkeys, True, [f'LC{d}0', f'LC{d}1'])
                else:
                    lc_r = r3(LCm[d, 0], 128); lc_i = r3(LCm[d, 1], 128)
                    cplx_outer('dve', lc_r[:, :, win], lc_i[:, :, win], KR3, KI3, CTR3, CTI3, u, tabkeys, True, [f'LC{d}0', f'LC{d}1'])

            def zero_windows(carry):
                if not carry:
                    MEMSET('pool', WB[0], 0.0, ['WB0'])
                    MEMSET('pool', WB[1], 0.0, ['WB1'])
                for d in range(2):
                    for ri in range(2):
                        MEMSET('pool', LCm[d, ri], 0.0, [f'LC{d}{ri}'])

            def slots(d, bi, plus):
                tok0, ntok = BLOCKS[bi]
                nch = ntok // 8
                if d == 0:
                    jj0 = tok0 // 8
                    return slice(jj0 + plus, jj0 + plus + nch)
                if bi == 0:
                    hi = 31 + plus
                else:
                    cl0 = (tok0 - NCTX) // 8
                    hi = 287 - cl0 + plus
                lo = hi - nch
                return slice(hi, lo if lo >= 0 else None, -1)

            order = [(q, pair) for q in range(4) for pair in range(q, 16, 4)]
            def gen_B(d, pair):
                u = d * 16 + pair
                q = pair % 4
                win = slice(32 * q, 32 * q + 32)
                WBr3 = r3(WB[0], 128); WBi3 = r3(WB[1], 128)
                cplx_outer('dve', WBr3[:, :, win], WBi3[:, :, win], FR3, FI3, BRE3, BIM3, u, tabkeys, False, ['WB0', 'WB1'])
                for ri in range(2):
                    w3 = r3(WB[ri], 128)
                    for hb in range(2):
                        pb = 6 + hb
                        for s4 in range(4):
                            s_ = hb * 4 + s4
                            TR(PS[pb][:, s4 * 128:(s4 + 1) * 128], w3[:, s_, :], [f'WB{ri}'], [f'ps{pb}'])
                        CP('act', LBm[d, ri][:, hb * 512:(hb + 1) * 512], PS[pb], [f'ps{pb}'], [f'LB{d}{ri}'])
                dk = f'DEC{d}'
                CP('pool', r3(DEC[d], 8), R1[:, u:u + 1].unsqueeze(2).to_broadcast([128, 64, 8]), ['R1'], [dk])
                MEMSET('pool', r3(DEC[d], 8)[:, :, 0], 0.0, [dk])

            def gen_C(d, pair, carry, lcset, lckey):
                u = d * 16 + pair
                q = pair % 4
                win = slice(32 * q, 32 * q + 32)
                lc_r = r3(lcset[d, 0], 128); lc_i = r3(lcset[d, 1], 128)
                if carry:
                    cplx_outer('dve', lc_r[:, :, win], lc_i[:, :, win], KR3, KI3, CTR3, CTI3, u, tabkeys, True, [f'{lckey}{d}0', f'{lckey}{d}1'])
                else:
                    cplx_outer('dve', lc_r[:, :, win], lc_i[:, :, win], ELR3, ELI3, CTR3, CTI3, u, tabkeys, True, [f'{lckey}{d}0', f'{lckey}{d}1'])

            def emit_B(pair, bi, par):
                cc = pair // 4
                tok0, ntok = BLOCKS[bi]
                for d in range(2):
                    u = d * 16 + pair
                    for ri in range(2):
                        pb = 2 * d + ri
                        lb3 = r3(LBm[d, ri], 128)
                        for s_ in range(8):
                            tau = s_ if d == 0 else 7 - s_
                            MM(r3(PS[pb][:, 0:ntok], 8)[:, :, s_], lb3[:, s_, :],
                               r3(UT3[:, cc, tok0:tok0 + ntok], 8)[:, :, tau], True, True,
                               [f'LB{d}{ri}', 'UT'], [f'ps{pb}'])
                        bp = BPB[par, d, ri]; bk = f'BP{par}{d}{ri}'
                        CP('act', bp[:, 0:ntok], PS[pb][:, 0:ntok], [f'ps{pb}'], [bk])
                        g = GB[par, d, ri]; gk = f'G{par}{d}{ri}'
                        OP('dve', 'tensor_tensor_scan', [f'DEC{d}', bk], [gk],
                           out=g[:, 0:ntok], data0=DEC[d][:, 0:ntok], data1=bp[:, 0:ntok], initial=0.0,
                           op0=ALU.mult, op1=ALU.add)
                        CP('pool', XZ4[:, u, slots(d, bi, 1), ri], r3(g[:, 0:ntok], 8)[:, :, 7], [gk], ['XZ'])

            def emit_C(pair, bi, par):
                cc = pair // 4
                tok0, ntok = BLOCKS[bi]
                pby = 4 + par
                for d in range(2):
                    for ri in range(2):
                        lc3 = r3(LCm[d, ri], 128)
                        g = GB[par, d, ri]; gk = f'G{par}{d}{ri}'
                        for s_ in range(8):
                            tau = s_ if d == 0 else 7 - s_
                            first = (d == 0 and ri == 0 and s_ == 0)
                            lastmm = (d == 1 and ri == 1 and s_ == 7)
                            MM(r3(PS[pby][:, 0:ntok], 8)[:, :, tau], lc3[:, s_, :], r3(g[:, 0:ntok], 8)[:, :, s_],
                               first, lastmm, [f'LC{d}{ri}', gk], [f'ps{pby}'])
                TT('dve', YACC3[:, cc, tok0:tok0 + ntok], YACC3[:, cc, tok0:tok0 + ntok], PS[pby][:, 0:ntok], ALU.add,
                   ['YACC', f'ps{pby}'], ['YACC'])

            BPB = {}
            GB = {}
            s5blk0 = bump[0]
            for par in range(2):
                for d in range(2):
                    for ri in range(2):
                        BPB[par, d, ri] = alloc(512)
                        GB[par, d, ri] = alloc(512, BF16)
            MODSB1 = arena[:, s5blk0:s5blk0 + 3072]
            BM1 = arena[:, s5blk0 + 3072:s5blk0 + 6144]
            assert bump[0] - s5blk0 >= 6144
            curqB = -1; curqC = -1
            prev = None
            cnt_items = 0
            for (q, pair) in order:
                if q != curqB:
                    MEMSET('pool', WB[0], 0.0, ['WB0'])
                    MEMSET('pool', WB[1], 0.0, ['WB1'])
                    curqB = q
                for d in range(2):
                    gen_B(d, pair)
                for bi in range(len(BLOCKS)):
                    par = cnt_items % 2
                    cnt_items += 1
                    emit_B(pair, bi, par)
                    if prev is not None:
                        emit_C(*prev)
                    if bi == 0:
                        if q != curqC:
                            for d in range(2):
                                for ri in range(2):
                                    MEMSET('pool', LCm[d, ri], 0.0, [f'LC{d}{ri}'])
                            curqC = q
                        for d in range(2):
                            gen_C(d, pair, False, LCm, 'LC')
                    prev = (pair, bi, par)
            emit_C(*prev)
            if stop == 'pass1':
                break
            if l == 0:
                S.barrier()
                emit_mod(1, [UT.bitcast(F32)[:, 0:3072], CN[:, 0:3072]], MODSB1, BM1, 'L1')
            ZP = [alloc(64), alloc(64)]
            CAB = alloc(128)
            LL4 = alloc(128)
            CAB4 = r4(CAB, 2, 2); LL4v = r4(LL4, 2, 2)
            CP('pool', LL4v[:, :, 0, :], LRR3, ['LRR'], ['LL4'])
            CP('pool', LL4v[:, :, 1, :], LII3, ['LII'], ['LL4'])
            MEMSET('pool', ZP[0], 0.0, ['ZP0h0r', 'ZP0h0i', 'ZP0h1r', 'ZP0h1i'])
            CH_E = 'dve'
            for k in range(NCH):
                zp = ZP[k % 2]; zn = ZP[(k + 1) % 2]
                zn3 = r3(zn, 2); zp3 = r3(zp, 2)
                for h in range(2):
                    us = slice(16 * h, 16 * h + 16)
                    zpk = f'ZP{k % 2}h{h}'; znk = f'ZP{(k + 1) % 2}h{h}'
                    ck = f'CAB{h}'
                    xk = XZ4[:, us, k + 1, :]
                    cab = CAB4[:, us]
                    TT(CH_E, cab, zp3[:, us, :].unsqueeze(2).to_broadcast([128, 16, 2, 2]), LL4v[:, us], ALU.mult,
                       [zpk + 'r', zpk + 'i', 'LL4'], [ck])
                    TT(CH_E, cab[:, :, 0, :], cab[:, :, 0, :], xk, ALU.add, [ck, 'XZ'], [ck])
                    TT(CH_E, zn3[:, us, 0], cab[:, :, 0, 0], cab[:, :, 1, 1], ALU.subtract, [ck], [znk + 'r'])
                    TT(CH_E, zn3[:, us, 1], cab[:, :, 0, 1], cab[:, :, 1, 0], ALU.add, [ck], [znk + 'i'])
                nk = f'ZP{(k + 1) % 2}'
                CP('pool', XZ4[:, :, k + 1, :], zn3, [nk + 'h0r', nk + 'h0i', nk + 'h1r', nk + 'h1i'], ['XZ' + str(k)])
            if stop == 'chain':
                break
            S.barrier()
            LCsets = [LCm, LBm]
            LCkeys = ['LC', 'LB']
            setq = [-1, -1]

            def gen_pair2(idx):
                q, pair = order[idx]
                pp = idx % 2
                if setq[pp] != q:
                    for d in range(2):
                        for ri in range(2):
                            MEMSET('pool', LCsets[pp][d, ri], 0.0, [f'{LCkeys[pp]}{d}{ri}'])
                    setq[pp] = q
                for d in range(2):
                    gen_C(d, pair, True, LCsets[pp], LCkeys[pp])
            gen_pair2(0)
            it2 = 0
            for idx, (q, pair) in enumerate(order):
                cc = pair // 4
                pp = idx % 2
                if idx + 1 < len(order):
                    gen_pair2(idx + 1)
                for bi, (tok0, ntok) in enumerate(BLOCKS):
                    pby = it2 % 4
                    it2 += 1
                    for d in range(2):
                        u = d * 16 + pair
                        for ri in range(2):
                            lc3 = r3(LCsets[pp][d, ri], 128)
                            for s_ in range(8):
                                tau = s_ if d == 0 else 7 - s_
                                first = (d == 0 and ri == 0 and s_ == 0)
                                lastmm = (d == 1 and ri == 1 and s_ == 7)
                                MM(r3(PS[pby][:, 0:ntok], 8)[:, :, tau], lc3[:, s_, :], XZ4[:, u, slots(d, bi, 0), ri],
                                   first, lastmm, [f'{LCkeys[pp]}{d}{ri}', 'XZ'], [f'ps{pby}'])
                    TT('dve', YACC3[:, cc, tok0:tok0 + ntok], YACC3[:, cc, tok0:tok0 + ntok], PS[pby][:, 0:ntok], ALU.add,
                       ['YACC', f'ps{pby}'], ['YACC'])
            S.barrier()
            if dbg and l == 0:
                for c in range(4):
                    S.dma('sp', dbg_out[0, 0:128, c * NTOK:(c + 1) * NTOK], YACC3[:, c, :], reads=['YACC'], writes=['dbg0'])
                S.barrier()
                if stop == 's5':
                    break
            bump[0] = s5_mark
            limit[0] = ARENA_COLS - YCAT_COLS
            WG_ST = alloc(4 * 512); WG = alloc(4 * 512, BF16); WG3 = r3(WG, 512)
            S.dma('sp', r3(WG_ST, 512), w_glu[l].rearrange("(kc k) f -> k kc f", k=128), writes=['WGST'])
            CP('act', WG, WG_ST, ['WGST'], ['WG'])
            BG = alloc(4)
            S.dma('sp', BG, b_glu[l].rearrange("(c p) -> p c", p=128), writes=['BG'], allow_slow_non_contiguous=True)
            GT = alloc(4 * NTOK, BF16); GT3 = r3(GT, NTOK)
            SG = [alloc(512), alloc(512)]
            for c in range(4):
                ACT(YACC3[:, c, :], YACC3[:, c, :], AF.Gelu, ['YACC'], ['YACC'])
                CP('pool', GT3[:, c, :], YACC3[:, c, :], ['YACC'], ['GT'])
            for (tok0, ntok) in BLOCKS:
                for oc in range(4):
                    pb = oc % 2
                    for kc in range(4):
                        MM(PS[pb][:, 0:ntok], WG3[:, kc, oc * 128:(oc + 1) * 128], GT3[:, kc, tok0:tok0 + ntok], kc == 0, kc == 3,
                           ['WG', 'GT'], [f'ps{pb}'])
                    ACT(SG[pb][:, 0:ntok], PS[pb][:, 0:ntok], AF.Sigmoid, [f'ps{pb}', 'BG'], [f'SG{pb}'], bias=BG[:, oc:oc + 1])
                    TT('dve', YCAT3[:, oc, tok0:tok0 + ntok], YACC3[:, oc, tok0:tok0 + ntok], SG[pb][:, 0:ntok], ALU.mult,
                       ['YACC', f'SG{pb}'], ['YCAT'])
            S.barrier()

            if stop == 'gate':
                break
            bump[0] = lay_mark
            SH1 = modtile(0, 'SH1'); SC1 = modtile(1, 'SC1')
            for kind in range(2):
                TS('dve', SC1[kind], SC1[kind], 1.0, None, ALU.add, None, [f'SC1{kind}'], [f'SC1{kind}'])
            GLU = alloc(2 * NTOK); GLU3 = r3(GLU, NTOK)
            NF2 = 1280
            WST = alloc(8 * 640); WST3 = r3(WST, 640)
            WINR = alloc(8 * NF2, BF16); WINR3 = r3(WINR, NF2)
            for hf in range(2):
                S.dma('sp', WST3, w_in[l, :, 512 + hf * 640:512 + (hf + 1) * 640].rearrange("(kc k) f -> k kc f", k=128), writes=['WST'])
                CP('act', WINR3[:, :, hf * 640:(hf + 1) * 640], WST3, ['WST'], ['WINR'])
            WSC = alloc(8)
            WSC3 = r3(WSC[:, 0:6], 3)
            for c_ in range(2):
                S.dma('sp', WSC3[:, c_, :], w_sc[l][:, c_ * 128:(c_ + 1) * 128].rearrange("k p -> p k"), writes=['WSC'], allow_slow_non_contiguous=True)
            XT = [alloc(1024), alloc(1024)]
            HTs3 = [r3(alloc(8 * 512, BF16), 512), r3(alloc(8 * 512, BF16), 512)]
            BGT = alloc(512); CGT = alloc(512); CV = alloc(512); ACC = alloc(512); SIG = alloc(512)
            make_hT(BLOCKS[0][0], BLOCKS[0][1], SH1, SC1, 'SH1', 'SC1', 0)
            for bix, (tok0, ntok) in enumerate(BLOCKS):
                hpar = bix % 2
                HT3 = HTs3[hpar]
                if bix + 1 < len(BLOCKS):
                    make_hT(BLOCKS[bix + 1][0], BLOCKS[bix + 1][1], SH1, SC1, 'SH1', 'SC1', (bix + 1) % 2)
                W = 64 if tok0 >= NCTX else 256
                nr = ntok // W

                def zmm(fc, pb):
                    f0 = fc * 128 - 512
                    for kc in range(8):
                        MM(PS[pb][:, 0:ntok], WINR3[:, kc, f0:f0 + 128], HT3[:, kc, 0:ntok], kc == 0, kc == 7,
                           ['WINR', f'HT{hpar}'], [f'ps{pb}'])
                for j in range(2):
                    zmm(4 + j, 0)
                    CP('act', BGT[:, 0:ntok], PS[0][:, 0:ntok], ['ps0'], ['BGT'])
                    zmm(6 + j, 1)
                    CP('act', CGT[:, 0:ntok], PS[1][:, 0:ntok], ['ps1'], ['CGT'])
                    zmm(8 + j, 2)
                    TT('dve', CV[:, 0:ntok], CGT[:, 0:ntok], PS[2][:, 0:ntok], ALU.mult, ['CGT', 'ps2'], ['CV'])
                    cv3 = r3(CV[:, 0:ntok], W); ac3 = r3(ACC[:, 0:ntok], W)
                    TS('dve', ACC[:, 0:ntok], CV[:, 0:ntok], WSC3[:, j, 1:2], None, ALU.mult, None, ['CV', 'WSC'], ['ACC'])
                    STT(ac3[:, :, 1:W], cv3[:, :, 0:W - 1], WSC3[:, j, 0:1], ac3[:, :, 1:W], ALU.mult, ALU.add, ['CV', 'WSC', 'ACC'], ['ACC'])
                    STT(ac3[:, :, 0:W - 1], cv3[:, :, 1:W], WSC3[:, j, 2:3], ac3[:, :, 0:W - 1], ALU.mult, ALU.add, ['CV', 'WSC', 'ACC'], ['ACC'])
                    TT('dve', YCAT3[:, 4 + j, tok0:tok0 + ntok], ACC[:, 0:ntok], BGT[:, 0:ntok], ALU.mult, ['ACC', 'BGT'], ['YCAT'])
                    zmm(12 + j, 3)
                    ACT(SIG[:, 0:ntok], PS[3][:, 0:ntok], AF.Sigmoid, ['ps3'], ['SIG'])
                    zmm(10 + j, 4)
                    TT('dve', GLU3[:, j, tok0:tok0 + ntok], SIG[:, 0:ntok], PS[4][:, 0:ntok], ALU.mult, ['SIG', 'ps4'], ['GLU'])
            if stop == 'b2':
                S.barrier()
                break
            WDW = alloc(64)
            WDW3 = r3(WDW[:, 0:62], 31)
            for c_ in range(2):
                S.dma('sp', WDW3[:, c_, :], w_dw[l][:, c_ * 128:(c_ + 1) * 128].rearrange("k p -> p k"), writes=['WDW'], allow_slow_non_contiguous=True)
            CFV = alloc(8)
            CFV3 = r3(CFV[:, 0:6], 2)
            for i_, src in enumerate((b_dw, ln_cf_g, ln_cf_b)):
                S.dma('sp', CFV3[:, i_, :], src[l].rearrange("(c p) -> p c", p=128), writes=['CFV'], allow_slow_non_contiguous=True)
            ONES = alloc(128)
            MEMSET('pool', ONES, 1.0 / 256.0, ['ONES'])
            TC = alloc(2 * NTOK); TC3 = r3(TC, NTOK)
            SQ = alloc(2 * 512); SQ3 = r3(SQ, 512)
            S.barrier()
            GLB = WST[:, 0:NTOK].bitcast(BF16); GLB3 = r3(GLB, NTOK)
            DG3 = r3(WINR[:, 0:62 * 128], 128)
            for j in range(2):
                CP('act', GLB3[:, j, :], GLU3[:, j, :], ['GLU'], ['GLB'])
                for k in range(31):
                    TS('dve', DG3[:, j * 31 + k, :], IDENT, WDW3[:, j, k:k + 1], None, ALU.mult, None, ['IDENT', 'WDW'], ['DG'])
            cbank = 0
            taporder = [15] + [k for k in range(31) if k != 15]
            for j in range(2):
                pb = 2 + cbank % 4; cbank += 1
                for idx, k in enumerate(taporder):
                    dlt = k - 15
                    lo = max(0, -dlt); hi = min(NCTX, NCTX - dlt)
                    MM(PS[pb][:, lo:hi], DG3[:, j * 31 + k, :], GLB3[:, j, lo + dlt:hi + dlt], idx == 0, idx == 30,
                       ['DG', 'GLB'], [f'ps{pb}'])
                ACT(TC3[:, j, 0:NCTX], PS[pb][:, 0:NCTX], AF.Identity, [f'ps{pb}', 'CFV'], ['TC'], bias=CFV3[:, 0, j:j + 1])
                for b in range(4):
                    pb = 2 + cbank % 4; cbank += 1
                    taps = []
                    for k in taporder:
                        dlt = k - 15
                        lo = max(8 * b, -dlt); hi = min(8 * b + 8, 32 - dlt)
                        if hi > lo:
                            taps.append((k, dlt, lo, hi))
                    for idx, (k, dlt, lo, hi) in enumerate(taps):
                        MM(PS[pb][:, (lo - 8 * b) * 64:(hi - 8 * b) * 64], DG3[:, j * 31 + k, :],
                           GLB3[:, j, NCTX + (lo + dlt) * 64:NCTX + (hi + dlt) * 64], idx == 0, idx == len(taps) - 1,
                           ['DG', 'GLB'], [f'ps{pb}'])
                    ACT(TC3[:, j, NCTX + b * 512:NCTX + (b + 1) * 512], PS[pb], AF.Identity, [f'ps{pb}', 'CFV'], ['TC'],
                        bias=CFV3[:, 0, j:j + 1])
            MEAN = alloc(512); RSTD = alloc(512); TN = alloc(512)
            for (tok0, ntok) in BLOCKS:
                for j in range(2):
                    ACT(SQ3[:, j, 0:ntok], TC3[:, j, tok0:tok0 + ntok], AF.Square, ['TC'], ['SQ'])
                for j in range(2):
                    MM(PS[0][:, 0:ntok], ONES, TC3[:, j, tok0:tok0 + ntok], j == 0, j == 1, ['ONES', 'TC'], ['ps0'])
                for j in range(2):
                    MM(PS[1][:, 0:ntok], ONES, SQ3[:, j, 0:ntok], j == 0, j == 1, ['ONES', 'SQ'], ['ps1'])
                CP('act', MEAN[:, 0:ntok], PS[0][:, 0:ntok], ['ps0'], ['MEAN'])
                TT('dve', RSTD[:, 0:ntok], MEAN[:, 0:ntok], MEAN[:, 0:ntok], ALU.mult, ['MEAN'], ['RSTD'])
                TT('dve', RSTD[:, 0:ntok], PS[1][:, 0:ntok], RSTD[:, 0:ntok], ALU.subtract, ['ps1', 'RSTD'], ['RSTD'])
                ACT(RSTD[:, 0:ntok], RSTD[:, 0:ntok], AF.Sqrt, ['RSTD', 'EPS'], ['RSTD'], bias=EPS[:, 0:1])
                OP('dve', 'reciprocal', ['RSTD'], ['RSTD'], out=RSTD[:, 0:ntok], in_=RSTD[:, 0:ntok])
                for j in range(2):
                    TT('dve', TN[:, 0:ntok], TC3[:, j, tok0:tok0 + ntok], MEAN[:, 0:ntok], ALU.subtract, ['TC', 'MEAN'], ['TN'])
                    TT('dve', TN[:, 0:ntok], TN[:, 0:ntok], RSTD[:, 0:ntok], ALU.mult, ['TN', 'RSTD'], ['TN'])
                    ACT(YCAT3[:, 6 + j, tok0:tok0 + ntok], TN[:, 0:ntok], AF.Silu, ['TN', 'CFV'], ['YCAT'],
                        scale=CFV3[:, 1, j:j + 1], bias=CFV3[:, 2, j:j + 1])
            S.barrier()

            if stop == 'c':
                break
            bump[0] = lay_mark
            G1 = modtile(2, 'G1')
            LG = alloc(1024); LB_ = alloc(1024)
            load_bc(LG, ln1_g[l:l + 1, :], 'LG'); load_bc(LB_, ln1_b[l:l + 1, :], 'LB_')
            WO_ST = alloc(8 * 512); WO = alloc(8 * 1024, BF16); WO3 = r3(WO, 1024)
            for hf in range(2):
                S.dma('sp', r3(WO_ST, 512), w_o[l, :, hf * 512:(hf + 1) * 512].rearrange("(kc k) f -> k kc f", k=128), writes=['WOST'])
                CP('act', WO3[:, :, hf * 512:(hf + 1) * 512], r3(WO_ST, 512), ['WOST'], ['WO'])
            XT = [alloc(1024), alloc(1024)]
            RT = [alloc(1024), alloc(1024)]
            ST6 = alloc(16); MV = alloc(8)

            def layer_norm_tile(rt, rk, lg, lb, gkeys):
                OP('dve', 'bn_stats', [rk], ['ST6'], out=ST6[:, 0:6], in_=rt[:, 0:512])
                OP('dve', 'bn_stats', [rk], ['ST6'], out=ST6[:, 6:12], in_=rt[:, 512:1024])
                OP('dve', 'bn_aggr', ['ST6'], ['MV'], out=MV[:, 0:2], in_=ST6[:, 0:12])
                ACT(MV[:, 2:3], MV[:, 1:2], AF.Sqrt, ['MV', 'EPS'], ['MV'], bias=EPS[:, 0:1])
                OP('dve', 'reciprocal', ['MV'], ['MV'], out=MV[:, 3:4], in_=MV[:, 2:3])
                STT(MV[:, 4:5], MV[:, 0:1], -1.0, MV[:, 3:4], ALU.mult, ALU.mult, ['MV'], ['MV'])
                ACT(rt, rt, AF.Identity, [rk, 'MV'], [rk], scale=MV[:, 3:4], bias=MV[:, 4:5])
                TT('dve', rt, rt, lg, ALU.mult, [rk] + gkeys, [rk])
                TT('pool', rt, rt, lb, ALU.add, [rk] + gkeys, [rk])

            tiles_E = range(18) if not last else range(2, 18)
            for ti in tiles_E:
                kd = kind_of(ti)
                xt = XT[ti % 2]; xk = f'XT{ti % 2}'
                rt = RT[ti % 2]; rk = f'RT{ti % 2}'
                S.dma('sp', xt, xs[ti * 128:(ti + 1) * 128, :], reads=['xs', 'xs2'], writes=[xk])
                for hf in range(2):
                    pb = (ti % 2) * 2 + hf
                    for kc in range(8):
                        MM(PS[pb], YCAT3[:, kc, ti * 128:(ti + 1) * 128], WO3[:, kc, hf * 512:(hf + 1) * 512], kc == 0, kc == 7,
                           ['YCAT', 'WO'], [f'ps{pb}'])
                    TT('dve', rt[:, hf * 512:(hf + 1) * 512], PS[pb], G1[kd][:, hf * 512:(hf + 1) * 512], ALU.mult,
                       [f'ps{pb}', f'G1{kd}'], [rk])
                STT(rt, xt, DN_ALPHA, rt, ALU.mult, ALU.add, [xk, rk], [rk])
                layer_norm_tile(rt, rk, LG, LB_, ['LG', 'LB_'])
                S.dma('pool', xs[ti * 128:(ti + 1) * 128, :], rt, reads=[rk], writes=[f'xs_t{ti}'])
            S.barrier()
            if dbg and l == 0:
                S.dma('sp', dbg_out[1, :, 0:D], xs, reads=[], writes=['dbg1'])
                S.barrier()
                if stop == 'ln1':
                    break

            bump[0] = persist_mark
            limit[0] = ARENA_COLS
            SH2 = modtile(3, 'SH2'); SC2 = modtile(4, 'SC2')
            for kind in range(2):
                TS('dve', SC2[kind], SC2[kind], 1.0, None, ALU.add, None, [f'SC2{kind}'], [f'SC2{kind}'])
            tiles_F = list(range(18)) if not last else list(range(2, 18))
            blocks_F = BLOCKS if not last else BLOCKS[1:]
            H2T = alloc(8 * NTOK, BF16); H2T3 = r3(H2T, NTOK)
            RW = alloc(18 * 16); RW3 = r3(RW, 16)
            WR = alloc(8 * 20); WR3 = r3(WR, 20)
            S.dma('sp', WR3[:, :, 0:4], w_rg[l].rearrange("(kc k) f -> k kc f", k=128), writes=['WR'])
            S.dma('sp', WR3[:, :, 4:20], w_rexp[l].rearrange("(kc k) f -> k kc f", k=128), writes=['WR'])
            BR = alloc(24)
            S.dma('sp', BR[:, 0:4], b_rg[l:l + 1, :].partition_broadcast(128), writes=['BR'])
            S.dma('sp', BR[:, 4:20], b_rexp[l:l + 1, :].partition_broadcast(128), writes=['BR'])
            RWT = alloc(NTOK, BF16)
            SEL = alloc(16 * 128, BF16); SEL3 = r3(SEL, 128)
            XTB = alloc(2048)
            XT = [XTB[:, 0:1024], XTB[:, 1024:2048]]
            SELF3 = r3(XTB, 128)
            CP('pool', SELF3[0:32], IDENT[0:32, 0:16].unsqueeze(2).to_broadcast([32, 16, 128]), ['IDENT'], ['XT0', 'XT1'])
            TT('pool', SELF3[0:32], SELF3[0:32], IDENT[0:32, 16:32].unsqueeze(2).to_broadcast([32, 16, 128]), ALU.add, ['IDENT', 'XT0', 'XT1'], ['XT0', 'XT1'])
            CP('pool', SEL3[0:32], SELF3[0:32], ['XT0', 'XT1'], ['SEL'])
            H32 = alloc(1024); H32_3 = r3(H32, 128)
            SM = alloc(64)
            LGT = SM[:, 0:20]; MG = SM[:, 20:24]; PEN = SM[:, 24:28]; SC_ = SM[:, 28:40]; EG = SM[:, 40:44]
            EM = alloc(16); EM2 = alloc(16); MK1 = alloc(16); MK2 = alloc(16)
            def f1_stageA(ti):
                kd = kind_of(ti)
                xt = XT[ti % 2]; xk = f'XT{ti % 2}'
                S.dma('sp', xt, xs[ti * 128:(ti + 1) * 128, :], reads=[f'xs_t{ti}'], writes=[xk])
                TT('dve', xt, xt, SC2[kd], ALU.mult, [xk, f'SC2{kd}'], [xk])
                TT('pool', xt, xt, SH2[kd], ALU.add, [xk, f'SH2{kd}'], [xk])

            def f1_stageA2(ti):
                xt = XT[ti % 2]; xk = f'XT{ti % 2}'
                for hh in range(2):
                    pb = 6 + hh
                    for k4 in range(4):
                        kc = hh * 4 + k4
                        TR(PS[pb][:, k4 * 128:(k4 + 1) * 128], xt[:, kc * 128:(kc + 1) * 128], [xk], [f'ps{pb}'])
                    CP('act', H2T3[:, hh * 4:(hh + 1) * 4, ti * 128:(ti + 1) * 128], r3(PS[pb], 128), [f'ps{pb}'], ['H2T'])
                    CP('act', H32_3[:, hh * 4:(hh + 1) * 4, :], r3(PS[pb], 128), [f'ps{pb}'], ['H32'])
                for kc in range(8):
                    MM(PS[5][:, 0:20], H32_3[:, kc, :], WR3[:, kc, :], kc == 0, kc == 7, ['H32', 'WR'], ['ps5'])

            NT = 18
            LGA = alloc(NT * 20); LGA3 = r3(LGA, 20)
            MEMSET('pool', LGA, 0.0, ['LGA'])

            def f1_stageB(ti):
                TT('dve', LGA3[:, ti, :], PS[5][:, 0:20], BR[:, 0:20], ALU.add, ['ps5', 'BR'], ['LGA'])

            f1_stageA(tiles_F[0])
            f1_stageA2(tiles_F[0])
            for ix, ti in enumerate(tiles_F):
                if ix + 1 < len(tiles_F):
                    f1_stageA(tiles_F[ix + 1])
                f1_stageB(ti)
                if ix + 1 < len(tiles_F):
                    f1_stageA2(tiles_F[ix + 1])
            def bc2(v, n):
                return v.unsqueeze(2).to_broadcast([128, NT, n])
            GMX = alloc(NT); GSUM = alloc(NT); GW = alloc(NT); M1 = alloc(NT); M2 = alloc(NT); DL = alloc(NT); EX = alloc(NT)
            P1 = alloc(NT); P2 = alloc(NT)
            MGA = alloc(NT * 4); D4 = alloc(NT * 4); PENA = alloc(NT * 4)
            EMA = alloc(NT * 16); EM2A = alloc(NT * 16); MK1A = alloc(NT * 16); MK2A = alloc(NT * 16)
            RW2 = alloc(NT * 32); RW2_3 = r3(RW2, 32)
            RWH = alloc(NT * 16, BF16)
            G4 = LGA3[:, :, 0:4]
            OP('dve', 'tensor_reduce', ['LGA'], ['GMX'], out=GMX, in_=G4, axis=AX.X, op=ALU.max)
            TT('dve', r3(MGA, 4), G4, bc2(GMX, 4), ALU.is_ge, ['LGA', 'GMX'], ['MGA'])
            TT('dve', r3(D4, 4), G4, bc2(GMX, 4), ALU.subtract, ['LGA', 'GMX'], ['D4'])
            ACT(D4, D4, AF.Exp, ['D4'], ['D4'])
            OP('dve', 'tensor_reduce', ['D4'], ['GSUM'], out=GSUM, in_=r3(D4, 4), axis=AX.X, op=ALU.add)
            OP('dve', 'reciprocal', ['GSUM'], ['GW'], out=GW, in_=GSUM)
            TS('dve', PENA, MGA, -1.0, 1e30, ALU.add, ALU.mult, ['MGA'], ['PENA'])
            TT('dve', r4(EMA, 4, 4), LGA3[:, :, 4:20].rearrange("p t (g j) -> p t g j", j=4),
               r3(PENA, 4).unsqueeze(3).to_broadcast([128, NT, 4, 4]), ALU.add, ['LGA', 'PENA'], ['EMA'])
            OP('dve', 'tensor_reduce', ['EMA'], ['M1'], out=M1, in_=r3(EMA, 16), axis=AX.X, op=ALU.max)
            TT('dve', r3(MK1A, 16), r3(EMA, 16), bc2(M1, 16), ALU.is_ge, ['EMA', 'M1'], ['MK1A'])
            STT(EM2A, MK1A, -1e30, EMA, ALU.mult, ALU.add, ['MK1A', 'EMA'], ['EM2A'])
            OP('dve', 'tensor_reduce', ['EM2A'], ['M2'], out=M2, in_=r3(EM2A, 16), axis=AX.X, op=ALU.max)
            TT('dve', r3(MK2A, 16), r3(EM2A, 16), bc2(M2, 16), ALU.is_ge, ['EM2A', 'M2'], ['MK2A'])
            TT('dve', DL, M2, M1, ALU.subtract, ['M1', 'M2'], ['DL'])
            ACT(EX, DL, AF.Exp, ['DL'], ['EX'])
            TS('dve', P1, EX, 1.0, None, ALU.add, None, ['EX'], ['P1'])
            OP('dve', 'reciprocal', ['P1'], ['P1'], out=P1, in_=P1)
            TT('dve', P2, EX, P1, ALU.mult, ['EX', 'P1'], ['P2'])
            TT('dve', P1, P1, GW, ALU.mult, ['P1', 'GW'], ['P1'])
            TT('dve', P2, P2, GW, ALU.mult, ['P2', 'GW'], ['P2'])
            TT('dve', r3(MK1A, 16), r3(MK1A, 16), bc2(P1, 16), ALU.mult, ['MK1A', 'P1'], ['MK1A'])
            TT('dve', r3(MK2A, 16), r3(MK2A, 16), bc2(P2, 16), ALU.mult, ['MK2A', 'P2'], ['MK2A'])
            TT('dve', MK1A, MK1A, MK2A, ALU.add, ['MK1A', 'MK2A'], ['MK1A'])
            CP('dve', RWH, MK1A, ['MK1A'], ['RWH'])
            CP('dve', RW2_3[:, :, 0:16], r3(RWH, 16), ['RWH'], ['RW2'])
            TT('dve', RW2_3[:, :, 16:32], r3(MK1A, 16), RW2_3[:, :, 0:16], ALU.subtract, ['MK1A', 'RW2'], ['RW2'])
            for ti in tiles_F:
                TR(PS[4][0:32, 0:128], RW2_3[:, ti, :], ['RW2'], ['ps4'])
                CP('act', RWT[0:32, ti * 128:(ti + 1) * 128], PS[4][0:32, 0:128], ['ps4'], ['RWT'])
            if stop in ('f1', 'f1a', 'f1b', 'f1c', 'f1d', 'f1a0', 'f1a1'):
                S.barrier()
                break
            S.barrier()
            FACC = alloc(18 * 1024); FACC3 = r3(FACC, 1024)
            EST = [alloc(2048), alloc(2048)]
            WGa_ = [alloc(2048, BF16), SC2[0].bitcast(BF16)]
            WUp_ = [alloc(2048, BF16), SC2[1].bitcast(BF16)]
            WDn_ = [alloc(2048, BF16), H32.bitcast(BF16)]

            def load_expert(e_):
                wp = e_ % 2
                S.dma('sp', r3(EST[0], 256), w_gate[l, e_].rearrange("(kc k) f -> k kc f", k=128), writes=['EST0'])
                CP('pool', WGa_[wp], EST[0], ['EST0'], [f'WGa{wp}'])
                S.dma('act', r3(EST[1], 256), w_up[l, e_].rearrange("(kc k) f -> k kc f", k=128), writes=['EST1'])
                CP('pool', WUp_[wp], EST[1], ['EST1'], [f'WUp{wp}'])
                S.dma('sp', r3(EST[0], 1024), w_down[l, e_].rearrange("(fc f) d -> f fc d", f=128), writes=['EST0'])
                CP('pool', WDn_[wp], EST[0], ['EST0'], [f'WDn{wp}'])
            SIL = [alloc(512), alloc(512)]
            ACTT = alloc(2 * 512, BF16); ACTT3 = r3(ACTT, 512)
            load_expert(0)
            for e_ in range(NE):
                wp = e_ % 2
                WGa3 = r3(WGa_[wp], 256); WUp3 = r3(WUp_[wp], 256); WDn3 = r3(WDn_[wp], 1024)
                if e_ + 1 < NE:
                    load_expert(e_ + 1)
                for (tok0, ntok) in blocks_F:
                    for fc in range(2):
                        for kc in range(8):
                            MM(PS[fc][:, 0:ntok], WGa3[:, kc, fc * 128:(fc + 1) * 128], H2T3[:, kc, tok0:tok0 + ntok], kc == 0, kc == 7,
                               [f'WGa{wp}', 'H2T'], [f'ps{fc}'])
                        for kc in range(8):
                            MM(PS[2 + fc][:, 0:ntok], WUp3[:, kc, fc * 128:(fc + 1) * 128], H2T3[:, kc, tok0:tok0 + ntok], kc == 0, kc == 7,
                               [f'WUp{wp}', 'H2T'], [f'ps{2 + fc}'])
                        if fc == 0:
                            MM(PS[4][:, 0:ntok], SEL3[0:32, e_, :], RWT[0:32, tok0:tok0 + ntok], True, True, ['SEL', 'RWT'], ['ps4'])
                        ACT(SIL[fc][:, 0:ntok], PS[fc][:, 0:ntok], AF.Silu, [f'ps{fc}'], [f'SIL{fc}'])
                        TT('dve', SIL[fc][:, 0:ntok], SIL[fc][:, 0:ntok], PS[2 + fc][:, 0:ntok], ALU.mult,
                           [f'SIL{fc}', f'ps{2 + fc}'], [f'SIL{fc}'])
                        TT('dve', ACTT3[:, fc, 0:ntok], SIL[fc][:, 0:ntok], PS[4][:, 0:ntok], ALU.mult,
                           [f'SIL{fc}', 'ps4'], [f'ACTT{fc}'])
                    for i in range(ntok // 128):
                        ti = tok0 // 128 + i
                        for hf in range(2):
                            pb = 5 + (i * 2 + hf) % 3
                            for fc in range(2):
                                MM(PS[pb], ACTT3[:, fc, i * 128:(i + 1) * 128], WDn3[:, fc, hf * 512:(hf + 1) * 512], fc == 0, fc == 1,
                                   [f'ACTT{fc}', f'WDn{wp}'], [f'ps{pb}'])
                            fk = f'FACC{ti}'
                            if e_ == 0:
                                CP('act', FACC3[:, ti, hf * 512:(hf + 1) * 512], PS[pb], [f'ps{pb}'], [fk])
                            else:
                                TT('dve', FACC3[:, ti, hf * 512:(hf + 1) * 512], FACC3[:, ti, hf * 512:(hf + 1) * 512], PS[pb], ALU.add,
                                   [f'ps{pb}', fk], [fk])
            S.barrier()
            if stop == 'f2':
                break
            G2 = SH2
            for kind in range(2):
                load_bc(G2[kind], modv[l, kind:kind + 1, 5 * 1024:6 * 1024], f'G2{kind}')
            LG2 = EST[0][:, 0:1024]; LB2 = EST[0][:, 1024:2048]
            load_bc(LG2, ln2_g[l:l + 1, :], 'LG2'); load_bc(LB2, ln2_b[l:l + 1, :], 'LB2')
            ST6 = alloc(16); MV = alloc(8)
            for ti in tiles_F:
                kd = kind_of(ti)
                xt = XT[ti % 2]; xk = f'XT{ti % 2}'
                S.dma('sp', xt, xs[ti * 128:(ti + 1) * 128, :], reads=[f'xs_t{ti}'], writes=[xk])
                fk = f'FACC{ti}'
                ft = FACC3[:, ti, :]
                TT('pool', ft, ft, G2[kd], ALU.mult, [fk, f'G2{kd}'], [fk])
                STT(ft, xt, DN_ALPHA, ft, ALU.mult, ALU.add, [xk, fk], [fk])
                if stop == 'l0dbg':
                    S.dma('sp', dbg_out[2, 0:128, 0:1024], ft, reads=[fk], writes=['dbgx'])
                    S.dma('sp', dbg_out[2, 128:256, 0:1024], xt, reads=[xk], writes=['dbgx2'])
                    S.dma('sp', dbg_out[2, 256:384, 0:288], RW, reads=['RW'], writes=['dbgx3'])
                    break
                layer_norm_tile(ft, fk, LG2, LB2, ['LG2', 'LB2'])
                if last:
                    S.dma('pool', out[(ti - 2) * 128:(ti - 1) * 128, :], ft, reads=[fk], writes=[f'out{ti}'])
                else:
                    S.dma('pool', xs[ti * 128:(ti + 1) * 128, :], ft, reads=[fk], writes=['xs'])
            S.barrier()
            if dbg and l == 0:
                S.dma('sp', dbg_out[2, :, 0:D], xs, reads=[], writes=['dbg2'])
                S.barrier()
                if stop in ('l0', 'l0dbg'):
                    break
        S.barrier()
        S.emit()
        print("n ops", S.nops, {k: len(v) for k, v in S.ops.items()})
    return nc


_NC_CACHE = {}


def make_in_maps(inputs):
    ident = np.eye(128, dtype=np.float32)
    x = np.asarray(inputs['x'], np.float32)
    c = np.asarray(inputs['c'], np.float32)
    ctx = np.asarray(inputs['ctx'], np.float32)
    c_ctx = np.asarray(inputs['c_ctx'], np.float32)
    wnames = ['w_mod', 'b_mod', 'w_in', 's5_a_re', 's5_a_im', 's5_log_dt', 's5_b_re', 's5_b_im', 's5_c_re', 's5_c_im',
              's5_d', 'w_glu', 'b_glu', 'w_sc', 'w_dw', 'b_dw', 'ln_cf_g', 'ln_cf_b', 'w_o', 'ln1_g', 'ln1_b',
              'w_rg', 'b_rg', 'w_rexp', 'b_rexp', 'w_gate', 'w_up', 'w_down', 'ln2_g', 'ln2_b']
    shared = {n: np.ascontiguousarray(np.asarray(inputs[n], np.float32)) for n in wnames}
    maps = []
    for b in range(x.shape[0]):
        m = dict(shared)
        m['xin'] = np.ascontiguousarray(np.concatenate([ctx[b], x[b]], axis=0))
        cv = np.stack([c[b], c_ctx], axis=0)
        m['cT'] = np.ascontiguousarray(cv.reshape(2, 8, 128).transpose(2, 1, 0))
        m['ident'] = ident
        maps.append(m)
    return maps


def kernel(**inputs):
    if 'nc' not in _NC_CACHE:
        _NC_CACHE['nc'] = build_nc()
    nc = _NC_CACHE['nc']
    maps = make_in_maps(inputs)
    res = run_bass_kernel_spmd(nc, maps, core_ids=list(range(8)))
    return np.stack([np.asarray(r['out'], np.float32) for r in res.results], axis=0)
```

```python
import math
from contextlib import ExitStack
import numpy as np
import concourse.bass as bass
import concourse.mybir as mybir
from concourse.bass_utils import run_bass_kernel_spmd

F32 = mybir.dt.float32
BF16 = mybir.dt.bfloat16
I32 = mybir.dt.int32
AF = mybir.ActivationFunctionType
ALU = mybir.AluOpType
AX = mybir.AxisListType

D = 1024
DEPTH = 2
NTOK = 2304
NCTX = 256
NLAT = 2048
D_IN = 1792
NE = 16
DN_ALPHA = (2 * DEPTH) ** 0.25
LN_EPS = 1e-5
NCH = 288
BLOCKS = [(0, 256)] + [(256 + 512 * i, 512) for i in range(4)]
TWO_PI = 2.0 * math.pi
C1 = 6.28125
C2 = TWO_PI - C1


class Sched:
    ENG = ('pe', 'dve', 'act', 'pool', 'sp')

    def __init__(self, nc, stack):
        self.nc = nc
        self.ops = {e: [] for e in self.ENG}
        self.sem = {}
        for e in ('pe', 'dve', 'act', 'pool'):
            self.sem['c_' + e] = stack.enter_context(nc.semaphore('c_' + e))
        self.NDMA = 12
        self.rr = {}
        for q in ('sp', 'act', 'pool'):
            self.rr[q] = 0
            for i in range(self.NDMA):
                self.sem[f'd_{q}{i}'] = stack.enter_context(nc.semaphore(f'd_{q}{i}'))
        self.cnt = {k: 0 for k in self.sem}
        self.lastw = {}
        self.readers = {}
        self.waited = {e: {} for e in self.ENG}
        self.nops = 0
        self.debug = False

    def _deps(self, eng, reads, writes, extra=()):
        deps = {}
        for s, v in extra:
            deps[s] = v

        def add(s, v):
            if deps.get(s, 0) < v:
                deps[s] = v
        for k in reads:
            if k in self.lastw:
                add(*self.lastw[k])
        for k in writes:
            if k in self.lastw:
                add(*self.lastw[k])
            for s, v in self.readers.get(k, {}).items():
                add(s, v)
        waits = []
        w = self.waited[eng]
        for s, v in deps.items():
            if eng == 'pe' and s == 'c_pe':
                continue
            if w.get(s, 0) < v:
                w[s] = v
                waits.append((s, v))
        return waits

    def _mark(self, me, reads, writes):
        s, v = me
        for k in reads:
            self.readers.setdefault(k, {})[s] = v
        for k in writes:
            self.lastw[k] = me
            self.readers[k] = {}

    def op(self, eng, fn, reads=(), writes=()):
        if self.debug:
            import sys as _s
            fr = _s._getframe(1)
            ln = []
            while fr is not None and len(ln) < 3:
                ln.append(fr.f_lineno)
                fr = fr.f_back
            fn0 = fn
            fn = lambda e, fn0=fn0, ln=tuple(ln): fn0(e).annotate(f"L{ln}")
        waits = self._deps(eng, reads, writes)
        s = 'c_' + eng
        self.cnt[s] += 1
        self.ops[eng].append((waits, fn, s, 1))
        self._mark((s, self.cnt[s]), reads, writes)
        self.nops += 1

    def dma(self, q, out, in_, reads=(), writes=(), **kw):
        i = self.rr[q]
        self.rr[q] = (i + 1) % self.NDMA
        s = f'd_{q}{i}'
        extra = [(s, self.cnt[s])] if self.cnt[s] > 0 else []
        waits = self._deps(q, reads, writes, extra)
        self.cnt[s] += 16
        self.ops[q].append((waits, lambda e: e.dma_start(out=out, in_=in_, **kw), s, 16))
        self._mark((s, self.cnt[s]), reads, writes)
        self.nops += 1

    def barrier(self):
        for e in self.ENG:
            waits = []
            w = self.waited[e]
            for s, v in self.cnt.items():
                if v > 0 and w.get(s, 0) < v:
                    w[s] = v
                    waits.append((s, v))
            if waits:
                self.ops[e].append((waits, None, None, 0))
        self.lastw = {}
        self.readers = {}

    def emit(self):
        nc = self.nc
        with nc.Block() as block:
            decos = {'pe': block.tensor, 'dve': block.vector, 'act': block.scalar,
                     'pool': block.gpsimd, 'sp': block.sync}
            for e in self.ENG:
                ops = self.ops[e]

                def body(engine, ops=ops):
                    for waits, fn, s, inc in ops:
                        for (ws, wv) in waits:
                            engine.wait_ge(self.sem[ws], wv)
                        if fn is not None:
                            fn(engine).then_inc(self.sem[s], inc)
                decos[e](body)


def build_nc(dbg=False, stop=None):
    nc = bass.Bass("TRN2", target_bir_lowering=False)
    di = {}

    def inp(name, shape):
        di[name] = nc.dram_tensor(name, list(shape), F32, kind="ExternalInput").ap()
        return di[name]
    L = DEPTH
    xin = inp('xin', (NTOK, D))
    cT = inp('cT', (128, 8, 2))
    ident_d = inp('ident', (128, 128))
    w_mod = inp('w_mod', (L, D, 6 * D)); b_mod = inp('b_mod', (L, 6 * D))
    w_in = inp('w_in', (L, D, D_IN))
    a_re = inp('s5_a_re', (L, 2, 32, 64)); a_im = inp('s5_a_im', (L, 2, 32, 64)); log_dt = inp('s5_log_dt', (L, 2, 32))
    b_re = inp('s5_b_re', (L, 2, 32, 64, 16)); b_im = inp('s5_b_im', (L, 2, 32, 64, 16))
    c_re = inp('s5_c_re', (L, 2, 32, 16, 64)); c_im = inp('s5_c_im', (L, 2, 32, 16, 64))
    s5_d = inp('s5_d', (L, 512)); w_glu = inp('w_glu', (L, 512, 512)); b_glu = inp('b_glu', (L, 512))
    w_sc = inp('w_sc', (L, 3, 256)); w_dw = inp('w_dw', (L, 31, 256)); b_dw = inp('b_dw', (L, 256))
    ln_cf_g = inp('ln_cf_g', (L, 256)); ln_cf_b = inp('ln_cf_b', (L, 256))
    w_o = inp('w_o', (L, D, D)); ln1_g = inp('ln1_g', (L, D)); ln1_b = inp('ln1_b', (L, D))
    w_rg = inp('w_rg', (L, D, 4)); b_rg = inp('b_rg', (L, 4)); w_rexp = inp('w_rexp', (L, D, 16)); b_rexp = inp('b_rexp', (L, 16))
    w_gate = inp('w_gate', (L, NE, D, 256)); w_up = inp('w_up', (L, NE, D, 256)); w_down = inp('w_down', (L, NE, 256, D))
    ln2_g = inp('ln2_g', (L, D)); ln2_b = inp('ln2_b', (L, D))
    out = nc.dram_tensor('out', [NLAT, D], F32, kind="ExternalOutput").ap()
    xs = nc.dram_tensor('xs', [NTOK, D], F32).ap()
    modv = nc.dram_tensor('modv', [L, 2, 6 * D], F32).ap()
    dbg_out = None
    if dbg:
        dbg_out = nc.dram_tensor('dbg', [3, NTOK, 9216], F32, kind="ExternalOutput").ap()

    with ExitStack() as st:
        S = Sched(nc, st)
        S.debug = dbg
        ARENA_COLS = 51000
        arena = st.enter_context(nc.sbuf_tensor("arena", [128, ARENA_COLS], F32))[:]
        PS = [st.enter_context(nc.psum_tensor(f"ps{i}", [128, 512], F32)) for i in range(8)]
        PS = [p_[:] for p_ in PS]
        bump = [0]
        limit = [ARENA_COLS]
        YCAT_COLS = 8 * NTOK // 2

        def alloc(cols, dt=F32, shape=None):
            c32 = cols if dt == F32 else (cols + 1) // 2
            c32 = (c32 + 7) // 8 * 8
            o = bump[0]
            bump[0] += c32
            assert bump[0] <= limit[0], (bump[0], limit[0])
            v = arena[:, o:o + c32]
            if dt != F32:
                v = v.bitcast(dt)
            v = v[:, 0:cols]
            return v

        def r3(v, b):
            return v.rearrange("p (a b) -> p a b", b=b)

        def r4(v, b, c):
            return v.rearrange("p (a b c) -> p a b c", b=b, c=c)

        IDENT = alloc(128)
        EPS = alloc(8)
        S.dma('sp', IDENT, ident_d, writes=['IDENT'])
        S.op('pool', lambda e: e.memset(EPS, LN_EPS), writes=['EPS'])
        CT = alloc(16)
        SCT = alloc(16)
        persist_mark = bump[0]

        def TT(eng, out_, a, b, op, reads, writes):
            S.op(eng, lambda e: e.tensor_tensor(out=out_, in0=a, in1=b, op=op), reads, writes)

        def TS(eng, out_, a, s1, s2, op0, op1, reads, writes):
            if op1 is None:
                S.op(eng, lambda e: e.tensor_scalar(out=out_, in0=a, scalar1=s1, scalar2=None, op0=op0), reads, writes)
            else:
                S.op(eng, lambda e: e.tensor_scalar(out=out_, in0=a, scalar1=s1, scalar2=s2, op0=op0, op1=op1), reads, writes)

        def STT(out_, a, sc, b, op0, op1, reads, writes):
            S.op('dve', lambda e: e.scalar_tensor_tensor(out=out_, in0=a, scalar=sc, in1=b, op0=op0, op1=op1), reads, writes)

        def ACT(out_, in_, func, reads, writes, scale=None, bias=None, accum_out=None):
            kw = {}
            if scale is not None:
                kw['scale'] = scale
            if bias is not None:
                kw['bias'] = bias
            if accum_out is not None:
                kw['accum_out'] = accum_out
            S.op('act', lambda e: e.activation(out=out_, in_=in_, func=func, **kw), reads, writes)

        def CP(eng, out_, in_, reads, writes):
            if eng == 'act':
                S.op('act', lambda e: e.copy(out=out_, in_=in_), reads, writes)
            else:
                S.op(eng, lambda e: e.tensor_copy(out=out_, in_=in_), reads, writes)

        def MM(out_, lhsT, rhs, start, stop, reads, writes):
            S.op('pe', lambda e: e.matmul(out_, lhsT=lhsT, rhs=rhs, start=start, stop=stop), reads, writes)

        def TR(out_, in_, reads, writes):
            n = in_.shape[0]
            S.op('pe', lambda e: e.transpose(out=out_, in_=in_, identity=IDENT[0:n, 0:n]), list(reads) + ['IDENT'], writes)

        def OP(eng, meth, reads, writes, **kw):
            S.op(eng, lambda e: getattr(e, meth)(**kw), reads, writes)

        def MEMSET(eng, ap, val, writes):
            S.op(eng, lambda e: e.memset(ap, val), (), writes)

        S.dma('sp', xs[0:1152], xin[0:1152], writes=['xs'])
        S.dma('pool', xs[1152:2304], xin[1152:2304], writes=['xs2'])
        S.dma('sp', CT, cT.rearrange("k a r -> k (a r)"), writes=['CT'])
        ACT(SCT, CT, AF.Silu, ['CT'], ['SCT'])
        SCT3 = r3(SCT, 2)
        def emit_mod(l_, WM, MODSB, BM, sfx):
            for half in range(2):
                S.dma('pool', BM[0:2, :], b_mod[l_:l_ + 1, half * 3072:(half + 1) * 3072].partition_broadcast(2), writes=['BM' + sfx])
                for kc in range(8):
                    wb = WM[kc % 2]
                    S.dma('sp' if kc % 2 == 0 else 'act', wb, w_mod[l_, kc * 128:(kc + 1) * 128, half * 3072:(half + 1) * 3072],
                          writes=[f'WM{kc % 2}' + sfx])
                    for n in range(6):
                        MM(PS[n][0:2, :], SCT3[:, kc, :], wb[:, n * 512:(n + 1) * 512], kc == 0, kc == 7,
                           ['SCT', f'WM{kc % 2}' + sfx], [f'ps{n}'])
                for n in range(6):
                    CP('act', MODSB[0:2, n * 512:(n + 1) * 512], PS[n][0:2, :], [f'ps{n}'], ['MODSB' + sfx])
                TT('pool', MODSB[0:2, :], MODSB[0:2, :], BM[0:2, :], ALU.add, ['MODSB' + sfx, 'BM' + sfx], ['MODSB' + sfx])
                S.dma('sp', modv[l_, :, half * 3072:(half + 1) * 3072], MODSB[0:2, :], reads=['MODSB' + sfx], writes=['modv'])

        WM = [alloc(3072), alloc(3072)]
        MODSB = alloc(3072)
        BM = alloc(3072)
        emit_mod(0, WM, MODSB, BM, '')
        S.barrier()

        for l in range(L):
            if stop == 'p0':
                break
            last = (l == L - 1)
            bump[0] = persist_mark

            def load_bc(dst, src_row, key):
                S.dma('sp', dst, src_row.partition_broadcast(128), reads=['modv'], writes=[key])

            def modtile(j, key):
                ts = []
                for kind in range(2):
                    t = alloc(1024)
                    load_bc(t, modv[l, kind:kind + 1, j * 1024:(j + 1) * 1024], f'{key}{kind}')
                    ts.append(t)
                return ts

            def kind_of(ti):
                return 1 if ti < 2 else 0

            YCAT = arena[:, ARENA_COLS - YCAT_COLS:ARENA_COLS].bitcast(BF16); YCAT3 = r3(YCAT, NTOK)
            lay_mark = bump[0]
            limit[0] = ARENA_COLS

            UT = alloc(4 * NTOK, BF16); UT3 = r3(UT, NTOK)
            YACC = alloc(4 * NTOK); YACC3 = r3(YACC, NTOK)
            s5_mark = bump[0]
            SH1 = modtile(0, 'SH1'); SC1 = modtile(1, 'SC1')
            for kind in range(2):
                TS('dve', SC1[kind], SC1[kind], 1.0, None, ALU.add, None, [f'SC1{kind}'], [f'SC1{kind}'])
            if stop == 'b1a':
                break
            WST = alloc(8 * 512)
            WST3 = r3(WST, 512)
            WINU = alloc(8 * 512, BF16); WINU3 = r3(WINU, 512)
            S.dma('sp', WST3, w_in[l, :, 0:512].rearrange("(kc k) f -> k kc f", k=128), writes=['WST'])
            CP('act', WINU, WST, ['WST'], ['WINU'])
            DSK = alloc(4)
            S.dma('sp', DSK, s5_d[l].rearrange("(c p) -> p c", p=128), writes=['DSK'], allow_slow_non_contiguous=True)
            if stop == 'b1b':
                break
            XT = [alloc(1024), alloc(1024)]
            HTs3 = [r3(alloc(8 * 512, BF16), 512), r3(alloc(8 * 512, BF16), 512)]

            def make_hT(tok0, ntok, sh, sc, shk, sck, hpar=0):
                HT3 = HTs3[hpar]
                for i in range(ntok // 128):
                    ti = (tok0 // 128) + i
                    kd = kind_of(ti)
                    xt = XT[ti % 2]
                    xk = f'XT{ti % 2}'
                    S.dma('sp', xt, xs[ti * 128:(ti + 1) * 128, :], reads=['xs', 'xs2'], writes=[xk])
                    TT('dve', xt, xt, sc[kd], ALU.mult, [xk, f'{sck}{kd}'], [xk])
                    TT('pool', xt, xt, sh[kd], ALU.add, [xk, f'{shk}{kd}'], [xk])
                    for hh in range(2):
                        pb = 6 + hh
                        for k4 in range(4):
                            kc = hh * 4 + k4
                            TR(PS[pb][:, k4 * 128:(k4 + 1) * 128], xt[:, kc * 128:(kc + 1) * 128], [xk], [f'ps{pb}'])
                        CP('act', HT3[:, hh * 4:(hh + 1) * 4, i * 128:(i + 1) * 128],
                           r3(PS[pb], 128), [f'ps{pb}'], [f'HT{hpar}'])

            make_hT(BLOCKS[0][0], BLOCKS[0][1], SH1, SC1, 'SH1', 'SC1', 0)
            for bix, (tok0, ntok) in enumerate(BLOCKS):
                hpar = bix % 2
                HT3 = HTs3[hpar]
                if bix + 1 < len(BLOCKS):
                    make_hT(BLOCKS[bix + 1][0], BLOCKS[bix + 1][1], SH1, SC1, 'SH1', 'SC1', (bix + 1) % 2)
                if stop == 'b1c':
                    break
                for fc in range(4):
                    pb = fc % 2
                    for kc in range(8):
                        MM(PS[pb][:, 0:ntok], WINU3[:, kc, fc * 128:(fc + 1) * 128], HT3[:, kc, 0:ntok], kc == 0, kc == 7,
                           ['WINU', f'HT{hpar}'], [f'ps{pb}'])
                    CP('act', UT3[:, fc, tok0:tok0 + ntok], PS[pb][:, 0:ntok], [f'ps{pb}'], ['UT'])
                    if stop == 'b1d':
                        continue
                    ACT(YACC3[:, fc, tok0:tok0 + ntok], PS[pb][:, 0:ntok], AF.Copy, [f'ps{pb}', 'DSK'], ['YACC'], scale=DSK[:, fc:fc + 1])
                if stop in ('b1d', 'b1e'):
                    break
            S.barrier()
            bump[0] = s5_mark
            if stop in ('b1', 'b1c', 'b1d', 'b1e'):
                break

            def T32():
                return alloc(32)
            NAT = alloc(128)
            NAT2 = alloc(8)
            ARE = T32(); AIM = T32(); DT = T32()

            def load_T(dst, src2d, key):
                S.dma('sp', NAT[0:32, :], src2d, writes=['NAT'])
                TR(PS[0][:, 0:32], NAT[0:32, :], ['NAT'], ['ps0'])
                CP('act', dst, PS[0][:, 0:32], ['ps0'], [key])
            load_T(ARE, a_re[l].rearrange("d (pr g2) p -> (d pr) (g2 p)", g2=2), 'ARE')
            load_T(AIM, a_im[l].rearrange("d (pr g2) p -> (d pr) (g2 p)", g2=2), 'AIM')
            S.dma('sp', NAT2[0:32, 0:2], log_dt[l].rearrange("d (pr g2) -> (d pr) g2", g2=2), writes=['NAT2'])
            CP('dve', r3(NAT[0:32, :], 64), NAT2[0:32, 0:2].unsqueeze(2).to_broadcast([32, 2, 64]), ['NAT2', 'NAT'], ['NAT'])
            TR(PS[0][:, 0:32], NAT[0:32, :], ['NAT'], ['ps0'])
            ACT(DT, PS[0][:, 0:32], AF.Exp, ['ps0'], ['DT'])
            RHO = T32(); TH = T32(); KF = T32(); KI = alloc(32).bitcast(I32); TMP = T32(); TMP2 = T32()
            TT('dve', RHO, ARE, DT, ALU.mult, ['ARE', 'DT'], ['RHO'])
            TT('dve', TH, AIM, DT, ALU.mult, ['AIM', 'DT'], ['TH'])
            TS('dve', TMP, TH, 1.0 / TWO_PI, None, ALU.mult, None, ['TH'], ['TMP'])
            CP('dve', KI, TMP, ['TMP'], ['KI'])
            CP('dve', KF, KI, ['KI'], ['KF'])
            STT(TMP, KF, -C1, TH, ALU.mult, ALU.add, ['KF', 'TH'], ['TMP'])
            STT(TMP2, KF, -C2, TMP, ALU.mult, ALU.add, ['KF', 'TMP'], ['TMP2'])
            SHh = T32(); CHh = T32(); HPI = alloc(8)
            MEMSET('pool', HPI, math.pi / 2, ['HPI'])
            ACT(SHh, TMP2, AF.Sin, ['TMP2'], ['SHh'], scale=0.5)
            ACT(CHh, TMP2, AF.Sin, ['TMP2', 'HPI'], ['CHh'], scale=-0.5, bias=HPI[:, 0:1])
            C1T = T32(); S1T = T32(); R1 = T32()
            TT('dve', TMP, CHh, CHh, ALU.mult, ['CHh'], ['TMP'])
            TT('dve', TMP2, SHh, SHh, ALU.mult, ['SHh'], ['TMP2'])
            TT('dve', C1T, TMP, TMP2, ALU.subtract, ['TMP', 'TMP2'], ['C1T'])
            TT('dve', TMP, SHh, CHh, ALU.mult, ['SHh', 'CHh'], ['TMP'])
            TS('dve', S1T, TMP, 2.0, None, ALU.mult, None, ['TMP'], ['S1T'])
            ACT(R1, RHO, AF.Exp, ['RHO'], ['R1'])
            ER = alloc(16 * 32); EI = alloc(16 * 32); ER3 = r3(ER, 32); EI3 = r3(EI, 32)
            MEMSET('pool', ER3[:, 0, :], 1.0, ['ER'])
            MEMSET('pool', EI3[:, 0, :], 0.0, ['EI'])
            for s in range(15):
                TT('dve', TMP, ER3[:, s, :], C1T, ALU.mult, ['ER', 'C1T'], ['TMP'])
                TT('dve', TMP2, EI3[:, s, :], S1T, ALU.mult, ['EI', 'S1T'], ['TMP2'])
                TT('dve', ER3[:, s + 1, :], TMP, TMP2, ALU.subtract, ['TMP', 'TMP2'], ['ER'])
                TT('dve', TMP, ER3[:, s, :], S1T, ALU.mult, ['ER', 'S1T'], ['TMP'])
                TT('dve', TMP2, EI3[:, s, :], C1T, ALU.mult, ['EI', 'C1T'], ['TMP2'])
                TT('dve', EI3[:, s + 1, :], TMP, TMP2, ALU.add, ['TMP', 'TMP2'], ['EI'])
            RP = alloc(8 * 32); RP3 = r3(RP, 32)
            for k in range(1, 9):
                ACT(RP3[:, k - 1, :], RHO, AF.Exp, ['RHO'], ['RP'], scale=float(k))
            QR = T32(); QI = T32(); NR = T32(); NI = T32(); DEN = T32()
            TT('dve', TMP, R1, C1T, ALU.mult, ['R1', 'C1T'], ['TMP'])
            TS('dve', NR, TMP, -1.0, None, ALU.add, None, ['TMP'], ['NR'])
            TT('dve', NI, R1, S1T, ALU.mult, ['R1', 'S1T'], ['NI'])
            TT('dve', TMP, ARE, ARE, ALU.mult, ['ARE'], ['TMP'])
            TT('dve', TMP2, AIM, AIM, ALU.mult, ['AIM'], ['TMP2'])
            TT('dve', DEN, TMP, TMP2, ALU.add, ['TMP', 'TMP2'], ['DEN'])
            OP('dve', 'reciprocal', ['DEN'], ['DEN'], out=DEN, in_=DEN)
            TT('dve', TMP, NR, ARE, ALU.mult, ['NR', 'ARE'], ['TMP'])
            TT('dve', TMP2, NI, AIM, ALU.mult, ['NI', 'AIM'], ['TMP2'])
            TT('dve', TMP, TMP, TMP2, ALU.add, ['TMP', 'TMP2'], ['TMP'])
            TT('dve', QR, TMP, DEN, ALU.mult, ['TMP', 'DEN'], ['QR'])
            TT('dve', TMP, NI, ARE, ALU.mult, ['NI', 'ARE'], ['TMP'])
            TT('dve', TMP2, NR, AIM, ALU.mult, ['NR', 'AIM'], ['TMP2'])
            TT('dve', TMP, TMP, TMP2, ALU.subtract, ['TMP', 'TMP2'], ['TMP'])
            TT('dve', QI, TMP, DEN, ALU.mult, ['TMP', 'DEN'], ['QI'])
            FR = alloc(256); FI = alloc(256); ELR = alloc(256); ELI = alloc(256); KR = alloc(256); KIm = alloc(256)
            FR3 = r3(FR, 8); FI3 = r3(FI, 8); ELR3 = r3(ELR, 8); ELI3 = r3(ELI, 8); KR3 = r3(KR, 8); KI3 = r3(KIm, 8)
            for s in range(8):
                TT('dve', TMP, ER3[:, s, :], QR, ALU.mult, ['ER', 'QR'], ['TMP'])
                TT('dve', TMP2, EI3[:, s, :], QI, ALU.mult, ['EI', 'QI'], ['TMP2'])
                TT('dve', FR3[:, :, s], TMP, TMP2, ALU.add, ['TMP', 'TMP2'], ['FR'])
                TT('dve', TMP, ER3[:, s, :], QI, ALU.mult, ['ER', 'QI'], ['TMP'])
                TT('dve', TMP2, EI3[:, s, :], QR, ALU.mult, ['EI', 'QR'], ['TMP2'])
                TT('dve', FI3[:, :, s], TMP, TMP2, ALU.subtract, ['TMP', 'TMP2'], ['FI'])
                CP('pool', ELR3[:, :, s], ER3[:, s, :], ['ER'], ['ELR'])
                CP('pool', ELI3[:, :, s], EI3[:, s, :], ['EI'], ['ELI'])
                TT('dve', KR3[:, :, s], ER3[:, s + 8, :], RP3[:, s, :], ALU.mult, ['ER', 'RP'], ['KR'])
                TT('dve', KI3[:, :, s], EI3[:, s + 8, :], RP3[:, s, :], ALU.mult, ['EI', 'RP'], ['KI_'])
            LRR = alloc(64); LII = alloc(64); LRR3 = r3(LRR, 2); LII3 = r3(LII, 2)
            for c in range(2):
                TT('dve', LRR3[:, :, c], ER3[:, 8, :], RP3[:, 7, :], ALU.mult, ['ER', 'RP'], ['LRR'])
                TT('dve', LII3[:, :, c], EI3[:, 8, :], RP3[:, 7, :], ALU.mult, ['EI', 'RP'], ['LII'])
            BRE = alloc(1024); BIM = alloc(1024); CTR = alloc(1024); CTI = alloc(1024)
            BRE3 = r3(BRE, 32); BIM3 = r3(BIM, 32); CTR3 = r3(CTR, 32); CTI3 = r3(CTI, 32)
            CN = alloc(32 * 128); CN3 = r3(CN, 128)
            for (dst3, src, key) in ((BRE3, b_re, 'BRE'), (BIM3, b_im, 'BIM')):
                MEMSET('pool', dst3, 0.0, [key])
                v = src[l].rearrange("d (pr g2) p n -> g2 p (d pr) n", g2=2)
                for g2 in range(2):
                    S.dma('sp', dst3[g2 * 64:(g2 + 1) * 64, :, g2 * 16:(g2 + 1) * 16], v[g2], writes=[key])
            for (dst3, src, key) in ((CTR3, c_re, 'CTR'), (CTI3, c_im, 'CTI')):
                MEMSET('pool', CN3[0:32], 0.0, ['CN'])
                v = src[l].rearrange("d (pr g2) n p -> g2 n (d pr) p", g2=2)
                for g2 in range(2):
                    S.dma('sp', CN3[g2 * 16:(g2 + 1) * 16, :, g2 * 64:(g2 + 1) * 64], v[g2], writes=['CN'])
                for ub in range(2):
                    for uu in range(16):
                        u = ub * 16 + uu
                        TR(PS[ub][:, uu * 32:(uu + 1) * 32], CN3[0:32, u, :], ['CN'], [f'ps{ub}'])
                    CP('act', dst3[:, ub * 16:(ub + 1) * 16, :], r3(PS[ub], 32), [f'ps{ub}'], [key])
            if stop == 'tab':
                break
            XZ = alloc(32 * 289 * 2, BF16); XZ4 = r4(XZ, 289, 2)
            MEMSET('pool', XZ, 0.0, ['XZ'])
            WB = [alloc(8 * 128), alloc(8 * 128)]
            T1 = alloc(256); T2 = alloc(256)
            LBm = {}
            LCm = {}
            for d in range(2):
                for ri in range(2):
                    LBm[d, ri] = alloc(8 * 128, BF16)
                    LCm[d, ri] = alloc(8 * 128, BF16)
            DEC = [alloc(512), alloc(512)]

            def bc_s(tab3, u):
                return tab3[:, u, :].unsqueeze(2).to_broadcast([128, 8, 32])

            def bc_m(mat3, u):
                return mat3[:, u, :].unsqueeze(1).to_broadcast([128, 8, 32])

            def cplx_outer(eng, out_re, out_im, Ar, Ai, Br, Bi, u, keys_r, neg_im, okeys):
                t1 = r3(T1, 32); t2 = r3(T2, 32)
                TT(eng, t1, bc_s(Ar, u), bc_m(Br, u), ALU.mult, keys_r, ['T1'])
                TT(eng, t2, bc_s(Ai, u), bc_m(Bi, u), ALU.mult, keys_r, ['T2'])
                TT(eng, out_re, t1, t2, ALU.subtract, ['T1', 'T2'], [okeys[0]])
                TT(eng, t1, bc_s(Ar, u), bc_m(Bi, u), ALU.mult, keys_r, ['T1'])
                TT(eng, t2, bc_s(Ai, u), bc_m(Br, u), ALU.mult, keys_r, ['T2'])
                if neg_im:
                    S.op('dve', lambda e: e.scalar_tensor_tensor(out=out_im, in0=t1, scalar=-1.0, in1=t2, op0=ALU.mult, op1=ALU.subtract),
                         ['T1', 'T2'], [okeys[1]])
                else:
                    TT(eng, out_im, t1, t2, ALU.add, ['T1', 'T2'], [okeys[1]])

            tabkeys = ['FR', 'FI', 'ELR', 'ELI', 'KR', 'KI_', 'BRE', 'BIM', 'CTR', 'CTI']

            def gen_unit(d, pair, carry):
                u = d * 16 + pair
                q = pair % 4
                win = slice(32 * q, 32 * q + 32)
                if not carry:
                    WBr3 = r3(WB[0], 128); WBi3 = r3(WB[1], 128)
                    cplx_outer('dve', WBr3[:, :, win], WBi3[:, :, win], FR3, FI3, BRE3, BIM3, u, tabkeys, False, ['WB0', 'WB1'])
                    for ri in range(2):
                        w3 = r3(WB[ri], 128)
                        for hb in range(2):
                            pb = 6 + hb
                            for s4 in range(4):
                                s = hb * 4 + s4
                                TR(PS[pb][:, s4 * 128:(s4 + 1) * 128], w3[:, s, :], [f'WB{ri}'], [f'ps{pb}'])
                            CP('act', LBm[d, ri][:, hb * 512:(hb + 1) * 512], PS[pb], [f'ps{pb}'], [f'LB{d}{ri}'])
                    lc_r = r3(LCm[d, 0], 128); lc_i = r3(LCm[d, 1], 128)
                    cplx_outer('dve', lc_r[:, :, win], lc_i[:, :, win], ELR3, ELI3, CTR3, CTI3, u, tabkeys, True, [f'LC{d}0', f'LC{d}1'])
                else:
                    lc_r = r3(LCm[d, 0], 128); lc_i = r3(LCm[d, 1], 128)
                    cplx_outer('dve', lc_r[:, :, win], lc_i[:, :, win], KR3, KI3, CTR3, CTI3, u, tabkeys, True, [f'LC{d}0', f'LC{d}1'])

            def zero_windows(carry):
                if not carry:
                    MEMSET('pool', WB[0], 0.0, ['WB0'])
                    MEMSET('pool', WB[1], 0.0, ['WB1'])
                for d in range(2):
                    for ri in range(2):
                        MEMSET('pool', LCm[d, ri], 0.0, [f'LC{d}{ri}'])

            def slots(d, bi, plus):
                tok0, ntok = BLOCKS[bi]
                nch = ntok // 8
                if d == 0:
                    jj0 = tok0 // 8
                    return slice(jj0 + plus, jj0 + plus + nch)
                if bi == 0:
                    hi = 31 + plus
                else:
                    cl0 = (tok0 - NCTX) // 8
                    hi = 287 - cl0 + plus
                lo = hi - nch
                return slice(hi, lo if lo >= 0 else None, -1)

            order = [(q, pair) for q in range(4) for pair in range(q, 16, 4)]
            def gen_B(d, pair):
                u = d * 16 + pair
                q = pair % 4
                win = slice(32 * q, 32 * q + 32)
                WBr3 = r3(WB[0], 128); WBi3 = r3(WB[1], 128)
                cplx_outer('dve', WBr3[:, :, win], WBi3[:, :, win], FR3, FI3, BRE3, BIM3, u, tabkeys, False, ['WB0', 'WB1'])
                for ri in range(2):
                    w3 = r3(WB[ri], 128)
                    for hb in range(2):
                        pb = 6 + hb
                        for s4 in range(4):
                            s_ = hb * 4 + s4
                            TR(PS[pb][:, s4 * 128:(s4 + 1) * 128], w3[:, s_, :], [f'WB{ri}'], [f'ps{pb}'])
                        CP('act', LBm[d, ri][:, hb * 512:(hb + 1) * 512], PS[pb], [f'ps{pb}'], [f'LB{d}{ri}'])
                dk = f'DEC{d}'
                CP('pool', r3(DEC[d], 8), R1[:, u:u + 1].unsqueeze(2).to_broadcast([128, 64, 8]), ['R1'], [dk])
                MEMSET('pool', r3(DEC[d], 8)[:, :, 0], 0.0, [dk])

            def gen_C(d, pair, carry, lcset, lckey):
                u = d * 16 + pair
                q = pair % 4
                win = slice(32 * q, 32 * q + 32)
                lc_r = r3(lcset[d, 0], 128); lc_i = r3(lcset[d, 1], 128)
                if carry:
                    cplx_outer('dve', lc_r[:, :, win], lc_i[:, :, win], KR3, KI3, CTR3, CTI3, u, tabkeys, True, [f'{lckey}{d}0', f'{lckey}{d}1'])
                else:
                    cplx_outer('dve', lc_r[:, :, win], lc_i[:, :, win], ELR3, ELI3, CTR3, CTI3, u, tabkeys, True, [f'{lckey}{d}0', f'{lckey}{d}1'])

            def emit_B(pair, bi, par):
                cc = pair // 4
                tok0, ntok = BLOCKS[bi]
                for d in range(2):
                    u = d * 16 + pair
                    for ri in range(2):
                        pb = 2 * d + ri
                        lb3 = r3(LBm[d, ri], 128)
                        for s_ in range(8):
                            tau = s_ if d == 0 else 7 - s_
                            MM(r3(PS[pb][:, 0:ntok], 8)[:, :, s_], lb3[:, s_, :],
                               r3(UT3[:, cc, tok0:tok0 + ntok], 8)[:, :, tau], True, True,
                               [f'LB{d}{ri}', 'UT'], [f'ps{pb}'])
                        bp = BPB[par, d, ri]; bk = f'BP{par}{d}{ri}'
                        CP('act', bp[:, 0:ntok], PS[pb][:, 0:ntok], [f'ps{pb}'], [bk])
                        g = GB[par, d, ri]; gk = f'G{par}{d}{ri}'
                        OP('dve', 'tensor_tensor_scan', [f'DEC{d}', bk], [gk],
                           out=g[:, 0:ntok], data0=DEC[d][:, 0:ntok], data1=bp[:, 0:ntok], initial=0.0,
                           op0=ALU.mult, op1=ALU.add)
                        CP('pool', XZ4[:, u, slots(d, bi, 1), ri], r3(g[:, 0:ntok], 8)[:, :, 7], [gk], ['XZ'])

            def emit_C(pair, bi, par):
                cc = pair // 4
                tok0, ntok = BLOCKS[bi]
                pby = 4 + par
                for d in range(2):
                    for ri in range(2):
                        lc3 = r3(LCm[d, ri], 128)
                        g = GB[par, d, ri]; gk = f'G{par}{d}{ri}'
                        for s_ in range(8):
                            tau = s_ if d == 0 else 7 - s_
                            first = (d == 0 and ri == 0 and s_ == 0)
                            lastmm = (d == 1 and ri == 1 and s_ == 7)
                            MM(r3(PS[pby][:, 0:ntok], 8)[:, :, tau], lc3[:, s_, :], r3(g[:, 0:ntok], 8)[:, :, s_],
                               first, lastmm, [f'LC{d}{ri}', gk], [f'ps{pby}'])
                TT('dve', YACC3[:, cc, tok0:tok0 + ntok], YACC3[:, cc, tok0:tok0 + ntok], PS[pby][:, 0:ntok], ALU.add,
                   ['YACC', f'ps{pby}'], ['YACC'])

            BPB = {}
            GB = {}
            s5blk0 = bump[0]
            for par in range(2):
                for d in range(2):
                    for ri in range(2):
                        BPB[par, d, ri] = alloc(512)
                        GB[par, d, ri] = alloc(512, BF16)
            MODSB1 = arena[:, s5blk0:s5blk0 + 3072]
            BM1 = arena[:, s5blk0 + 3072:s5blk0 + 6144]
            assert bump[0] - s5blk0 >= 6144
            curqB = -1; curqC = -1
            prev = None
            cnt_items = 0
            for (q, pair) in order:
                if q != curqB:
                    MEMSET('pool', WB[0], 0.0, ['WB0'])
                    MEMSET('pool', WB[1], 0.0, ['WB1'])
                    curqB = q
                for d in range(2):
                    gen_B(d, pair)
                for bi in range(len(BLOCKS)):
                    par = cnt_items % 2
                    cnt_items += 1
                    emit_B(pair, bi, par)
                    if prev is not None:
                        emit_C(*prev)
                    if bi == 0:
                        if q != curqC:
                            for d in range(2):
                                for ri in range(2):
                                    MEMSET('pool', LCm[d, ri], 0.0, [f'LC{d}{ri}'])
                            curqC = q
                        for d in range(2):
                            gen_C(d, pair, False, LCm, 'LC')
                    prev = (pair, bi, par)
            emit_C(*prev)
            if stop == 'pass1':
                break
            if l == 0:
                S.barrier()
                emit_mod(1, [UT.bitcast(F32)[:, 0:3072], CN[:, 0:3072]], MODSB1, BM1, 'L1')
            ZP = [alloc(64), alloc(64)]
            CA = alloc(64); CB = alloc(64)
            MEMSET('pool', ZP[0], 0.0, ['ZP0r', 'ZP0i'])
            CA3 = r3(CA, 2); CB3 = r3(CB, 2)
            CH_E = 'dve'
            for k in range(NCH):
                zp = ZP[k % 2]; zn = ZP[(k + 1) % 2]
                zpk = f'ZP{k % 2}'; znk = f'ZP{(k + 1) % 2}'
                zn3 = r3(zn, 2)
                xk = XZ4[:, :, k + 1, :]
                TT(CH_E, CA3, r3(zp, 2), LRR3, ALU.mult, [zpk + 'r', zpk + 'i', 'LRR'], ['CA'])
                TT(CH_E, CB3, r3(zp, 2), LII3, ALU.mult, [zpk + 'r', zpk + 'i', 'LII'], ['CB'])
                TT(CH_E, CA3, CA3, xk, ALU.add, ['CA', 'XZ'], ['CA'])
                TT(CH_E, zn3[:, :, 0], CA3[:, :, 0], CB3[:, :, 1], ALU.subtract, ['CA', 'CB'], [znk + 'r'])
                TT(CH_E, zn3[:, :, 1], CA3[:, :, 1], CB3[:, :, 0], ALU.add, ['CA', 'CB'], [znk + 'i'])
                CP('pool', xk, zn3, [znk + 'r', znk + 'i'], ['XZ' + str(k)])
            if stop == 'chain':
                break
            S.barrier()
            LCsets = [LCm, LBm]
            LCkeys = ['LC', 'LB']
            setq = [-1, -1]

            def gen_pair2(idx):
                q, pair = order[idx]
                pp = idx % 2
                if setq[pp] != q:
                    for d in range(2):
                        for ri in range(2):
                            MEMSET('pool', LCsets[pp][d, ri], 0.0, [f'{LCkeys[pp]}{d}{ri}'])
                    setq[pp] = q
                for d in range(2):
                    gen_C(d, pair, True, LCsets[pp], LCkeys[pp])
            gen_pair2(0)
            it2 = 0
            for idx, (q, pair) in enumerate(order):
                cc = pair // 4
                pp = idx % 2
                if idx + 1 < len(order):
                    gen_pair2(idx + 1)
                for bi, (tok0, ntok) in enumerate(BLOCKS):
                    pby = it2 % 4
                    it2 += 1
                    for d in range(2):
                        u = d * 16 + pair
                        for ri in range(2):
                            lc3 = r3(LCsets[pp][d, ri], 128)
                            for s_ in range(8):
                                tau = s_ if d == 0 else 7 - s_
                                first = (d == 0 and ri == 0 and s_ == 0)
                                lastmm = (d == 1 and ri == 1 and s_ == 7)
                                MM(r3(PS[pby][:, 0:ntok], 8)[:, :, tau], lc3[:, s_, :], XZ4[:, u, slots(d, bi, 0), ri],
                                   first, lastmm, [f'{LCkeys[pp]}{d}{ri}', 'XZ'], [f'ps{pby}'])
                    TT('dve', YACC3[:, cc, tok0:tok0 + ntok], YACC3[:, cc, tok0:tok0 + ntok], PS[pby][:, 0:ntok], ALU.add,
                       ['YACC', f'ps{pby}'], ['YACC'])
            S.barrier()
            if dbg and l == 0:
                for c in range(4):
                    S.dma('sp', dbg_out[0, 0:128, c * NTOK:(c + 1) * NTOK], YACC3[:, c, :], reads=['YACC'], writes=['dbg0'])
                S.barrier()
                if stop == 's5':
                    break
            bump[0] = s5_mark
            limit[0] = ARENA_COLS - YCAT_COLS
            WG_ST = alloc(4 * 512); WG = alloc(4 * 512, BF16); WG3 = r3(WG, 512)
            S.dma('sp', r3(WG_ST, 512), w_glu[l].rearrange("(kc k) f -> k kc f", k=128), writes=['WGST'])
            CP('act', WG, WG_ST, ['WGST'], ['WG'])
            BG = alloc(4)
            S.dma('sp', BG, b_glu[l].rearrange("(c p) -> p c", p=128), writes=['BG'], allow_slow_non_contiguous=True)
            GT = alloc(4 * NTOK, BF16); GT3 = r3(GT, NTOK)
            SG = [alloc(512), alloc(512)]
            for c in range(4):
                ACT(YACC3[:, c, :], YACC3[:, c, :], AF.Gelu, ['YACC'], ['YACC'])
                CP('pool', GT3[:, c, :], YACC3[:, c, :], ['YACC'], ['GT'])
            for (tok0, ntok) in BLOCKS:
                for oc in range(4):
                    pb = oc % 2
                    for kc in range(4):
                        MM(PS[pb][:, 0:ntok], WG3[:, kc, oc * 128:(oc + 1) * 128], GT3[:, kc, tok0:tok0 + ntok], kc == 0, kc == 3,
                           ['WG', 'GT'], [f'ps{pb}'])
                    ACT(SG[pb][:, 0:ntok], PS[pb][:, 0:ntok], AF.Sigmoid, [f'ps{pb}', 'BG'], [f'SG{pb}'], bias=BG[:, oc:oc + 1])
                    TT('dve', YCAT3[:, oc, tok0:tok0 + ntok], YACC3[:, oc, tok0:tok0 + ntok], SG[pb][:, 0:ntok], ALU.mult,
                       ['YACC', f'SG{pb}'], ['YCAT'])
            S.barrier()

            if stop == 'gate':
                break
            bump[0] = lay_mark
            SH1 = modtile(0, 'SH1'); SC1 = modtile(1, 'SC1')
            for kind in range(2):
                TS('dve', SC1[kind], SC1[kind], 1.0, None, ALU.add, None, [f'SC1{kind}'], [f'SC1{kind}'])
            GLU = alloc(2 * NTOK); GLU3 = r3(GLU, NTOK)
            NF2 = 1280
            WST = alloc(8 * 640); WST3 = r3(WST, 640)
            WINR = alloc(8 * NF2, BF16); WINR3 = r3(WINR, NF2)
            for hf in range(2):
                S.dma('sp', WST3, w_in[l, :, 512 + hf * 640:512 + (hf + 1) * 640].rearrange("(kc k) f -> k kc f", k=128), writes=['WST'])
                CP('act', WINR3[:, :, hf * 640:(hf + 1) * 640], WST3, ['WST'], ['WINR'])
            WSC = alloc(8)
            WSC3 = r3(WSC[:, 0:6], 3)
            for c_ in range(2):
                S.dma('sp', WSC3[:, c_, :], w_sc[l][:, c_ * 128:(c_ + 1) * 128].rearrange("k p -> p k"), writes=['WSC'], allow_slow_non_contiguous=True)
            XT = [alloc(1024), alloc(1024)]
            HTs3 = [r3(alloc(8 * 512, BF16), 512), r3(alloc(8 * 512, BF16), 512)]
            BGT = alloc(512); CGT = alloc(512); CV = alloc(512); ACC = alloc(512); SIG = alloc(512)
            make_hT(BLOCKS[0][0], BLOCKS[0][1], SH1, SC1, 'SH1', 'SC1', 0)
            for bix, (tok0, ntok) in enumerate(BLOCKS):
                hpar = bix % 2
                HT3 = HTs3[hpar]
                if bix + 1 < len(BLOCKS):
                    make_hT(BLOCKS[bix + 1][0], BLOCKS[bix + 1][1], SH1, SC1, 'SH1', 'SC1', (bix + 1) % 2)
                W = 64 if tok0 >= NCTX else 256
                nr = ntok // W

                def zmm(fc, pb):
                    f0 = fc * 128 - 512
                    for kc in range(8):
                        MM(PS[pb][:, 0:ntok], WINR3[:, kc, f0:f0 + 128], HT3[:, kc, 0:ntok], kc == 0, kc == 7,
                           ['WINR', f'HT{hpar}'], [f'ps{pb}'])
                for j in range(2):
                    zmm(4 + j, 0)
                    CP('act', BGT[:, 0:ntok], PS[0][:, 0:ntok], ['ps0'], ['BGT'])
                    zmm(6 + j, 1)
                    CP('act', CGT[:, 0:ntok], PS[1][:, 0:ntok], ['ps1'], ['CGT'])
                    zmm(8 + j, 2)
                    TT('dve', CV[:, 0:ntok], CGT[:, 0:ntok], PS[2][:, 0:ntok], ALU.mult, ['CGT', 'ps2'], ['CV'])
                    cv3 = r3(CV[:, 0:ntok], W); ac3 = r3(ACC[:, 0:ntok], W)
                    TS('dve', ACC[:, 0:ntok], CV[:, 0:ntok], WSC3[:, j, 1:2], None, ALU.mult, None, ['CV', 'WSC'], ['ACC'])
                    STT(ac3[:, :, 1:W], cv3[:, :, 0:W - 1], WSC3[:, j, 0:1], ac3[:, :, 1:W], ALU.mult, ALU.add, ['CV', 'WSC', 'ACC'], ['ACC'])
                    STT(ac3[:, :, 0:W - 1], cv3[:, :, 1:W], WSC3[:, j, 2:3], ac3[:, :, 0:W - 1], ALU.mult, ALU.add, ['CV', 'WSC', 'ACC'], ['ACC'])
                    TT('dve', YCAT3[:, 4 + j, tok0:tok0 + ntok], ACC[:, 0:ntok], BGT[:, 0:ntok], ALU.mult, ['ACC', 'BGT'], ['YCAT'])
                    zmm(12 + j, 3)
                    ACT(SIG[:, 0:ntok], PS[3][:, 0:ntok], AF.Sigmoid, ['ps3'], ['SIG'])
                    zmm(10 + j, 4)
                    TT('dve', GLU3[:, j, tok0:tok0 + ntok], SIG[:, 0:ntok], PS[4][:, 0:ntok], ALU.mult, ['SIG', 'ps4'], ['GLU'])
            if stop == 'b2':
                S.barrier()
                break
            WDW = alloc(64)
            WDW3 = r3(WDW[:, 0:62], 31)
            for c_ in range(2):
                S.dma('sp', WDW3[:, c_, :], w_dw[l][:, c_ * 128:(c_ + 1) * 128].rearrange("k p -> p k"), writes=['WDW'], allow_slow_non_contiguous=True)
            CFV = alloc(8)
            CFV3 = r3(CFV[:, 0:6], 2)
            for i_, src in enumerate((b_dw, ln_cf_g, ln_cf_b)):
                S.dma('sp', CFV3[:, i_, :], src[l].rearrange("(c p) -> p c", p=128), writes=['CFV'], allow_slow_non_contiguous=True)
            ONES = alloc(128)
            MEMSET('pool', ONES, 1.0 / 256.0, ['ONES'])
            TC = alloc(2 * NTOK); TC3 = r3(TC, NTOK)
            SQ = alloc(2 * 512); SQ3 = r3(SQ, 512)
            S.barrier()
            GLB = WST[:, 0:NTOK].bitcast(BF16); GLB3 = r3(GLB, NTOK)
            DG3 = r3(WINR[:, 0:62 * 128], 128)
            for j in range(2):
                CP('act', GLB3[:, j, :], GLU3[:, j, :], ['GLU'], ['GLB'])
                for k in range(31):
                    TS('dve', DG3[:, j * 31 + k, :], IDENT, WDW3[:, j, k:k + 1], None, ALU.mult, None, ['IDENT', 'WDW'], ['DG'])
            cbank = 0
            taporder = [15] + [k for k in range(31) if k != 15]
            for j in range(2):
                pb = 2 + cbank % 4; cbank += 1
                for idx, k in enumerate(taporder):
                    dlt = k - 15
                    lo = max(0, -dlt); hi = min(NCTX, NCTX - dlt)
                    MM(PS[pb][:, lo:hi], DG3[:, j * 31 + k, :], GLB3[:, j, lo + dlt:hi + dlt], idx == 0, idx == 30,
                       ['DG', 'GLB'], [f'ps{pb}'])
                ACT(TC3[:, j, 0:NCTX], PS[pb][:, 0:NCTX], AF.Identity, [f'ps{pb}', 'CFV'], ['TC'], bias=CFV3[:, 0, j:j + 1])
                for b in range(4):
                    pb = 2 + cbank % 4; cbank += 1
                    taps = []
                    for k in taporder:
                        dlt = k - 15
                        lo = max(8 * b, -dlt); hi = min(8 * b + 8, 32 - dlt)
                        if hi > lo:
                            taps.append((k, dlt, lo, hi))
                    for idx, (k, dlt, lo, hi) in enumerate(taps):
                        MM(PS[pb][:, (lo - 8 * b) * 64:(hi - 8 * b) * 64], DG3[:, j * 31 + k, :],
                           GLB3[:, j, NCTX + (lo + dlt) * 64:NCTX + (hi + dlt) * 64], idx == 0, idx == len(taps) - 1,
                           ['DG', 'GLB'], [f'ps{pb}'])
                    ACT(TC3[:, j, NCTX + b * 512:NCTX + (b + 1) * 512], PS[pb], AF.Identity, [f'ps{pb}', 'CFV'], ['TC'],
                        bias=CFV3[:, 0, j:j + 1])
            MEAN = alloc(512); RSTD = alloc(512); TN = alloc(512)
            for (tok0, ntok) in BLOCKS:
                for j in range(2):
                    ACT(SQ3[:, j, 0:ntok], TC3[:, j, tok0:tok0 + ntok], AF.Square, ['TC'], ['SQ'])
                for j in range(2):
                    MM(PS[0][:, 0:ntok], ONES, TC3[:, j, tok0:tok0 + ntok], j == 0, j == 1, ['ONES', 'TC'], ['ps0'])
                for j in range(2):
                    MM(PS[1][:, 0:ntok], ONES, SQ3[:, j, 0:ntok], j == 0, j == 1, ['ONES', 'SQ'], ['ps1'])
                CP('act', MEAN[:, 0:ntok], PS[0][:, 0:ntok], ['ps0'], ['MEAN'])
                TT('dve', RSTD[:, 0:ntok], MEAN[:, 0:ntok], MEAN[:, 0:ntok], ALU.mult, ['MEAN'], ['RSTD'])
                TT('dve', RSTD[:, 0:ntok], PS[1][:, 0:ntok], RSTD[:, 0:ntok], ALU.subtract, ['ps1', 'RSTD'], ['RSTD'])
                ACT(RSTD[:, 0:ntok], RSTD[:, 0:ntok], AF.Sqrt, ['RSTD', 'EPS'], ['RSTD'], bias=EPS[:, 0:1])
                OP('dve', 'reciprocal', ['RSTD'], ['RSTD'], out=RSTD[:, 0:ntok], in_=RSTD[:, 0:ntok])
                for j in range(2):
                    TT('dve', TN[:, 0:ntok], TC3[:, j, tok0:tok0 + ntok], MEAN[:, 0:ntok], ALU.subtract, ['TC', 'MEAN'], ['TN'])
                    TT('dve', TN[:, 0:ntok], TN[:, 0:ntok], RSTD[:, 0:ntok], ALU.mult, ['TN', 'RSTD'], ['TN'])
                    ACT(YCAT3[:, 6 + j, tok0:tok0 + ntok], TN[:, 0:ntok], AF.Silu, ['TN', 'CFV'], ['YCAT'],
                        scale=CFV3[:, 1, j:j + 1], bias=CFV3[:, 2, j:j + 1])
            S.barrier()

            if stop == 'c':
                break
            bump[0] = lay_mark
            G1 = modtile(2, 'G1')
            LG = alloc(1024); LB_ = alloc(1024)
            load_bc(LG, ln1_g[l:l + 1, :], 'LG'); load_bc(LB_, ln1_b[l:l + 1, :], 'LB_')
            WO_ST = alloc(8 * 512); WO = alloc(8 * 1024, BF16); WO3 = r3(WO, 1024)
            for hf in range(2):
                S.dma('sp', r3(WO_ST, 512), w_o[l, :, hf * 512:(hf + 1) * 512].rearrange("(kc k) f -> k kc f", k=128), writes=['WOST'])
                CP('act', WO3[:, :, hf * 512:(hf + 1) * 512], r3(WO_ST, 512), ['WOST'], ['WO'])
            XT = [alloc(1024), alloc(1024)]
            RT = [alloc(1024), alloc(1024)]
            ST6 = alloc(16); MV = alloc(8)

            def layer_norm_tile(rt, rk, lg, lb, gkeys):
                OP('dve', 'bn_stats', [rk], ['ST6'], out=ST6[:, 0:6], in_=rt[:, 0:512])
                OP('dve', 'bn_stats', [rk], ['ST6'], out=ST6[:, 6:12], in_=rt[:, 512:1024])
                OP('dve', 'bn_aggr', ['ST6'], ['MV'], out=MV[:, 0:2], in_=ST6[:, 0:12])
                ACT(MV[:, 2:3], MV[:, 1:2], AF.Sqrt, ['MV', 'EPS'], ['MV'], bias=EPS[:, 0:1])
                OP('dve', 'reciprocal', ['MV'], ['MV'], out=MV[:, 3:4], in_=MV[:, 2:3])
                STT(MV[:, 4:5], MV[:, 0:1], -1.0, MV[:, 3:4], ALU.mult, ALU.mult, ['MV'], ['MV'])
                ACT(rt, rt, AF.Identity, [rk, 'MV'], [rk], scale=MV[:, 3:4], bias=MV[:, 4:5])
                TT('dve', rt, rt, lg, ALU.mult, [rk] + gkeys, [rk])
                TT('pool', rt, rt, lb, ALU.add, [rk] + gkeys, [rk])

            tiles_E = range(18) if not last else range(2, 18)
            for ti in tiles_E:
                kd = kind_of(ti)
                xt = XT[ti % 2]; xk = f'XT{ti % 2}'
                rt = RT[ti % 2]; rk = f'RT{ti % 2}'
                S.dma('sp', xt, xs[ti * 128:(ti + 1) * 128, :], reads=['xs', 'xs2'], writes=[xk])
                for hf in range(2):
                    pb = (ti % 2) * 2 + hf
                    for kc in range(8):
                        MM(PS[pb], YCAT3[:, kc, ti * 128:(ti + 1) * 128], WO3[:, kc, hf * 512:(hf + 1) * 512], kc == 0, kc == 7,
                           ['YCAT', 'WO'], [f'ps{pb}'])
                    TT('dve', rt[:, hf * 512:(hf + 1) * 512], PS[pb], G1[kd][:, hf * 512:(hf + 1) * 512], ALU.mult,
                       [f'ps{pb}', f'G1{kd}'], [rk])
                STT(rt, xt, DN_ALPHA, rt, ALU.mult, ALU.add, [xk, rk], [rk])
                layer_norm_tile(rt, rk, LG, LB_, ['LG', 'LB_'])
                S.dma('pool', xs[ti * 128:(ti + 1) * 128, :], rt, reads=[rk], writes=[f'xs_t{ti}'])
            S.barrier()
            if dbg and l == 0:
                S.dma('sp', dbg_out[1, :, 0:D], xs, reads=[], writes=['dbg1'])
                S.barrier()
                if stop == 'ln1':
                    break

            bump[0] = persist_mark
            limit[0] = ARENA_COLS
            SH2 = modtile(3, 'SH2'); SC2 = modtile(4, 'SC2')
            for kind in range(2):
                TS('dve', SC2[kind], SC2[kind], 1.0, None, ALU.add, None, [f'SC2{kind}'], [f'SC2{kind}'])
            tiles_F = list(range(18)) if not last else list(range(2, 18))
            blocks_F = BLOCKS if not last else BLOCKS[1:]
            H2T = alloc(8 * NTOK, BF16); H2T3 = r3(H2T, NTOK)
            RW = alloc(18 * 16); RW3 = r3(RW, 16)
            WR = alloc(8 * 20); WR3 = r3(WR, 20)
            S.dma('sp', WR3[:, :, 0:4], w_rg[l].rearrange("(kc k) f -> k kc f", k=128), writes=['WR'])
            S.dma('sp', WR3[:, :, 4:20], w_rexp[l].rearrange("(kc k) f -> k kc f", k=128), writes=['WR'])
            BR = alloc(24)
            S.dma('sp', BR[:, 0:4], b_rg[l:l + 1, :].partition_broadcast(128), writes=['BR'])
            S.dma('sp', BR[:, 4:20], b_rexp[l:l + 1, :].partition_broadcast(128), writes=['BR'])
            RWT = alloc(NTOK, BF16)
            SEL = alloc(16 * 128, BF16); SEL3 = r3(SEL, 128)
            XTB = alloc(2048)
            XT = [XTB[:, 0:1024], XTB[:, 1024:2048]]
            SELF3 = r3(XTB, 128)
            CP('pool', SELF3[0:32], IDENT[0:32, 0:16].unsqueeze(2).to_broadcast([32, 16, 128]), ['IDENT'], ['XT0', 'XT1'])
            TT('pool', SELF3[0:32], SELF3[0:32], IDENT[0:32, 16:32].unsqueeze(2).to_broadcast([32, 16, 128]), ALU.add, ['IDENT', 'XT0', 'XT1'], ['XT0', 'XT1'])
            CP('pool', SEL3[0:32], SELF3[0:32], ['XT0', 'XT1'], ['SEL'])
            H32 = alloc(1024); H32_3 = r3(H32, 128)
            SM = alloc(64)
            LGT = SM[:, 0:20]; MG = SM[:, 20:24]; PEN = SM[:, 24:28]; SC_ = SM[:, 28:40]; EG = SM[:, 40:44]
            EM = alloc(16); EM2 = alloc(16); MK1 = alloc(16); MK2 = alloc(16)
            def f1_stageA(ti):
                kd = kind_of(ti)
                xt = XT[ti % 2]; xk = f'XT{ti % 2}'
                S.dma('sp', xt, xs[ti * 128:(ti + 1) * 128, :], reads=[f'xs_t{ti}'], writes=[xk])
                TT('dve', xt, xt, SC2[kd], ALU.mult, [xk, f'SC2{kd}'], [xk])
                TT('pool', xt, xt, SH2[kd], ALU.add, [xk, f'SH2{kd}'], [xk])

            def f1_stageA2(ti):
                xt = XT[ti % 2]; xk = f'XT{ti % 2}'
                for hh in range(2):
                    pb = 6 + hh
                    for k4 in range(4):
                        kc = hh * 4 + k4
                        TR(PS[pb][:, k4 * 128:(k4 + 1) * 128], xt[:, kc * 128:(kc + 1) * 128], [xk], [f'ps{pb}'])
                    CP('act', H2T3[:, hh * 4:(hh + 1) * 4, ti * 128:(ti + 1) * 128], r3(PS[pb], 128), [f'ps{pb}'], ['H2T'])
                    CP('act', H32_3[:, hh * 4:(hh + 1) * 4, :], r3(PS[pb], 128), [f'ps{pb}'], ['H32'])
                for kc in range(8):
                    MM(PS[5][:, 0:20], H32_3[:, kc, :], WR3[:, kc, :], kc == 0, kc == 7, ['H32', 'WR'], ['ps5'])

            NT = 18
            LGA = alloc(NT * 20); LGA3 = r3(LGA, 20)
            MEMSET('pool', LGA, 0.0, ['LGA'])

            def f1_stageB(ti):
                TT('dve', LGA3[:, ti, :], PS[5][:, 0:20], BR[:, 0:20], ALU.add, ['ps5', 'BR'], ['LGA'])

            f1_stageA(tiles_F[0])
            f1_stageA2(tiles_F[0])
            for ix, ti in enumerate(tiles_F):
                if ix + 1 < len(tiles_F):
                    f1_stageA(tiles_F[ix + 1])
                f1_stageB(ti)
                if ix + 1 < len(tiles_F):
                    f1_stageA2(tiles_F[ix + 1])
            def bc2(v, n):
                return v.unsqueeze(2).to_broadcast([128, NT, n])
            GMX = alloc(NT); GSUM = alloc(NT); GW = alloc(NT); M1 = alloc(NT); M2 = alloc(NT); DL = alloc(NT); EX = alloc(NT)
            P1 = alloc(NT); P2 = alloc(NT)
            MGA = alloc(NT * 4); D4 = alloc(NT * 4); PENA = alloc(NT * 4)
            EMA = alloc(NT * 16); EM2A = alloc(NT * 16); MK1A = alloc(NT * 16); MK2A = alloc(NT * 16)
            RW2 = alloc(NT * 32); RW2_3 = r3(RW2, 32)
            RWH = alloc(NT * 16, BF16)
            G4 = LGA3[:, :, 0:4]
            OP('dve', 'tensor_reduce', ['LGA'], ['GMX'], out=GMX, in_=G4, axis=AX.X, op=ALU.max)
            TT('dve', r3(MGA, 4), G4, bc2(GMX, 4), ALU.is_ge, ['LGA', 'GMX'], ['MGA'])
            TT('dve', r3(D4, 4), G4, bc2(GMX, 4), ALU.subtract, ['LGA', 'GMX'], ['D4'])
            ACT(D4, D4, AF.Exp, ['D4'], ['D4'])
            OP('dve', 'tensor_reduce', ['D4'], ['GSUM'], out=GSUM, in_=r3(D4, 4), axis=AX.X, op=ALU.add)
            OP('dve', 'reciprocal', ['GSUM'], ['GW'], out=GW, in_=GSUM)
            TS('dve', PENA, MGA, -1.0, 1e30, ALU.add, ALU.mult, ['MGA'], ['PENA'])
            TT('dve', r4(EMA, 4, 4), LGA3[:, :, 4:20].rearrange("p t (g j) -> p t g j", j=4),
               r3(PENA, 4).unsqueeze(3).to_broadcast([128, NT, 4, 4]), ALU.add, ['LGA', 'PENA'], ['EMA'])
            OP('dve', 'tensor_reduce', ['EMA'], ['M1'], out=M1, in_=r3(EMA, 16), axis=AX.X, op=ALU.max)
            TT('dve', r3(MK1A, 16), r3(EMA, 16), bc2(M1, 16), ALU.is_ge, ['EMA', 'M1'], ['MK1A'])
            STT(EM2A, MK1A, -1e30, EMA, ALU.mult, ALU.add, ['MK1A', 'EMA'], ['EM2A'])
            OP('dve', 'tensor_reduce', ['EM2A'], ['M2'], out=M2, in_=r3(EM2A, 16), axis=AX.X, op=ALU.max)
            TT('dve', r3(MK2A, 16), r3(EM2A, 16), bc2(M2, 16), ALU.is_ge, ['EM2A', 'M2'], ['MK2A'])
            TT('dve', DL, M2, M1, ALU.subtract, ['M1', 'M2'], ['DL'])
            ACT(EX, DL, AF.Exp, ['DL'], ['EX'])
            TS('dve', P1, EX, 1.0, None, ALU.add, None, ['EX'], ['P1'])
            OP('dve', 'reciprocal', ['P1'], ['P1'], out=P1, in_=P1)
            TT('dve', P2, EX, P1, ALU.mult, ['EX', 'P1'], ['P2'])
            TT('dve', P1, P1, GW, ALU.mult, ['P1', 'GW'], ['P1'])
            TT('dve', P2, P2, GW, ALU.mult, ['P2', 'GW'], ['P2'])
            TT('dve', r3(MK1A, 16), r3(MK1A, 16), bc2(P1, 16), ALU.mult, ['MK1A', 'P1'], ['MK1A'])
            TT('dve', r3(MK2A, 16), r3(MK2A, 16), bc2(P2, 16), ALU.mult, ['MK2A', 'P2'], ['MK2A'])
            TT('dve', MK1A, MK1A, MK2A, ALU.add, ['MK1A', 'MK2A'], ['MK1A'])
            CP('dve', RWH, MK1A, ['MK1A'], ['RWH'])
            CP('dve', RW2_3[:, :, 0:16], r3(RWH, 16), ['RWH'], ['RW2'])
            TT('dve', RW2_3[:, :, 16:32], r3(MK1A, 16), RW2_3[:, :, 0:16], ALU.subtract, ['MK1A', 'RW2'], ['RW2'])
            for ti in tiles_F:
                TR(PS[4][0:32, 0:128], RW2_3[:, ti, :], ['RW2'], ['ps4'])
                CP('act', RWT[0:32, ti * 128:(ti + 1) * 128], PS[4][0:32, 0:128], ['ps4'], ['RWT'])
            if stop in ('f1', 'f1a', 'f1b', 'f1c', 'f1d', 'f1a0', 'f1a1'):
                S.barrier()
                break
            S.barrier()
            FACC = alloc(18 * 1024); FACC3 = r3(FACC, 1024)
            EST = [alloc(2048), alloc(2048)]
            WGa_ = [alloc(2048, BF16), SC2[0].bitcast(BF16)]
            WUp_ = [alloc(2048, BF16), SC2[1].bitcast(BF16)]
            WDn_ = [alloc(2048, BF16), H32.bitcast(BF16)]

            def load_expert(e_):
                wp = e_ % 2
                S.dma('sp', r3(EST[0], 256), w_gate[l, e_].rearrange("(kc k) f -> k kc f", k=128), writes=['EST0'])
                CP('pool', WGa_[wp], EST[0], ['EST0'], [f'WGa{wp}'])
                S.dma('act', r3(EST[1], 256), w_up[l, e_].rearrange("(kc k) f -> k kc f", k=128), writes=['EST1'])
                CP('pool', WUp_[wp], EST[1], ['EST1'], [f'WUp{wp}'])
                S.dma('sp', r3(EST[0], 1024), w_down[l, e_].rearrange("(fc f) d -> f fc d", f=128), writes=['EST0'])
                CP('pool', WDn_[wp], EST[0], ['EST0'], [f'WDn{wp}'])
            SIL = [alloc(512), alloc(512)]
            ACTT = alloc(2 * 512, BF16); ACTT3 = r3(ACTT, 512)
            ACTTs3 = [ACTT3, r3(alloc(2 * 512, BF16), 512)]

            def exp_U(e_, bi, par):
                wp = e_ % 2
                WGa3 = r3(WGa_[wp], 256); WUp3 = r3(WUp_[wp], 256)
                tok0, ntok = blocks_F[bi]
                at3 = ACTTs3[par]
                for fc in range(2):
                    for kc in range(8):
                        MM(PS[fc][:, 0:ntok], WGa3[:, kc, fc * 128:(fc + 1) * 128], H2T3[:, kc, tok0:tok0 + ntok], kc == 0, kc == 7,
                           [f'WGa{wp}', 'H2T'], [f'ps{fc}'])
                    for kc in range(8):
                        MM(PS[2 + fc][:, 0:ntok], WUp3[:, kc, fc * 128:(fc + 1) * 128], H2T3[:, kc, tok0:tok0 + ntok], kc == 0, kc == 7,
                           [f'WUp{wp}', 'H2T'], [f'ps{2 + fc}'])
                    if fc == 0:
                        MM(PS[4][:, 0:ntok], SEL3[0:32, e_, :], RWT[0:32, tok0:tok0 + ntok], True, True, ['SEL', 'RWT'], ['ps4'])
                    ACT(SIL[fc][:, 0:ntok], PS[fc][:, 0:ntok], AF.Silu, [f'ps{fc}'], [f'SIL{fc}'])
                    TT('dve', SIL[fc][:, 0:ntok], SIL[fc][:, 0:ntok], PS[2 + fc][:, 0:ntok], ALU.mult,
                       [f'SIL{fc}', f'ps{2 + fc}'], [f'SIL{fc}'])
                    TT('dve', at3[:, fc, 0:ntok], SIL[fc][:, 0:ntok], PS[4][:, 0:ntok], ALU.mult,
                       [f'SIL{fc}', 'ps4'], [f'ACTT{par}{fc}'])

            dcount = [0]

            def exp_D(e_, bi, par):
                wp = e_ % 2
                WDn3 = r3(WDn_[wp], 1024)
                tok0, ntok = blocks_F[bi]
                at3 = ACTTs3[par]
                for i in range(ntok // 128):
                    ti = tok0 // 128 + i
                    for hf in range(2):
                        pb = 5 + dcount[0] % 3
                        dcount[0] += 1
                        for fc in range(2):
                            MM(PS[pb], at3[:, fc, i * 128:(i + 1) * 128], WDn3[:, fc, hf * 512:(hf + 1) * 512], fc == 0, fc == 1,
                               [f'ACTT{par}{fc}', f'WDn{wp}'], [f'ps{pb}'])
                        fk = f'FACC{ti}'
                        if e_ == 0:
                            CP('act', FACC3[:, ti, hf * 512:(hf + 1) * 512], PS[pb], [f'ps{pb}'], [fk])
                        else:
                            TT('dve', FACC3[:, ti, hf * 512:(hf + 1) * 512], FACC3[:, ti, hf * 512:(hf + 1) * 512], PS[pb], ALU.add,
                               [f'ps{pb}', fk], [fk])

            items_x = [(e_, bi) for e_ in range(NE) for bi in range(len(blocks_F))]
            load_expert(0)
            prev_x = None
            for ix, (e_, bi) in enumerate(items_x):
                exp_U(e_, bi, ix % 2)
                if prev_x is not None:
                    exp_D(*prev_x)
                if bi == 0 and e_ + 1 < NE:
                    load_expert(e_ + 1)
                prev_x = (e_, bi, ix % 2)
            exp_D(*prev_x)
            S.barrier()
            if stop == 'f2':
                break
            G2 = SH2
            for kind in range(2):
                load_bc(G2[kind], modv[l, kind:kind + 1, 5 * 1024:6 * 1024], f'G2{kind}')
            LG2 = EST[0][:, 0:1024]; LB2 = EST[0][:, 1024:2048]
            load_bc(LG2, ln2_g[l:l + 1, :], 'LG2'); load_bc(LB2, ln2_b[l:l + 1, :], 'LB2')
            ST6 = alloc(16); MV = alloc(8)
            for ti in tiles_F:
                kd = kind_of(ti)
                xt = XT[ti % 2]; xk = f'XT{ti % 2}'
                S.dma('sp', xt, xs[ti * 128:(ti + 1) * 128, :], reads=[f'xs_t{ti}'], writes=[xk])
                fk = f'FACC{ti}'
                ft = FACC3[:, ti, :]
                TT('pool', ft, ft, G2[kd], ALU.mult, [fk, f'G2{kd}'], [fk])
                STT(ft, xt, DN_ALPHA, ft, ALU.mult, ALU.add, [xk, fk], [fk])
                if stop == 'l0dbg':
                    S.dma('sp', dbg_out[2, 0:128, 0:1024], ft, reads=[fk], writes=['dbgx'])
                    S.dma('sp', dbg_out[2, 128:256, 0:1024], xt, reads=[xk], writes=['dbgx2'])
                    S.dma('sp', dbg_out[2, 256:384, 0:288], RW, reads=['RW'], writes=['dbgx3'])
                    break
                layer_norm_tile(ft, fk, LG2, LB2, ['LG2', 'LB2'])
                if last:
                    S.dma('pool', out[(ti - 2) * 128:(ti - 1) * 128, :], ft, reads=[fk], writes=[f'out{ti}'])
                else:
                    S.dma('pool', xs[ti * 128:(ti + 1) * 128, :], ft, reads=[fk], writes=['xs'])
            S.barrier()
            if dbg and l == 0:
                S.dma('sp', dbg_out[2, :, 0:D], xs, reads=[], writes=['dbg2'])
                S.barrier()
                if stop in ('l0', 'l0dbg'):
                    break
        S.barrier()
        S.emit()
        print("n ops", S.nops, {k: len(v) for k, v in S.ops.items()})
    return nc


_NC_CACHE = {}


def make_in_maps(inputs):
    ident = np.eye(128, dtype=np.float32)
    x = np.asarray(inputs['x'], np.float32)
    c = np.asarray(inputs['c'], np.float32)
    ctx = np.asarray(inputs['ctx'], np.float32)
    c_ctx = np.asarray(inputs['c_ctx'], np.float32)
    wnames = ['w_mod', 'b_mod', 'w_in', 's5_a_re', 's5_a_im', 's5_log_dt', 's5_b_re', 's5_b_im', 's5_c_re', 's5_c_im',
              's5_d', 'w_glu', 'b_glu', 'w_sc', 'w_dw', 'b_dw', 'ln_cf_g', 'ln_cf_b', 'w_o', 'ln1_g', 'ln1_b',
              'w_rg', 'b_rg', 'w_rexp', 'b_rexp', 'w_gate', 'w_up', 'w_down', 'ln2_g', 'ln2_b']
    shared = {n: np.ascontiguousarray(np.asarray(inputs[n], np.float32)) for n in wnames}
    maps = []
    for b in range(x.shape[0]):
        m = dict(shared)
        m['xin'] = np.ascontiguousarray(np.concatenate([ctx[b], x[b]], axis=0))
        cv = np.stack([c[b], c_ctx], axis=0)
        m['cT'] = np.ascontiguousarray(cv.reshape(2, 8, 128).transpose(2, 1, 0))
        m['ident'] = ident
        maps.append(m)
    return maps


def kernel(**inputs):
    if 'nc' not in _NC_CACHE:
        _NC_CACHE['nc'] = build_nc()
    nc = _NC_CACHE['nc']
    maps = make_in_maps(inputs)
    res = run_bass_kernel_spmd(nc, maps, core_ids=list(range(8)))
    return np.stack([np.asarray(r['out'], np.float32) for r in res.results], axis=0)
```

```python
import math
from contextlib import ExitStack
import numpy as np
import concourse.bass as bass
import concourse.mybir as mybir
from concourse.bass_utils import run_bass_kernel_spmd

F32 = mybir.dt.float32
BF16 = mybir.dt.bfloat16
I32 = mybir.dt.int32
AF = mybir.ActivationFunctionType
ALU = mybir.AluOpType
AX = mybir.AxisListType

D = 1024
DEPTH = 2
NTOK = 2304
NCTX = 256
NLAT = 2048
D_IN = 1792
NE = 16
DN_ALPHA = (2 * DEPTH) ** 0.25
LN_EPS = 1e-5
NCH = 288
BLOCKS = [(0, 256)] + [(256 + 512 * i, 512) for i in range(4)]
TWO_PI = 2.0 * math.pi
C1 = 6.28125
C2 = TWO_PI - C1


class Sched:
    ENG = ('pe', 'dve', 'act', 'pool', 'sp')

    def __init__(self, nc, stack):
        self.nc = nc
        self.ops = {e: [] for e in self.ENG}
        self.sem = {}
        for e in ('pe', 'dve', 'act', 'pool'):
            self.sem['c_' + e] = stack.enter_context(nc.semaphore('c_' + e))
        self.NDMA = 12
        self.rr = {}
        for q in ('sp', 'act', 'pool'):
            self.rr[q] = 0
            for i in range(self.NDMA):
                self.sem[f'd_{q}{i}'] = stack.enter_context(nc.semaphore(f'd_{q}{i}'))
        self.cnt = {k: 0 for k in self.sem}
        self.lastw = {}
        self.readers = {}
        self.waited = {e: {} for e in self.ENG}
        self.nops = 0
        self.debug = False

    def _deps(self, eng, reads, writes, extra=()):
        deps = {}
        for s, v in extra:
            deps[s] = v

        def add(s, v):
            if deps.get(s, 0) < v:
                deps[s] = v
        for k in reads:
            if k in self.lastw:
                add(*self.lastw[k])
        for k in writes:
            if k in self.lastw:
                add(*self.lastw[k])
            for s, v in self.readers.get(k, {}).items():
                add(s, v)
        waits = []
        w = self.waited[eng]
        for s, v in deps.items():
            if eng == 'pe' and s == 'c_pe':
                continue
            if w.get(s, 0) < v:
                w[s] = v
                waits.append((s, v))
        return waits

    def _mark(self, me, reads, writes):
        s, v = me
        for k in reads:
            self.readers.setdefault(k, {})[s] = v
        for k in writes:
            self.lastw[k] = me
            self.readers[k] = {}

    def op(self, eng, fn, reads=(), writes=()):
        if self.debug:
            import sys as _s
            fr = _s._getframe(1)
            ln = []
            while fr is not None and len(ln) < 3:
                ln.append(fr.f_lineno)
                fr = fr.f_back
            fn0 = fn
            fn = lambda e, fn0=fn0, ln=tuple(ln): fn0(e).annotate(f"L{ln}")
        waits = self._deps(eng, reads, writes)
        s = 'c_' + eng
        self.cnt[s] += 1
        self.ops[eng].append((waits, fn, s, 1))
        self._mark((s, self.cnt[s]), reads, writes)
        self.nops += 1

    def dma(self, q, out, in_, reads=(), writes=(), **kw):
        i = self.rr[q]
        self.rr[q] = (i + 1) % self.NDMA
        s = f'd_{q}{i}'
        extra = [(s, self.cnt[s])] if self.cnt[s] > 0 else []
        waits = self._deps(q, reads, writes, extra)
        self.cnt[s] += 16
        self.ops[q].append((waits, lambda e: e.dma_start(out=out, in_=in_, **kw), s, 16))
        self._mark((s, self.cnt[s]), reads, writes)
        self.nops += 1

    def barrier(self):
        for e in self.ENG:
            waits = []
            w = self.waited[e]
            for s, v in self.cnt.items():
                if v > 0 and w.get(s, 0) < v:
                    w[s] = v
                    waits.append((s, v))
            if waits:
                self.ops[e].append((waits, None, None, 0))
        self.lastw = {}
        self.readers = {}

    def emit(self):
        nc = self.nc
        with nc.Block() as block:
            decos = {'pe': block.tensor, 'dve': block.vector, 'act': block.scalar,
                     'pool': block.gpsimd, 'sp': block.sync}
            for e in self.ENG:
                ops = self.ops[e]

                def body(engine, ops=ops):
                    for waits, fn, s, inc in ops:
                        for (ws, wv) in waits:
                            engine.wait_ge(self.sem[ws], wv)
                        if fn is not None:
                            fn(engine).then_inc(self.sem[s], inc)
                decos[e](body)


def build_nc(dbg=False, stop=None):
    nc = bass.Bass("TRN2", target_bir_lowering=False)
    di = {}

    def inp(name, shape):
        di[name] = nc.dram_tensor(name, list(shape), F32, kind="ExternalInput").ap()
        return di[name]
    L = DEPTH
    xin = inp('xin', (NTOK, D))
    cT = inp('cT', (128, 8, 2))
    ident_d = inp('ident', (128, 128))
    w_mod = inp('w_mod', (L, D, 6 * D)); b_mod = inp('b_mod', (L, 6 * D))
    w_in = inp('w_in', (L, D, D_IN))
    a_re = inp('s5_a_re', (L, 2, 32, 64)); a_im = inp('s5_a_im', (L, 2, 32, 64)); log_dt = inp('s5_log_dt', (L, 2, 32))
    b_re = inp('s5_b_re', (L, 2, 32, 64, 16)); b_im = inp('s5_b_im', (L, 2, 32, 64, 16))
    c_re = inp('s5_c_re', (L, 2, 32, 16, 64)); c_im = inp('s5_c_im', (L, 2, 32, 16, 64))
    s5_d = inp('s5_d', (L, 512)); w_glu = inp('w_glu', (L, 512, 512)); b_glu = inp('b_glu', (L, 512))
    w_sc = inp('w_sc', (L, 3, 256)); w_dw = inp('w_dw', (L, 31, 256)); b_dw = inp('b_dw', (L, 256))
    ln_cf_g = inp('ln_cf_g', (L, 256)); ln_cf_b = inp('ln_cf_b', (L, 256))
    w_o = inp('w_o', (L, D, D)); ln1_g = inp('ln1_g', (L, D)); ln1_b = inp('ln1_b', (L, D))
    w_rg = inp('w_rg', (L, D, 4)); b_rg = inp('b_rg', (L, 4)); w_rexp = inp('w_rexp', (L, D, 16)); b_rexp = inp('b_rexp', (L, 16))
    w_gate = inp('w_gate', (L, NE, D, 256)); w_up = inp('w_up', (L, NE, D, 256)); w_down = inp('w_down', (L, NE, 256, D))
    ln2_g = inp('ln2_g', (L, D)); ln2_b = inp('ln2_b', (L, D))
    out = nc.dram_tensor('out', [NLAT, D], F32, kind="ExternalOutput").ap()
    xs = nc.dram_tensor('xs', [NTOK, D], F32).ap()
    modv = nc.dram_tensor('modv', [L, 2, 6 * D], F32).ap()
    dbg_out = None
    if dbg:
        dbg_out = nc.dram_tensor('dbg', [3, NTOK, 9216], F32, kind="ExternalOutput").ap()

    with ExitStack() as st:
        S = Sched(nc, st)
        S.debug = dbg
        ARENA_COLS = 51000
        arena = st.enter_context(nc.sbuf_tensor("arena", [128, ARENA_COLS], F32))[:]
        PS = [st.enter_context(nc.psum_tensor(f"ps{i}", [128, 512], F32)) for i in range(8)]
        PS = [p_[:] for p_ in PS]
        bump = [0]
        limit = [ARENA_COLS]
        YCAT_COLS = 8 * NTOK // 2

        def alloc(cols, dt=F32, shape=None):
            c32 = cols if dt == F32 else (cols + 1) // 2
            c32 = (c32 + 7) // 8 * 8
            o = bump[0]
            bump[0] += c32
            assert bump[0] <= limit[0], (bump[0], limit[0])
            v = arena[:, o:o + c32]
            if dt != F32:
                v = v.bitcast(dt)
            v = v[:, 0:cols]
            return v

        def r3(v, b):
            return v.rearrange("p (a b) -> p a b", b=b)

        def r4(v, b, c):
            return v.rearrange("p (a b c) -> p a b c", b=b, c=c)

        IDENT = alloc(128)
        EPS = alloc(8)
        S.dma('sp', IDENT, ident_d, writes=['IDENT'])
        S.op('pool', lambda e: e.memset(EPS, LN_EPS), writes=['EPS'])
        CT = alloc(16)
        SCT = alloc(16)
        persist_mark = bump[0]

        def TT(eng, out_, a, b, op, reads, writes):
            S.op(eng, lambda e: e.tensor_tensor(out=out_, in0=a, in1=b, op=op), reads, writes)

        def TS(eng, out_, a, s1, s2, op0, op1, reads, writes):
            if op1 is None:
                S.op(eng, lambda e: e.tensor_scalar(out=out_, in0=a, scalar1=s1, scalar2=None, op0=op0), reads, writes)
            else:
                S.op(eng, lambda e: e.tensor_scalar(out=out_, in0=a, scalar1=s1, scalar2=s2, op0=op0, op1=op1), reads, writes)

        def STT(out_, a, sc, b, op0, op1, reads, writes):
            S.op('dve', lambda e: e.scalar_tensor_tensor(out=out_, in0=a, scalar=sc, in1=b, op0=op0, op1=op1), reads, writes)

        def ACT(out_, in_, func, reads, writes, scale=None, bias=None, accum_out=None):
            kw = {}
            if scale is not None:
                kw['scale'] = scale
            if bias is not None:
                kw['bias'] = bias
            if accum_out is not None:
                kw['accum_out'] = accum_out
            S.op('act', lambda e: e.activation(out=out_, in_=in_, func=func, **kw), reads, writes)

        def CP(eng, out_, in_, reads, writes):
            if eng == 'act':
                S.op('act', lambda e: e.copy(out=out_, in_=in_), reads, writes)
            else:
                S.op(eng, lambda e: e.tensor_copy(out=out_, in_=in_), reads, writes)

        def MM(out_, lhsT, rhs, start, stop, reads, writes):
            S.op('pe', lambda e: e.matmul(out_, lhsT=lhsT, rhs=rhs, start=start, stop=stop), reads, writes)

        def TR(out_, in_, reads, writes):
            n = in_.shape[0]
            S.op('pe', lambda e: e.transpose(out=out_, in_=in_, identity=IDENT[0:n, 0:n]), list(reads) + ['IDENT'], writes)

        def OP(eng, meth, reads, writes, **kw):
            S.op(eng, lambda e: getattr(e, meth)(**kw), reads, writes)

        def MEMSET(eng, ap, val, writes):
            S.op(eng, lambda e: e.memset(ap, val), (), writes)

        S.dma('sp', xs[0:1152], xin[0:1152], writes=['xs'])
        S.dma('pool', xs[1152:2304], xin[1152:2304], writes=['xs2'])
        S.dma('sp', CT, cT.rearrange("k a r -> k (a r)"), writes=['CT'])
        ACT(SCT, CT, AF.Silu, ['CT'], ['SCT'])
        SCT3 = r3(SCT, 2)
        def emit_mod(l_, WM, MODSB, BM, sfx):
            for half in range(2):
                S.dma('pool', BM[0:2, :], b_mod[l_:l_ + 1, half * 3072:(half + 1) * 3072].partition_broadcast(2), writes=['BM' + sfx])
                for kc in range(8):
                    wb = WM[kc % len(WM)]
                    S.dma(('sp', 'act', 'pool')[kc % 3], wb, w_mod[l_, kc * 128:(kc + 1) * 128, half * 3072:(half + 1) * 3072],
                          writes=[f'WM{kc % len(WM)}' + sfx])
                    for n in range(6):
                        MM(PS[n][0:2, :], SCT3[:, kc, :], wb[:, n * 512:(n + 1) * 512], kc == 0, kc == 7,
                           ['SCT', f'WM{kc % len(WM)}' + sfx], [f'ps{n}'])
                for n in range(6):
                    CP('act', MODSB[0:2, n * 512:(n + 1) * 512], PS[n][0:2, :], [f'ps{n}'], ['MODSB' + sfx])
                TT('pool', MODSB[0:2, :], MODSB[0:2, :], BM[0:2, :], ALU.add, ['MODSB' + sfx, 'BM' + sfx], ['MODSB' + sfx])
                S.dma('sp', modv[l_, :, half * 3072:(half + 1) * 3072], MODSB[0:2, :], reads=['MODSB' + sfx], writes=['modv'])

        WM = [alloc(3072), alloc(3072), alloc(3072)]
        MODSB = alloc(3072)
        BM = alloc(3072)
        emit_mod(0, WM, MODSB, BM, '')
        S.barrier()

        for l in range(L):
            if stop == 'p0':
                break
            last = (l == L - 1)
            bump[0] = persist_mark

            def load_bc(dst, src_row, key):
                S.dma('sp', dst, src_row.partition_broadcast(128), reads=['modv'], writes=[key])

            def modtile(j, key):
                ts = []
                for kind in range(2):
                    t = alloc(1024)
                    load_bc(t, modv[l, kind:kind + 1, j * 1024:(j + 1) * 1024], f'{key}{kind}')
                    ts.append(t)
                return ts

            def kind_of(ti):
                return 1 if ti < 2 else 0

            YCAT = arena[:, ARENA_COLS - YCAT_COLS:ARENA_COLS].bitcast(BF16); YCAT3 = r3(YCAT, NTOK)
            lay_mark = bump[0]
            limit[0] = ARENA_COLS

            UT = alloc(4 * NTOK, BF16); UT3 = r3(UT, NTOK)
            YACC = alloc(4 * NTOK); YACC3 = r3(YACC, NTOK)
            s5_mark = bump[0]
            SH1 = modtile(0, 'SH1'); SC1 = modtile(1, 'SC1')
            for kind in range(2):
                TS('dve', SC1[kind], SC1[kind], 1.0, None, ALU.add, None, [f'SC1{kind}'], [f'SC1{kind}'])
            if stop == 'b1a':
                break
            WST = alloc(8 * 512)
            WST3 = r3(WST, 512)
            WINU = alloc(8 * 512, BF16); WINU3 = r3(WINU, 512)
            S.dma('sp', WST3, w_in[l, :, 0:512].rearrange("(kc k) f -> k kc f", k=128), writes=['WST'])
            CP('act', WINU, WST, ['WST'], ['WINU'])
            DSK = alloc(4)
            S.dma('sp', DSK, s5_d[l].rearrange("(c p) -> p c", p=128), writes=['DSK'], allow_slow_non_contiguous=True)
            if stop == 'b1b':
                break
            XT = [alloc(1024), alloc(1024)]
            HTs3 = [r3(alloc(8 * 512, BF16), 512), r3(alloc(8 * 512, BF16), 512)]

            def make_hT(tok0, ntok, sh, sc, shk, sck, hpar=0):
                HT3 = HTs3[hpar]
                for i in range(ntok // 128):
                    ti = (tok0 // 128) + i
                    kd = kind_of(ti)
                    xt = XT[ti % 2]
                    xk = f'XT{ti % 2}'
                    S.dma('sp', xt, xs[ti * 128:(ti + 1) * 128, :], reads=['xs', 'xs2'], writes=[xk])
                    TT('dve', xt, xt, sc[kd], ALU.mult, [xk, f'{sck}{kd}'], [xk])
                    TT('pool', xt, xt, sh[kd], ALU.add, [xk, f'{shk}{kd}'], [xk])
                    for hh in range(2):
                        pb = 6 + hh
                        for k4 in range(4):
                            kc = hh * 4 + k4
                            TR(PS[pb][:, k4 * 128:(k4 + 1) * 128], xt[:, kc * 128:(kc + 1) * 128], [xk], [f'ps{pb}'])
                        CP('act', HT3[:, hh * 4:(hh + 1) * 4, i * 128:(i + 1) * 128],
                           r3(PS[pb], 128), [f'ps{pb}'], [f'HT{hpar}'])

            make_hT(BLOCKS[0][0], BLOCKS[0][1], SH1, SC1, 'SH1', 'SC1', 0)
            for bix, (tok0, ntok) in enumerate(BLOCKS):
                hpar = bix % 2
                HT3 = HTs3[hpar]
                if bix + 1 < len(BLOCKS):
                    make_hT(BLOCKS[bix + 1][0], BLOCKS[bix + 1][1], SH1, SC1, 'SH1', 'SC1', (bix + 1) % 2)
                if stop == 'b1c':
                    break
                for fc in range(4):
                    pb = fc % 2
                    for kc in range(8):
                        MM(PS[pb][:, 0:ntok], WINU3[:, kc, fc * 128:(fc + 1) * 128], HT3[:, kc, 0:ntok], kc == 0, kc == 7,
                           ['WINU', f'HT{hpar}'], [f'ps{pb}'])
                    CP('act', UT3[:, fc, tok0:tok0 + ntok], PS[pb][:, 0:ntok], [f'ps{pb}'], ['UT'])
                    if stop == 'b1d':
                        continue
                    ACT(YACC3[:, fc, tok0:tok0 + ntok], PS[pb][:, 0:ntok], AF.Copy, [f'ps{pb}', 'DSK'], ['YACC'], scale=DSK[:, fc:fc + 1])
                if stop in ('b1d', 'b1e'):
                    break
            S.barrier()
            bump[0] = s5_mark
            if stop in ('b1', 'b1c', 'b1d', 'b1e'):
                break

            def T32():
                return alloc(32)
            NAT = alloc(128)
            NAT2 = alloc(8)
            ARE = T32(); AIM = T32(); DT = T32()

            def load_T(dst, src2d, key):
                S.dma('sp', NAT[0:32, :], src2d, writes=['NAT'])
                TR(PS[0][:, 0:32], NAT[0:32, :], ['NAT'], ['ps0'])
                CP('act', dst, PS[0][:, 0:32], ['ps0'], [key])
            load_T(ARE, a_re[l].rearrange("d (pr g2) p -> (d pr) (g2 p)", g2=2), 'ARE')
            load_T(AIM, a_im[l].rearrange("d (pr g2) p -> (d pr) (g2 p)", g2=2), 'AIM')
            S.dma('sp', NAT2[0:32, 0:2], log_dt[l].rearrange("d (pr g2) -> (d pr) g2", g2=2), writes=['NAT2'])
            CP('dve', r3(NAT[0:32, :], 64), NAT2[0:32, 0:2].unsqueeze(2).to_broadcast([32, 2, 64]), ['NAT2', 'NAT'], ['NAT'])
            TR(PS[0][:, 0:32], NAT[0:32, :], ['NAT'], ['ps0'])
            ACT(DT, PS[0][:, 0:32], AF.Exp, ['ps0'], ['DT'])
            RHO = T32(); TH = T32(); KF = T32(); KI = alloc(32).bitcast(I32); TMP = T32(); TMP2 = T32()
            TT('dve', RHO, ARE, DT, ALU.mult, ['ARE', 'DT'], ['RHO'])
            TT('dve', TH, AIM, DT, ALU.mult, ['AIM', 'DT'], ['TH'])
            TS('dve', TMP, TH, 1.0 / TWO_PI, None, ALU.mult, None, ['TH'], ['TMP'])
            CP('dve', KI, TMP, ['TMP'], ['KI'])
            CP('dve', KF, KI, ['KI'], ['KF'])
            STT(TMP, KF, -C1, TH, ALU.mult, ALU.add, ['KF', 'TH'], ['TMP'])
            STT(TMP2, KF, -C2, TMP, ALU.mult, ALU.add, ['KF', 'TMP'], ['TMP2'])
            SHh = T32(); CHh = T32(); HPI = alloc(8)
            MEMSET('pool', HPI, math.pi / 2, ['HPI'])
            ACT(SHh, TMP2, AF.Sin, ['TMP2'], ['SHh'], scale=0.5)
            ACT(CHh, TMP2, AF.Sin, ['TMP2', 'HPI'], ['CHh'], scale=-0.5, bias=HPI[:, 0:1])
            C1T = T32(); S1T = T32(); R1 = T32()
            TT('dve', TMP, CHh, CHh, ALU.mult, ['CHh'], ['TMP'])
            TT('dve', TMP2, SHh, SHh, ALU.mult, ['SHh'], ['TMP2'])
            TT('dve', C1T, TMP, TMP2, ALU.subtract, ['TMP', 'TMP2'], ['C1T'])
            TT('dve', TMP, SHh, CHh, ALU.mult, ['SHh', 'CHh'], ['TMP'])
            TS('dve', S1T, TMP, 2.0, None, ALU.mult, None, ['TMP'], ['S1T'])
            ACT(R1, RHO, AF.Exp, ['RHO'], ['R1'])
            ER = alloc(16 * 32); EI = alloc(16 * 32); ER3 = r3(ER, 32); EI3 = r3(EI, 32)
            MEMSET('pool', ER3[:, 0, :], 1.0, ['ER'])
            MEMSET('pool', EI3[:, 0, :], 0.0, ['EI'])
            for s in range(15):
                TT('dve', TMP, ER3[:, s, :], C1T, ALU.mult, ['ER', 'C1T'], ['TMP'])
                TT('dve', TMP2, EI3[:, s, :], S1T, ALU.mult, ['EI', 'S1T'], ['TMP2'])
                TT('dve', ER3[:, s + 1, :], TMP, TMP2, ALU.subtract, ['TMP', 'TMP2'], ['ER'])
                TT('dve', TMP, ER3[:, s, :], S1T, ALU.mult, ['ER', 'S1T'], ['TMP'])
                TT('dve', TMP2, EI3[:, s, :], C1T, ALU.mult, ['EI', 'C1T'], ['TMP2'])
                TT('dve', EI3[:, s + 1, :], TMP, TMP2, ALU.add, ['TMP', 'TMP2'], ['EI'])
            RP = alloc(8 * 32); RP3 = r3(RP, 32)
            for k in range(1, 9):
                ACT(RP3[:, k - 1, :], RHO, AF.Exp, ['RHO'], ['RP'], scale=float(k))
            QR = T32(); QI = T32(); NR = T32(); NI = T32(); DEN = T32()
            TT('dve', TMP, R1, C1T, ALU.mult, ['R1', 'C1T'], ['TMP'])
            TS('dve', NR, TMP, -1.0, None, ALU.add, None, ['TMP'], ['NR'])
            TT('dve', NI, R1, S1T, ALU.mult, ['R1', 'S1T'], ['NI'])
            TT('dve', TMP, ARE, ARE, ALU.mult, ['ARE'], ['TMP'])
            TT('dve', TMP2, AIM, AIM, ALU.mult, ['AIM'], ['TMP2'])
            TT('dve', DEN, TMP, TMP2, ALU.add, ['TMP', 'TMP2'], ['DEN'])
            OP('dve', 'reciprocal', ['DEN'], ['DEN'], out=DEN, in_=DEN)
            TT('dve', TMP, NR, ARE, ALU.mult, ['NR', 'ARE'], ['TMP'])
            TT('dve', TMP2, NI, AIM, ALU.mult, ['NI', 'AIM'], ['TMP2'])
            TT('dve', TMP, TMP, TMP2, ALU.add, ['TMP', 'TMP2'], ['TMP'])
            TT('dve', QR, TMP, DEN, ALU.mult, ['TMP', 'DEN'], ['QR'])
            TT('dve', TMP, NI, ARE, ALU.mult, ['NI', 'ARE'], ['TMP'])
            TT('dve', TMP2, NR, AIM, ALU.mult, ['NR', 'AIM'], ['TMP2'])
            TT('dve', TMP, TMP, TMP2, ALU.subtract, ['TMP', 'TMP2'], ['TMP'])
            TT('dve', QI, TMP, DEN, ALU.mult, ['TMP', 'DEN'], ['QI'])
            FR = alloc(256); FI = alloc(256); ELR = alloc(256); ELI = alloc(256); KR = alloc(256); KIm = alloc(256)
            FR3 = r3(FR, 8); FI3 = r3(FI, 8); ELR3 = r3(ELR, 8); ELI3 = r3(ELI, 8); KR3 = r3(KR, 8); KI3 = r3(KIm, 8)
            for s in range(8):
                TT('dve', TMP, ER3[:, s, :], QR, ALU.mult, ['ER', 'QR'], ['TMP'])
                TT('dve', TMP2, EI3[:, s, :], QI, ALU.mult, ['EI', 'QI'], ['TMP2'])
                TT('dve', FR3[:, :, s], TMP, TMP2, ALU.add, ['TMP', 'TMP2'], ['FR'])
                TT('dve', TMP, ER3[:, s, :], QI, ALU.mult, ['ER', 'QI'], ['TMP'])
                TT('dve', TMP2, EI3[:, s, :], QR, ALU.mult, ['EI', 'QR'], ['TMP2'])
                TT('dve', FI3[:, :, s], TMP, TMP2, ALU.subtract, ['TMP', 'TMP2'], ['FI'])
                CP('pool', ELR3[:, :, s], ER3[:, s, :], ['ER'], ['ELR'])
                CP('pool', ELI3[:, :, s], EI3[:, s, :], ['EI'], ['ELI'])
                TT('dve', KR3[:, :, s], ER3[:, s + 8, :], RP3[:, s, :], ALU.mult, ['ER', 'RP'], ['KR'])
                TT('dve', KI3[:, :, s], EI3[:, s + 8, :], RP3[:, s, :], ALU.mult, ['EI', 'RP'], ['KI_'])
            LRR = alloc(64); LII = alloc(64); LRR3 = r3(LRR, 2); LII3 = r3(LII, 2)
            for c in range(2):
                TT('dve', LRR3[:, :, c], ER3[:, 8, :], RP3[:, 7, :], ALU.mult, ['ER', 'RP'], ['LRR'])
                TT('dve', LII3[:, :, c], EI3[:, 8, :], RP3[:, 7, :], ALU.mult, ['EI', 'RP'], ['LII'])
            BRE = alloc(1024); BIM = alloc(1024); CTR = alloc(1024); CTI = alloc(1024)
            BRE3 = r3(BRE, 32); BIM3 = r3(BIM, 32); CTR3 = r3(CTR, 32); CTI3 = r3(CTI, 32)
            CN = alloc(32 * 128); CN3 = r3(CN, 128)
            for (dst3, src, key) in ((BRE3, b_re, 'BRE'), (BIM3, b_im, 'BIM')):
                MEMSET('pool', dst3, 0.0, [key])
                v = src[l].rearrange("d (pr g2) p n -> g2 p (d pr) n", g2=2)
                for g2 in range(2):
                    S.dma('sp', dst3[g2 * 64:(g2 + 1) * 64, :, g2 * 16:(g2 + 1) * 16], v[g2], writes=[key])
            for (dst3, src, key) in ((CTR3, c_re, 'CTR'), (CTI3, c_im, 'CTI')):
                MEMSET('pool', CN3[0:32], 0.0, ['CN'])
                v = src[l].rearrange("d (pr g2) n p -> g2 n (d pr) p", g2=2)
                for g2 in range(2):
                    S.dma('sp', CN3[g2 * 16:(g2 + 1) * 16, :, g2 * 64:(g2 + 1) * 64], v[g2], writes=['CN'])
                for ub in range(2):
                    for uu in range(16):
                        u = ub * 16 + uu
                        TR(PS[ub][:, uu * 32:(uu + 1) * 32], CN3[0:32, u, :], ['CN'], [f'ps{ub}'])
                    CP('act', dst3[:, ub * 16:(ub + 1) * 16, :], r3(PS[ub], 32), [f'ps{ub}'], [key])
            if stop == 'tab':
                break
            XZ = alloc(32 * 289 * 2, BF16); XZ4 = r4(XZ, 289, 2)
            MEMSET('pool', XZ, 0.0, ['XZ'])
            WB = [alloc(8 * 128), alloc(8 * 128)]
            T1 = alloc(256); T2 = alloc(256)
            LBm = {}
            LCm = {}
            for d in range(2):
                for ri in range(2):
                    LBm[d, ri] = alloc(8 * 128, BF16)
                    LCm[d, ri] = alloc(8 * 128, BF16)
            DEC = [alloc(512), alloc(512)]

            def bc_s(tab3, u):
                return tab3[:, u, :].unsqueeze(2).to_broadcast([128, 8, 32])

            def bc_m(mat3, u):
                return mat3[:, u, :].unsqueeze(1).to_broadcast([128, 8, 32])

            def cplx_outer(eng, out_re, out_im, Ar, Ai, Br, Bi, u, keys_r, neg_im, okeys):
                t1 = r3(T1, 32); t2 = r3(T2, 32)
                TT(eng, t1, bc_s(Ar, u), bc_m(Br, u), ALU.mult, keys_r, ['T1'])
                TT(eng, t2, bc_s(Ai, u), bc_m(Bi, u), ALU.mult, keys_r, ['T2'])
                TT(eng, out_re, t1, t2, ALU.subtract, ['T1', 'T2'], [okeys[0]])
                TT(eng, t1, bc_s(Ar, u), bc_m(Bi, u), ALU.mult, keys_r, ['T1'])
                TT(eng, t2, bc_s(Ai, u), bc_m(Br, u), ALU.mult, keys_r, ['T2'])
                if neg_im:
                    S.op('dve', lambda e: e.scalar_tensor_tensor(out=out_im, in0=t1, scalar=-1.0, in1=t2, op0=ALU.mult, op1=ALU.subtract),
                         ['T1', 'T2'], [okeys[1]])
                else:
                    TT(eng, out_im, t1, t2, ALU.add, ['T1', 'T2'], [okeys[1]])

            tabkeys = ['FR', 'FI', 'ELR', 'ELI', 'KR', 'KI_', 'BRE', 'BIM', 'CTR', 'CTI']

            def gen_unit(d, pair, carry):
                u = d * 16 + pair
                q = pair % 4
                win = slice(32 * q, 32 * q + 32)
                if not carry:
                    WBr3 = r3(WB[0], 128); WBi3 = r3(WB[1], 128)
                    cplx_outer('dve', WBr3[:, :, win], WBi3[:, :, win], FR3, FI3, BRE3, BIM3, u, tabkeys, False, ['WB0', 'WB1'])
                    for ri in range(2):
                        w3 = r3(WB[ri], 128)
                        for hb in range(2):
                            pb = 6 + hb
                            for s4 in range(4):
                                s = hb * 4 + s4
                                TR(PS[pb][:, s4 * 128:(s4 + 1) * 128], w3[:, s, :], [f'WB{ri}'], [f'ps{pb}'])
                            CP('act', LBm[d, ri][:, hb * 512:(hb + 1) * 512], PS[pb], [f'ps{pb}'], [f'LB{d}{ri}'])
                    lc_r = r3(LCm[d, 0], 128); lc_i = r3(LCm[d, 1], 128)
                    cplx_outer('dve', lc_r[:, :, win], lc_i[:, :, win], ELR3, ELI3, CTR3, CTI3, u, tabkeys, True, [f'LC{d}0', f'LC{d}1'])
                else:
                    lc_r = r3(LCm[d, 0], 128); lc_i = r3(LCm[d, 1], 128)
                    cplx_outer('dve', lc_r[:, :, win], lc_i[:, :, win], KR3, KI3, CTR3, CTI3, u, tabkeys, True, [f'LC{d}0', f'LC{d}1'])

            def zero_windows(carry):
                if not carry:
                    MEMSET('pool', WB[0], 0.0, ['WB0'])
                    MEMSET('pool', WB[1], 0.0, ['WB1'])
                for d in range(2):
                    for ri in range(2):
                        MEMSET('pool', LCm[d, ri], 0.0, [f'LC{d}{ri}'])

            def slots(d, bi, plus):
                tok0, ntok = BLOCKS[bi]
                nch = ntok // 8
                if d == 0:
                    jj0 = tok0 // 8
                    return slice(jj0 + plus, jj0 + plus + nch)
                if bi == 0:
                    hi = 31 + plus
                else:
                    cl0 = (tok0 - NCTX) // 8
                    hi = 287 - cl0 + plus
                lo = hi - nch
                return slice(hi, lo if lo >= 0 else None, -1)

            order = [(q, pair) for q in range(4) for pair in range(q, 16, 4)]
            def gen_B(d, pair):
                u = d * 16 + pair
                q = pair % 4
                win = slice(32 * q, 32 * q + 32)
                WBr3 = r3(WB[0], 128); WBi3 = r3(WB[1], 128)
                cplx_outer('dve', WBr3[:, :, win], WBi3[:, :, win], FR3, FI3, BRE3, BIM3, u, tabkeys, False, ['WB0', 'WB1'])
                for ri in range(2):
                    w3 = r3(WB[ri], 128)
                    for hb in range(2):
                        pb = 6 + hb
                        for s4 in range(4):
                            s_ = hb * 4 + s4
                            TR(PS[pb][:, s4 * 128:(s4 + 1) * 128], w3[:, s_, :], [f'WB{ri}'], [f'ps{pb}'])
                        CP('act', LBm[d, ri][:, hb * 512:(hb + 1) * 512], PS[pb], [f'ps{pb}'], [f'LB{d}{ri}'])
                dk = f'DEC{d}'
                CP('pool', r3(DEC[d], 8), R1[:, u:u + 1].unsqueeze(2).to_broadcast([128, 64, 8]), ['R1'], [dk])
                MEMSET('pool', r3(DEC[d], 8)[:, :, 0], 0.0, [dk])

            def gen_C(d, pair, carry, lcset, lckey):
                u = d * 16 + pair
                q = pair % 4
                win = slice(32 * q, 32 * q + 32)
                lc_r = r3(lcset[d, 0], 128); lc_i = r3(lcset[d, 1], 128)
                if carry:
                    cplx_outer('dve', lc_r[:, :, win], lc_i[:, :, win], KR3, KI3, CTR3, CTI3, u, tabkeys, True, [f'{lckey}{d}0', f'{lckey}{d}1'])
                else:
                    cplx_outer('dve', lc_r[:, :, win], lc_i[:, :, win], ELR3, ELI3, CTR3, CTI3, u, tabkeys, True, [f'{lckey}{d}0', f'{lckey}{d}1'])

            def emit_B(pair, bi, par):
                cc = pair // 4
                tok0, ntok = BLOCKS[bi]
                for d in range(2):
                    u = d * 16 + pair
                    for ri in range(2):
                        pb = 2 * d + ri
                        lb3 = r3(LBm[d, ri], 128)
                        for s_ in range(8):
                            tau = s_ if d == 0 else 7 - s_
                            MM(r3(PS[pb][:, 0:ntok], 8)[:, :, s_], lb3[:, s_, :],
                               r3(UT3[:, cc, tok0:tok0 + ntok], 8)[:, :, tau], True, True,
                               [f'LB{d}{ri}', 'UT'], [f'ps{pb}'])
                        bp = BPB[par, d, ri]; bk = f'BP{par}{d}{ri}'
                        CP('act', bp[:, 0:ntok], PS[pb][:, 0:ntok], [f'ps{pb}'], [bk])
                        g = GB[par, d, ri]; gk = f'G{par}{d}{ri}'
                        OP('dve', 'tensor_tensor_scan', [f'DEC{d}', bk], [gk],
                           out=g[:, 0:ntok], data0=DEC[d][:, 0:ntok], data1=bp[:, 0:ntok], initial=0.0,
                           op0=ALU.mult, op1=ALU.add)
                        CP('pool', XZ4[:, u, slots(d, bi, 1), ri], r3(g[:, 0:ntok], 8)[:, :, 7], [gk], ['XZ'])

            def emit_C(pair, bi, par):
                cc = pair // 4
                tok0, ntok = BLOCKS[bi]
                pby = 4 + par
                for d in range(2):
                    for ri in range(2):
                        lc3 = r3(LCm[d, ri], 128)
                        g = GB[par, d, ri]; gk = f'G{par}{d}{ri}'
                        for s_ in range(8):
                            tau = s_ if d == 0 else 7 - s_
                            first = (d == 0 and ri == 0 and s_ == 0)
                            lastmm = (d == 1 and ri == 1 and s_ == 7)
                            MM(r3(PS[pby][:, 0:ntok], 8)[:, :, tau], lc3[:, s_, :], r3(g[:, 0:ntok], 8)[:, :, s_],
                               first, lastmm, [f'LC{d}{ri}', gk], [f'ps{pby}'])
                TT('dve', YACC3[:, cc, tok0:tok0 + ntok], YACC3[:, cc, tok0:tok0 + ntok], PS[pby][:, 0:ntok], ALU.add,
                   ['YACC', f'ps{pby}'], ['YACC'])

            BPB = {}
            GB = {}
            s5blk0 = bump[0]
            for par in range(2):
                for d in range(2):
                    for ri in range(2):
                        BPB[par, d, ri] = alloc(512)
                        GB[par, d, ri] = alloc(512, BF16)
            MODSB1 = arena[:, s5blk0:s5blk0 + 3072]
            BM1 = arena[:, s5blk0 + 3072:s5blk0 + 6144]
            assert bump[0] - s5blk0 >= 6144
            curqB = -1; curqC = -1
            prev = None
            cnt_items = 0
            for (q, pair) in order:
                if q != curqB:
                    MEMSET('pool', WB[0], 0.0, ['WB0'])
                    MEMSET('pool', WB[1], 0.0, ['WB1'])
                    curqB = q
                for d in range(2):
                    gen_B(d, pair)
                for bi in range(len(BLOCKS)):
                    par = cnt_items % 2
                    cnt_items += 1
                    emit_B(pair, bi, par)
                    if prev is not None:
                        emit_C(*prev)
                    if bi == 0:
                        if q != curqC:
                            for d in range(2):
                                for ri in range(2):
                                    MEMSET('pool', LCm[d, ri], 0.0, [f'LC{d}{ri}'])
                            curqC = q
                        for d in range(2):
                            gen_C(d, pair, False, LCm, 'LC')
                    prev = (pair, bi, par)
            emit_C(*prev)
            if stop == 'pass1':
                break
            if l == 0:
                S.barrier()
                emit_mod(1, [UT.bitcast(F32)[:, 0:3072], CN[:, 0:3072]], MODSB1, BM1, 'L1')
            ZP = [alloc(64), alloc(64)]
            CAB = alloc(128)
            LL4 = alloc(128)
            CAB4 = r4(CAB, 2, 2); LL4v = r4(LL4, 2, 2)
            CP('pool', LL4v[:, :, 0, :], LRR3, ['LRR'], ['LL4'])
            CP('pool', LL4v[:, :, 1, :], LII3, ['LII'], ['LL4'])
            MEMSET('pool', ZP[0], 0.0, ['ZP0r', 'ZP0i'])
            CH_E = 'dve'
            for k in range(NCH):
                zp = ZP[k % 2]; zn = ZP[(k + 1) % 2]
                zpk = f'ZP{k % 2}'; znk = f'ZP{(k + 1) % 2}'
                zn3 = r3(zn, 2); zp3 = r3(zp, 2)
                xk = XZ4[:, :, k + 1, :]
                TT(CH_E, CAB4, zp3.unsqueeze(2).to_broadcast([128, 32, 2, 2]), LL4v, ALU.mult, [zpk + 'r', zpk + 'i', 'LL4'], ['CAB'])
                TT(CH_E, CAB4[:, :, 0, :], CAB4[:, :, 0, :], xk, ALU.add, ['CAB', 'XZ'], ['CAB'])
                TT(CH_E, zn3[:, :, 0], CAB4[:, :, 0, 0], CAB4[:, :, 1, 1], ALU.subtract, ['CAB'], [znk + 'r'])
                TT(CH_E, zn3[:, :, 1], CAB4[:, :, 0, 1], CAB4[:, :, 1, 0], ALU.add, ['CAB'], [znk + 'i'])
                CP('pool', xk, zn3, [znk + 'r', znk + 'i'], ['XZ' + str(k)])
            if stop == 'chain':
                break
            S.barrier()
            LCsets = [LCm, LBm]
            LCkeys = ['LC', 'LB']
            setq = [-1, -1]

            def gen_pair2(idx):
                q, pair = order[idx]
                pp = idx % 2
                if setq[pp] != q:
                    for d in range(2):
                        for ri in range(2):
                            MEMSET('pool', LCsets[pp][d, ri], 0.0, [f'{LCkeys[pp]}{d}{ri}'])
                    setq[pp] = q
                for d in range(2):
                    gen_C(d, pair, True, LCsets[pp], LCkeys[pp])
            gen_pair2(0)
            it2 = 0
            for idx, (q, pair) in enumerate(order):
                cc = pair // 4
                pp = idx % 2
                if idx + 1 < len(order):
                    gen_pair2(idx + 1)
                for bi, (tok0, ntok) in enumerate(BLOCKS):
                    pby = it2 % 4
                    it2 += 1
                    for d in range(2):
                        u = d * 16 + pair
                        for ri in range(2):
                            lc3 = r3(LCsets[pp][d, ri], 128)
                            for s_ in range(8):
                                tau = s_ if d == 0 else 7 - s_
                                first = (d == 0 and ri == 0 and s_ == 0)
                                lastmm = (d == 1 and ri == 1 and s_ == 7)
                                MM(r3(PS[pby][:, 0:ntok], 8)[:, :, tau], lc3[:, s_, :], XZ4[:, u, slots(d, bi, 0), ri],
                                   first, lastmm, [f'{LCkeys[pp]}{d}{ri}', 'XZ'], [f'ps{pby}'])
                    TT('dve', YACC3[:, cc, tok0:tok0 + ntok], YACC3[:, cc, tok0:tok0 + ntok], PS[pby][:, 0:ntok], ALU.add,
                       ['YACC', f'ps{pby}'], ['YACC'])
            S.barrier()
            if dbg and l == 0:
                for c in range(4):
                    S.dma('sp', dbg_out[0, 0:128, c * NTOK:(c + 1) * NTOK], YACC3[:, c, :], reads=['YACC'], writes=['dbg0'])
                S.barrier()
                if stop == 's5':
                    break
            bump[0] = s5_mark
            limit[0] = ARENA_COLS - YCAT_COLS
            WG_ST = alloc(4 * 512); WG = alloc(4 * 512, BF16); WG3 = r3(WG, 512)
            S.dma('sp', r3(WG_ST, 512), w_glu[l].rearrange("(kc k) f -> k kc f", k=128), writes=['WGST'])
            CP('act', WG, WG_ST, ['WGST'], ['WG'])
            BG = alloc(4)
            S.dma('sp', BG, b_glu[l].rearrange("(c p) -> p c", p=128), writes=['BG'], allow_slow_non_contiguous=True)
            GT = alloc(4 * NTOK, BF16); GT3 = r3(GT, NTOK)
            SG = [alloc(512), alloc(512)]
            for c in range(4):
                ACT(YACC3[:, c, :], YACC3[:, c, :], AF.Gelu, ['YACC'], ['YACC'])
                CP('dve', GT3[:, c, :], YACC3[:, c, :], ['YACC'], ['GT'])
            for (tok0, ntok) in BLOCKS:
                for oc in range(4):
                    pb = oc % 2
                    for kc in range(4):
                        MM(PS[pb][:, 0:ntok], WG3[:, kc, oc * 128:(oc + 1) * 128], GT3[:, kc, tok0:tok0 + ntok], kc == 0, kc == 3,
                           ['WG', 'GT'], [f'ps{pb}'])
                    ACT(SG[pb][:, 0:ntok], PS[pb][:, 0:ntok], AF.Sigmoid, [f'ps{pb}', 'BG'], [f'SG{pb}'], bias=BG[:, oc:oc + 1])
                    TT('dve', YCAT3[:, oc, tok0:tok0 + ntok], YACC3[:, oc, tok0:tok0 + ntok], SG[pb][:, 0:ntok], ALU.mult,
                       ['YACC', f'SG{pb}'], ['YCAT'])
            S.barrier()

            if stop == 'gate':
                break
            bump[0] = lay_mark
            SH1 = modtile(0, 'SH1'); SC1 = modtile(1, 'SC1')
            for kind in range(2):
                TS('dve', SC1[kind], SC1[kind], 1.0, None, ALU.add, None, [f'SC1{kind}'], [f'SC1{kind}'])
            GLU = alloc(2 * NTOK); GLU3 = r3(GLU, NTOK)
            NF2 = 1280
            WST = alloc(8 * 640); WST3 = r3(WST, 640)
            WINR = alloc(8 * NF2, BF16); WINR3 = r3(WINR, NF2)
            for hf in range(2):
                S.dma('sp', WST3, w_in[l, :, 512 + hf * 640:512 + (hf + 1) * 640].rearrange("(kc k) f -> k kc f", k=128), writes=['WST'])
                CP('act', WINR3[:, :, hf * 640:(hf + 1) * 640], WST3, ['WST'], ['WINR'])
            WSC = alloc(8)
            WSC3 = r3(WSC[:, 0:6], 3)
            for c_ in range(2):
                S.dma('sp', WSC3[:, c_, :], w_sc[l][:, c_ * 128:(c_ + 1) * 128].rearrange("k p -> p k"), writes=['WSC'], allow_slow_non_contiguous=True)
            XT = [alloc(1024), alloc(1024)]
            HTs3 = [r3(alloc(8 * 512, BF16), 512), r3(alloc(8 * 512, BF16), 512)]
            BGT = alloc(512); CGT = alloc(512); CV = alloc(512); ACC = alloc(512); SIG = alloc(512)
            make_hT(BLOCKS[0][0], BLOCKS[0][1], SH1, SC1, 'SH1', 'SC1', 0)
            for bix, (tok0, ntok) in enumerate(BLOCKS):
                hpar = bix % 2
                HT3 = HTs3[hpar]
                if bix + 1 < len(BLOCKS):
                    make_hT(BLOCKS[bix + 1][0], BLOCKS[bix + 1][1], SH1, SC1, 'SH1', 'SC1', (bix + 1) % 2)
                W = 64 if tok0 >= NCTX else 256
                nr = ntok // W

                def zmm(fc, pb):
                    f0 = fc * 128 - 512
                    for kc in range(8):
                        MM(PS[pb][:, 0:ntok], WINR3[:, kc, f0:f0 + 128], HT3[:, kc, 0:ntok], kc == 0, kc == 7,
                           ['WINR', f'HT{hpar}'], [f'ps{pb}'])
                for j in range(2):
                    zmm(4 + j, 0)
                    CP('act', BGT[:, 0:ntok], PS[0][:, 0:ntok], ['ps0'], ['BGT'])
                    zmm(6 + j, 1)
                    CP('act', CGT[:, 0:ntok], PS[1][:, 0:ntok], ['ps1'], ['CGT'])
                    zmm(8 + j, 2)
                    TT('dve', CV[:, 0:ntok], CGT[:, 0:ntok], PS[2][:, 0:ntok], ALU.mult, ['CGT', 'ps2'], ['CV'])
                    cv3 = r3(CV[:, 0:ntok], W); ac3 = r3(ACC[:, 0:ntok], W)
                    TS('dve', ACC[:, 0:ntok], CV[:, 0:ntok], WSC3[:, j, 1:2], None, ALU.mult, None, ['CV', 'WSC'], ['ACC'])
                    STT(ac3[:, :, 1:W], cv3[:, :, 0:W - 1], WSC3[:, j, 0:1], ac3[:, :, 1:W], ALU.mult, ALU.add, ['CV', 'WSC', 'ACC'], ['ACC'])
                    STT(ac3[:, :, 0:W - 1], cv3[:, :, 1:W], WSC3[:, j, 2:3], ac3[:, :, 0:W - 1], ALU.mult, ALU.add, ['CV', 'WSC', 'ACC'], ['ACC'])
                    TT('dve', YCAT3[:, 4 + j, tok0:tok0 + ntok], ACC[:, 0:ntok], BGT[:, 0:ntok], ALU.mult, ['ACC', 'BGT'], ['YCAT'])
                    zmm(12 + j, 3)
                    ACT(SIG[:, 0:ntok], PS[3][:, 0:ntok], AF.Sigmoid, ['ps3'], ['SIG'])
                    zmm(10 + j, 4)
                    TT('dve', GLU3[:, j, tok0:tok0 + ntok], SIG[:, 0:ntok], PS[4][:, 0:ntok], ALU.mult, ['SIG', 'ps4'], ['GLU'])
            if stop == 'b2':
                S.barrier()
                break
            WDW = alloc(64)
            WDW3 = r3(WDW[:, 0:62], 31)
            for c_ in range(2):
                S.dma('sp', WDW3[:, c_, :], w_dw[l][:, c_ * 128:(c_ + 1) * 128].rearrange("k p -> p k"), writes=['WDW'], allow_slow_non_contiguous=True)
            CFV = alloc(8)
            CFV3 = r3(CFV[:, 0:6], 2)
            for i_, src in enumerate((b_dw, ln_cf_g, ln_cf_b)):
                S.dma('sp', CFV3[:, i_, :], src[l].rearrange("(c p) -> p c", p=128), writes=['CFV'], allow_slow_non_contiguous=True)
            ONES = alloc(128)
            MEMSET('pool', ONES, 1.0 / 256.0, ['ONES'])
            TC = alloc(2 * NTOK); TC3 = r3(TC, NTOK)
            SQ = alloc(2 * 512); SQ3 = r3(SQ, 512)
            S.barrier()
            GLB = WST[:, 0:NTOK].bitcast(BF16); GLB3 = r3(GLB, NTOK)
            DG3 = r3(WINR[:, 0:62 * 128], 128)
            for j in range(2):
                CP('act', GLB3[:, j, :], GLU3[:, j, :], ['GLU'], ['GLB'])
                for k in range(31):
                    TS('dve', DG3[:, j * 31 + k, :], IDENT, WDW3[:, j, k:k + 1], None, ALU.mult, None, ['IDENT', 'WDW'], ['DG'])
            cbank = 0
            taporder = [15] + [k for k in range(31) if k != 15]
            for j in range(2):
                pb = 2 + cbank % 4; cbank += 1
                for idx, k in enumerate(taporder):
                    dlt = k - 15
                    lo = max(0, -dlt); hi = min(NCTX, NCTX - dlt)
                    MM(PS[pb][:, lo:hi], DG3[:, j * 31 + k, :], GLB3[:, j, lo + dlt:hi + dlt], idx == 0, idx == 30,
                       ['DG', 'GLB'], [f'ps{pb}'])
                ACT(TC3[:, j, 0:NCTX], PS[pb][:, 0:NCTX], AF.Identity, [f'ps{pb}', 'CFV'], ['TC'], bias=CFV3[:, 0, j:j + 1])
                for b in range(4):
                    pb = 2 + cbank % 4; cbank += 1
                    taps = []
                    for k in taporder:
                        dlt = k - 15
                        lo = max(8 * b, -dlt); hi = min(8 * b + 8, 32 - dlt)
                        if hi > lo:
                            taps.append((k, dlt, lo, hi))
                    for idx, (k, dlt, lo, hi) in enumerate(taps):
                        MM(PS[pb][:, (lo - 8 * b) * 64:(hi - 8 * b) * 64], DG3[:, j * 31 + k, :],
                           GLB3[:, j, NCTX + (lo + dlt) * 64:NCTX + (hi + dlt) * 64], idx == 0, idx == len(taps) - 1,
                           ['DG', 'GLB'], [f'ps{pb}'])
                    ACT(TC3[:, j, NCTX + b * 512:NCTX + (b + 1) * 512], PS[pb], AF.Identity, [f'ps{pb}', 'CFV'], ['TC'],
                        bias=CFV3[:, 0, j:j + 1])
            MEAN = alloc(512); RSTD = alloc(512); TN = alloc(512)
            for (tok0, ntok) in BLOCKS:
                for j in range(2):
                    ACT(SQ3[:, j, 0:ntok], TC3[:, j, tok0:tok0 + ntok], AF.Square, ['TC'], ['SQ'])
                for j in range(2):
                    MM(PS[0][:, 0:ntok], ONES, TC3[:, j, tok0:tok0 + ntok], j == 0, j == 1, ['ONES', 'TC'], ['ps0'])
                for j in range(2):
                    MM(PS[1][:, 0:ntok], ONES, SQ3[:, j, 0:ntok], j == 0, j == 1, ['ONES', 'SQ'], ['ps1'])
                CP('act', MEAN[:, 0:ntok], PS[0][:, 0:ntok], ['ps0'], ['MEAN'])
                TT('dve', RSTD[:, 0:ntok], MEAN[:, 0:ntok], MEAN[:, 0:ntok], ALU.mult, ['MEAN'], ['RSTD'])
                TT('dve', RSTD[:, 0:ntok], PS[1][:, 0:ntok], RSTD[:, 0:ntok], ALU.subtract, ['ps1', 'RSTD'], ['RSTD'])
                ACT(RSTD[:, 0:ntok], RSTD[:, 0:ntok], AF.Sqrt, ['RSTD', 'EPS'], ['RSTD'], bias=EPS[:, 0:1])
                OP('dve', 'reciprocal', ['RSTD'], ['RSTD'], out=RSTD[:, 0:ntok], in_=RSTD[:, 0:ntok])
                for j in range(2):
                    TT('dve', TN[:, 0:ntok], TC3[:, j, tok0:tok0 + ntok], MEAN[:, 0:ntok], ALU.subtract, ['TC', 'MEAN'], ['TN'])
                    TT('dve', TN[:, 0:ntok], TN[:, 0:ntok], RSTD[:, 0:ntok], ALU.mult, ['TN', 'RSTD'], ['TN'])
                    ACT(YCAT3[:, 6 + j, tok0:tok0 + ntok], TN[:, 0:ntok], AF.Silu, ['TN', 'CFV'], ['YCAT'],
                        scale=CFV3[:, 1, j:j + 1], bias=CFV3[:, 2, j:j + 1])
            S.barrier()

            if stop == 'c':
                break
            bump[0] = lay_mark
            G1 = modtile(2, 'G1')
            LG = alloc(1024); LB_ = alloc(1024)
            load_bc(LG, ln1_g[l:l + 1, :], 'LG'); load_bc(LB_, ln1_b[l:l + 1, :], 'LB_')
            WO_ST = alloc(8 * 512); WO = alloc(8 * 1024, BF16); WO3 = r3(WO, 1024)
            for hf in range(2):
                S.dma('sp', r3(WO_ST, 512), w_o[l, :, hf * 512:(hf + 1) * 512].rearrange("(kc k) f -> k kc f", k=128), writes=['WOST'])
                CP('act', WO3[:, :, hf * 512:(hf + 1) * 512], r3(WO_ST, 512), ['WOST'], ['WO'])
            XT = [alloc(1024), alloc(1024)]
            RT = [alloc(1024), alloc(1024)]
            ST6 = alloc(16); MV = alloc(8)

            def layer_norm_tile(rt, rk, lg, lb, gkeys):
                OP('dve', 'bn_stats', [rk], ['ST6'], out=ST6[:, 0:6], in_=rt[:, 0:512])
                OP('dve', 'bn_stats', [rk], ['ST6'], out=ST6[:, 6:12], in_=rt[:, 512:1024])
                OP('dve', 'bn_aggr', ['ST6'], ['MV'], out=MV[:, 0:2], in_=ST6[:, 0:12])
                ACT(MV[:, 2:3], MV[:, 1:2], AF.Sqrt, ['MV', 'EPS'], ['MV'], bias=EPS[:, 0:1])
                OP('dve', 'reciprocal', ['MV'], ['MV'], out=MV[:, 3:4], in_=MV[:, 2:3])
                STT(MV[:, 4:5], MV[:, 0:1], -1.0, MV[:, 3:4], ALU.mult, ALU.mult, ['MV'], ['MV'])
                ACT(rt, rt, AF.Identity, [rk, 'MV'], [rk], scale=MV[:, 3:4], bias=MV[:, 4:5])
                TT('dve', rt, rt, lg, ALU.mult, [rk] + gkeys, [rk])
                TT('pool', rt, rt, lb, ALU.add, [rk] + gkeys, [rk])

            tiles_E = range(18) if not last else range(2, 18)
            for ti in tiles_E:
                kd = kind_of(ti)
                xt = XT[ti % 2]; xk = f'XT{ti % 2}'
                rt = RT[ti % 2]; rk = f'RT{ti % 2}'
                S.dma('sp', xt, xs[ti * 128:(ti + 1) * 128, :], reads=['xs', 'xs2'], writes=[xk])
                for hf in range(2):
                    pb = (ti % 2) * 2 + hf
                    for kc in range(8):
                        MM(PS[pb], YCAT3[:, kc, ti * 128:(ti + 1) * 128], WO3[:, kc, hf * 512:(hf + 1) * 512], kc == 0, kc == 7,
                           ['YCAT', 'WO'], [f'ps{pb}'])
                    TT('dve', rt[:, hf * 512:(hf + 1) * 512], PS[pb], G1[kd][:, hf * 512:(hf + 1) * 512], ALU.mult,
                       [f'ps{pb}', f'G1{kd}'], [rk])
                STT(rt, xt, DN_ALPHA, rt, ALU.mult, ALU.add, [xk, rk], [rk])
                layer_norm_tile(rt, rk, LG, LB_, ['LG', 'LB_'])
                S.dma('pool', xs[ti * 128:(ti + 1) * 128, :], rt, reads=[rk], writes=[f'xs_t{ti}'])
            S.barrier()
            if dbg and l == 0:
                S.dma('sp', dbg_out[1, :, 0:D], xs, reads=[], writes=['dbg1'])
                S.barrier()
                if stop == 'ln1':
                    break

            bump[0] = persist_mark
            limit[0] = ARENA_COLS
            SH2 = modtile(3, 'SH2'); SC2 = modtile(4, 'SC2')
            for kind in range(2):
                TS('dve', SC2[kind], SC2[kind], 1.0, None, ALU.add, None, [f'SC2{kind}'], [f'SC2{kind}'])
            tiles_F = list(range(18)) if not last else list(range(2, 18))
            blocks_F = BLOCKS if not last else BLOCKS[1:]
            H2T = alloc(8 * NTOK, BF16); H2T3 = r3(H2T, NTOK)
            RW = alloc(18 * 16); RW3 = r3(RW, 16)
            WR = alloc(8 * 20); WR3 = r3(WR, 20)
            S.dma('sp', WR3[:, :, 0:4], w_rg[l].rearrange("(kc k) f -> k kc f", k=128), writes=['WR'])
            S.dma('sp', WR3[:, :, 4:20], w_rexp[l].rearrange("(kc k) f -> k kc f", k=128), writes=['WR'])
            BR = alloc(24)
            S.dma('sp', BR[:, 0:4], b_rg[l:l + 1, :].partition_broadcast(128), writes=['BR'])
            S.dma('sp', BR[:, 4:20], b_rexp[l:l + 1, :].partition_broadcast(128), writes=['BR'])
            RWT = alloc(NTOK, BF16)
            SEL = alloc(16 * 128, BF16); SEL3 = r3(SEL, 128)
            XTB = alloc(2048)
            XT = [XTB[:, 0:1024], XTB[:, 1024:2048]]
            SELF3 = r3(XTB, 128)
            CP('pool', SELF3[0:32], IDENT[0:32, 0:16].unsqueeze(2).to_broadcast([32, 16, 128]), ['IDENT'], ['XT0', 'XT1'])
            TT('pool', SELF3[0:32], SELF3[0:32], IDENT[0:32, 16:32].unsqueeze(2).to_broadcast([32, 16, 128]), ALU.add, ['IDENT', 'XT0', 'XT1'], ['XT0', 'XT1'])
            CP('pool', SEL3[0:32], SELF3[0:32], ['XT0', 'XT1'], ['SEL'])
            H32 = alloc(1024); H32_3 = r3(H32, 128)
            SM = alloc(64)
            LGT = SM[:, 0:20]; MG = SM[:, 20:24]; PEN = SM[:, 24:28]; SC_ = SM[:, 28:40]; EG = SM[:, 40:44]
            EM = alloc(16); EM2 = alloc(16); MK1 = alloc(16); MK2 = alloc(16)
            def f1_stageA(ti):
                kd = kind_of(ti)
                xt = XT[ti % 2]; xk = f'XT{ti % 2}'
                S.dma('sp', xt, xs[ti * 128:(ti + 1) * 128, :], reads=[f'xs_t{ti}'], writes=[xk])
                TT('dve', xt, xt, SC2[kd], ALU.mult, [xk, f'SC2{kd}'], [xk])
                TT('pool', xt, xt, SH2[kd], ALU.add, [xk, f'SH2{kd}'], [xk])

            def f1_stageA2(ti):
                xt = XT[ti % 2]; xk = f'XT{ti % 2}'
                for hh in range(2):
                    pb = 6 + hh
                    for k4 in range(4):
                        kc = hh * 4 + k4
                        TR(PS[pb][:, k4 * 128:(k4 + 1) * 128], xt[:, kc * 128:(kc + 1) * 128], [xk], [f'ps{pb}'])
                    CP('act', H2T3[:, hh * 4:(hh + 1) * 4, ti * 128:(ti + 1) * 128], r3(PS[pb], 128), [f'ps{pb}'], ['H2T'])
                    CP('act', H32_3[:, hh * 4:(hh + 1) * 4, :], r3(PS[pb], 128), [f'ps{pb}'], ['H32'])
                for kc in range(8):
                    MM(PS[5][:, 0:20], H32_3[:, kc, :], WR3[:, kc, :], kc == 0, kc == 7, ['H32', 'WR'], ['ps5'])

            NT = 18
            LGA = alloc(NT * 20); LGA3 = r3(LGA, 20)
            MEMSET('pool', LGA, 0.0, ['LGA'])

            def f1_stageB(ti):
                TT('dve', LGA3[:, ti, :], PS[5][:, 0:20], BR[:, 0:20], ALU.add, ['ps5', 'BR'], ['LGA'])

            f1_stageA(tiles_F[0])
            f1_stageA2(tiles_F[0])
            for ix, ti in enumerate(tiles_F):
                if ix + 1 < len(tiles_F):
                    f1_stageA(tiles_F[ix + 1])
                f1_stageB(ti)
                if ix + 1 < len(tiles_F):
                    f1_stageA2(tiles_F[ix + 1])
            def bc2(v, n):
                return v.unsqueeze(2).to_broadcast([128, NT, n])
            GMX = alloc(NT); GSUM = alloc(NT); GW = alloc(NT); M1 = alloc(NT); M2 = alloc(NT); DL = alloc(NT); EX = alloc(NT)
            P1 = alloc(NT); P2 = alloc(NT)
            MGA = alloc(NT * 4); D4 = alloc(NT * 4); PENA = alloc(NT * 4)
            EMA = alloc(NT * 16); EM2A = alloc(NT * 16); MK1A = alloc(NT * 16); MK2A = alloc(NT * 16)
            RW2 = alloc(NT * 32); RW2_3 = r3(RW2, 32)
            RWH = alloc(NT * 16, BF16)
            G4 = LGA3[:, :, 0:4]
            OP('dve', 'tensor_reduce', ['LGA'], ['GMX'], out=GMX, in_=G4, axis=AX.X, op=ALU.max)
            TT('dve', r3(MGA, 4), G4, bc2(GMX, 4), ALU.is_ge, ['LGA', 'GMX'], ['MGA'])
            TT('dve', r3(D4, 4), G4, bc2(GMX, 4), ALU.subtract, ['LGA', 'GMX'], ['D4'])
            ACT(D4, D4, AF.Exp, ['D4'], ['D4'])
            OP('dve', 'tensor_reduce', ['D4'], ['GSUM'], out=GSUM, in_=r3(D4, 4), axis=AX.X, op=ALU.add)
            OP('dve', 'reciprocal', ['GSUM'], ['GW'], out=GW, in_=GSUM)
            TS('dve', PENA, MGA, -1.0, 1e30, ALU.add, ALU.mult, ['MGA'], ['PENA'])
            TT('dve', r4(EMA, 4, 4), LGA3[:, :, 4:20].rearrange("p t (g j) -> p t g j", j=4),
               r3(PENA, 4).unsqueeze(3).to_broadcast([128, NT, 4, 4]), ALU.add, ['LGA', 'PENA'], ['EMA'])
            OP('dve', 'tensor_reduce', ['EMA'], ['M1'], out=M1, in_=r3(EMA, 16), axis=AX.X, op=ALU.max)
            TT('dve', r3(MK1A, 16), r3(EMA, 16), bc2(M1, 16), ALU.is_ge, ['EMA', 'M1'], ['MK1A'])
            STT(EM2A, MK1A, -1e30, EMA, ALU.mult, ALU.add, ['MK1A', 'EMA'], ['EM2A'])
            OP('dve', 'tensor_reduce', ['EM2A'], ['M2'], out=M2, in_=r3(EM2A, 16), axis=AX.X, op=ALU.max)
            TT('dve', r3(MK2A, 16), r3(EM2A, 16), bc2(M2, 16), ALU.is_ge, ['EM2A', 'M2'], ['MK2A'])
            TT('dve', DL, M2, M1, ALU.subtract, ['M1', 'M2'], ['DL'])
            ACT(EX, DL, AF.Exp, ['DL'], ['EX'])
            TS('dve', P1, EX, 1.0, None, ALU.add, None, ['EX'], ['P1'])
            OP('dve', 'reciprocal', ['P1'], ['P1'], out=P1, in_=P1)
            TT('dve', P2, EX, P1, ALU.mult, ['EX', 'P1'], ['P2'])
            TT('dve', P1, P1, GW, ALU.mult, ['P1', 'GW'], ['P1'])
            TT('dve', P2, P2, GW, ALU.mult, ['P2', 'GW'], ['P2'])
            TT('dve', r3(MK1A, 16), r3(MK1A, 16), bc2(P1, 16), ALU.mult, ['MK1A', 'P1'], ['MK1A'])
            TT('dve', r3(MK2A, 16), r3(MK2A, 16), bc2(P2, 16), ALU.mult, ['MK2A', 'P2'], ['MK2A'])
            TT('dve', MK1A, MK1A, MK2A, ALU.add, ['MK1A', 'MK2A'], ['MK1A'])
            CP('dve', RWH, MK1A, ['MK1A'], ['RWH'])
            CP('dve', RW2_3[:, :, 0:16], r3(RWH, 16), ['RWH'], ['RW2'])
            TT('dve', RW2_3[:, :, 16:32], r3(MK1A, 16), RW2_3[:, :, 0:16], ALU.subtract, ['MK1A', 'RW2'], ['RW2'])
            for ti in tiles_F:
                TR(PS[4][0:32, 0:128], RW2_3[:, ti, :], ['RW2'], ['ps4'])
                CP('act', RWT[0:32, ti * 128:(ti + 1) * 128], PS[4][0:32, 0:128], ['ps4'], ['RWT'])
            if stop in ('f1', 'f1a', 'f1b', 'f1c', 'f1d', 'f1a0', 'f1a1'):
                S.barrier()
                break
            S.barrier()
            FACC = alloc(18 * 1024); FACC3 = r3(FACC, 1024)
            EST = [alloc(2048), alloc(2048)]
            WGa_ = [alloc(2048, BF16), SC2[0].bitcast(BF16)]
            WUp_ = [alloc(2048, BF16), SC2[1].bitcast(BF16)]
            WDn_ = [alloc(2048, BF16), H32.bitcast(BF16)]

            def load_expert(e_):
                wp = e_ % 2
                S.dma('sp', r3(EST[0], 256), w_gate[l, e_].rearrange("(kc k) f -> k kc f", k=128), writes=['EST0'])
                CP('pool', WGa_[wp], EST[0], ['EST0'], [f'WGa{wp}'])
                S.dma('act', r3(EST[1], 256), w_up[l, e_].rearrange("(kc k) f -> k kc f", k=128), writes=['EST1'])
                CP('pool', WUp_[wp], EST[1], ['EST1'], [f'WUp{wp}'])
                S.dma('sp', r3(EST[0], 1024), w_down[l, e_].rearrange("(fc f) d -> f fc d", f=128), writes=['EST0'])
                CP('pool', WDn_[wp], EST[0], ['EST0'], [f'WDn{wp}'])
            SIL = [alloc(512), alloc(512)]
            ACTT = alloc(2 * 512, BF16); ACTT3 = r3(ACTT, 512)
            ACTTs3 = [ACTT3, r3(alloc(2 * 512, BF16), 512)]

            def exp_U(e_, bi, par):
                wp = e_ % 2
                WGa3 = r3(WGa_[wp], 256); WUp3 = r3(WUp_[wp], 256)
                tok0, ntok = blocks_F[bi]
                at3 = ACTTs3[par]
                for fc in range(2):
                    for kc in range(8):
                        MM(PS[fc][:, 0:ntok], WGa3[:, kc, fc * 128:(fc + 1) * 128], H2T3[:, kc, tok0:tok0 + ntok], kc == 0, kc == 7,
                           [f'WGa{wp}', 'H2T'], [f'ps{fc}'])
                    for kc in range(8):
                        MM(PS[2 + fc][:, 0:ntok], WUp3[:, kc, fc * 128:(fc + 1) * 128], H2T3[:, kc, tok0:tok0 + ntok], kc == 0, kc == 7,
                           [f'WUp{wp}', 'H2T'], [f'ps{2 + fc}'])
                    if fc == 0:
                        MM(PS[4][:, 0:ntok], SEL3[0:32, e_, :], RWT[0:32, tok0:tok0 + ntok], True, True, ['SEL', 'RWT'], ['ps4'])
                    ACT(SIL[fc][:, 0:ntok], PS[fc][:, 0:ntok], AF.Silu, [f'ps{fc}'], [f'SIL{fc}'])
                    TT('dve', SIL[fc][:, 0:ntok], SIL[fc][:, 0:ntok], PS[2 + fc][:, 0:ntok], ALU.mult,
                       [f'SIL{fc}', f'ps{2 + fc}'], [f'SIL{fc}'])
                    TT('dve', at3[:, fc, 0:ntok], SIL[fc][:, 0:ntok], PS[4][:, 0:ntok], ALU.mult,
                       [f'SIL{fc}', 'ps4'], [f'ACTT{par}{fc}'])

            dcount = [0]

            def exp_D(e_, bi, par):
                wp = e_ % 2
                WDn3 = r3(WDn_[wp], 1024)
                tok0, ntok = blocks_F[bi]
                at3 = ACTTs3[par]
                for i in range(ntok // 128):
                    ti = tok0 // 128 + i
                    for hf in range(2):
                        pb = 5 + dcount[0] % 3
                        dcount[0] += 1
                        for fc in range(2):
                            MM(PS[pb], at3[:, fc, i * 128:(i + 1) * 128], WDn3[:, fc, hf * 512:(hf + 1) * 512], fc == 0, fc == 1,
                               [f'ACTT{par}{fc}', f'WDn{wp}'], [f'ps{pb}'])
                        fk = f'FACC{ti}'
                        if e_ == 0:
                            CP('act', FACC3[:, ti, hf * 512:(hf + 1) * 512], PS[pb], [f'ps{pb}'], [fk])
                        else:
                            TT('dve', FACC3[:, ti, hf * 512:(hf + 1) * 512], FACC3[:, ti, hf * 512:(hf + 1) * 512], PS[pb], ALU.add,
                               [f'ps{pb}', fk], [fk])

            items_x = [(e_, bi) for e_ in range(NE) for bi in range(len(blocks_F))]
            load_expert(0)
            prev_x = None
            for ix, (e_, bi) in enumerate(items_x):
                exp_U(e_, bi, ix % 2)
                if prev_x is not None:
                    exp_D(*prev_x)
                if bi == 0 and e_ + 1 < NE:
                    load_expert(e_ + 1)
                prev_x = (e_, bi, ix % 2)
            exp_D(*prev_x)
            S.barrier()
            if stop == 'f2':
                break
            G2 = SH2
            for kind in range(2):
                load_bc(G2[kind], modv[l, kind:kind + 1, 5 * 1024:6 * 1024], f'G2{kind}')
            LG2 = EST[0][:, 0:1024]; LB2 = EST[0][:, 1024:2048]
            load_bc(LG2, ln2_g[l:l + 1, :], 'LG2'); load_bc(LB2, ln2_b[l:l + 1, :], 'LB2')
            ST6 = alloc(16); MV = alloc(8)
            for ti in tiles_F:
                kd = kind_of(ti)
                xt = XT[ti % 2]; xk = f'XT{ti % 2}'
                S.dma('sp', xt, xs[ti * 128:(ti + 1) * 128, :], reads=[f'xs_t{ti}'], writes=[xk])
                fk = f'FACC{ti}'
                ft = FACC3[:, ti, :]
                TT('pool', ft, ft, G2[kd], ALU.mult, [fk, f'G2{kd}'], [fk])
                STT(ft, xt, DN_ALPHA, ft, ALU.mult, ALU.add, [xk, fk], [fk])
                if stop == 'l0dbg':
                    S.dma('sp', dbg_out[2, 0:128, 0:1024], ft, reads=[fk], writes=['dbgx'])
                    S.dma('sp', dbg_out[2, 128:256, 0:1024], xt, reads=[xk], writes=['dbgx2'])
                    S.dma('sp', dbg_out[2, 256:384, 0:288], RW, reads=['RW'], writes=['dbgx3'])
                    break
                layer_norm_tile(ft, fk, LG2, LB2, ['LG2', 'LB2'])
                if last:
                    S.dma('pool', out[(ti - 2) * 128:(ti - 1) * 128, :], ft, reads=[fk], writes=[f'out{ti}'])
                else:
                    S.dma('pool', xs[ti * 128:(ti + 1) * 128, :], ft, reads=[fk], writes=['xs'])
            S.barrier()
            if dbg and l == 0:
                S.dma('sp', dbg_out[2, :, 0:D], xs, reads=[], writes=['dbg2'])
                S.barrier()
                if stop in ('l0', 'l0dbg'):
                    break
        S.barrier()
        S.emit()
        print("n ops", S.nops, {k: len(v) for k, v in S.ops.items()})
    return nc


_NC_CACHE = {}


def make_in_maps(inputs):
    ident = np.eye(128, dtype=np.float32)
    x = np.asarray(inputs['x'], np.float32)
    c = np.asarray(inputs['c'], np.float32)
    ctx = np.asarray(inputs['ctx'], np.float32)
    c_ctx = np.asarray(inputs['c_ctx'], np.float32)
    wnames = ['w_mod', 'b_mod', 'w_in', 's5_a_re', 's5_a_im', 's5_log_dt', 's5_b_re', 's5_b_im', 's5_c_re', 's5_c_im',
              's5_d', 'w_glu', 'b_glu', 'w_sc', 'w_dw', 'b_dw', 'ln_cf_g', 'ln_cf_b', 'w_o', 'ln1_g', 'ln1_b',
              'w_rg', 'b_rg', 'w_rexp', 'b_rexp', 'w_gate', 'w_up', 'w_down', 'ln2_g', 'ln2_b']
    shared = {n: np.ascontiguousarray(np.asarray(inputs[n], np.float32)) for n in wnames}
    maps = []
    for b in range(x.shape[0]):
        m = dict(shared)
        m['xin'] = np.ascontiguousarray(np.concatenate([ctx[b], x[b]], axis=0))
        cv = np.stack([c[b], c_ctx], axis=0)
        m['cT'] = np.ascontiguousarray(cv.reshape(2, 8, 128).transpose(2, 1, 0))
        m['ident'] = ident
        maps.append(m)
    return maps


def kernel(**inputs):
    if 'nc' not in _NC_CACHE:
        _NC_CACHE['nc'] = build_nc()
    nc = _NC_CACHE['nc']
    maps = make_in_maps(inputs)
    res = run_bass_kernel_spmd(nc, maps, core_ids=list(range(8)))
    return np.stack([np.asarray(r['out'], np.float32) for r in res.results], axis=0)
```

```python
import math
from contextlib import ExitStack
import numpy as np
import concourse.bass as bass
import concourse.mybir as mybir
from concourse.bass_utils import run_bass_kernel_spmd

F32 = mybir.dt.float32
BF16 = mybir.dt.bfloat16
I32 = mybir.dt.int32
AF = mybir.ActivationFunctionType
ALU = mybir.AluOpType
AX = mybir.AxisListType

D = 1024
DEPTH = 2
NTOK = 2304
NCTX = 256
NLAT = 2048
D_IN = 1792
NE = 16
DN_ALPHA = (2 * DEPTH) ** 0.25
LN_EPS = 1e-5
NCH = 288
BLOCKS = [(0, 256)] + [(256 + 512 * i, 512) for i in range(4)]
TWO_PI = 2.0 * math.pi
C1 = 6.28125
C2 = TWO_PI - C1


class Sched:
    ENG = ('pe', 'dve', 'act', 'pool', 'sp')

    def __init__(self, nc, stack):
        self.nc = nc
        self.ops = {e: [] for e in self.ENG}
        self.sem = {}
        for e in ('pe', 'dve', 'act', 'pool'):
            self.sem['c_' + e] = stack.enter_context(nc.semaphore('c_' + e))
        self.NDMA = 12
        self.rr = {}
        for q in ('sp', 'act', 'pool'):
            self.rr[q] = 0
            for i in range(self.NDMA):
                self.sem[f'd_{q}{i}'] = stack.enter_context(nc.semaphore(f'd_{q}{i}'))
        self.cnt = {k: 0 for k in self.sem}
        self.lastw = {}
        self.readers = {}
        self.waited = {e: {} for e in self.ENG}
        self.nops = 0
        self.debug = False

    def _deps(self, eng, reads, writes, extra=()):
        deps = {}
        for s, v in extra:
            deps[s] = v

        def add(s, v):
            if deps.get(s, 0) < v:
                deps[s] = v
        for k in reads:
            if k in self.lastw:
                add(*self.lastw[k])
        for k in writes:
            if k in self.lastw:
                add(*self.lastw[k])
            for s, v in self.readers.get(k, {}).items():
                add(s, v)
        waits = []
        w = self.waited[eng]
        for s, v in deps.items():
            if eng == 'pe' and s == 'c_pe':
                continue
            if w.get(s, 0) < v:
                w[s] = v
                waits.append((s, v))
        return waits

    def _mark(self, me, reads, writes):
        s, v = me
        for k in reads:
            self.readers.setdefault(k, {})[s] = v
        for k in writes:
            self.lastw[k] = me
            self.readers[k] = {}

    def op(self, eng, fn, reads=(), writes=()):
        if self.debug:
            import sys as _s
            fr = _s._getframe(1)
            ln = []
            while fr is not None and len(ln) < 3:
                ln.append(fr.f_lineno)
                fr = fr.f_back
            fn0 = fn
            fn = lambda e, fn0=fn0, ln=tuple(ln): fn0(e).annotate(f"L{ln}")
        waits = self._deps(eng, reads, writes)
        s = 'c_' + eng
        self.cnt[s] += 1
        self.ops[eng].append((waits, fn, s, 1))
        self._mark((s, self.cnt[s]), reads, writes)
        self.nops += 1

    def dma(self, q, out, in_, reads=(), writes=(), **kw):
        i = self.rr[q]
        self.rr[q] = (i + 1) % self.NDMA
        s = f'd_{q}{i}'
        extra = [(s, self.cnt[s])] if self.cnt[s] > 0 else []
        waits = self._deps(q, reads, writes, extra)
        self.cnt[s] += 16
        self.ops[q].append((waits, lambda e: e.dma_start(out=out, in_=in_, **kw), s, 16))
        self._mark((s, self.cnt[s]), reads, writes)
        self.nops += 1

    def barrier(self):
        for e in self.ENG:
            waits = []
            w = self.waited[e]
            for s, v in self.cnt.items():
                if v > 0 and w.get(s, 0) < v:
                    w[s] = v
                    waits.append((s, v))
            if waits:
                self.ops[e].append((waits, None, None, 0))
        self.lastw = {}
        self.readers = {}

    def emit(self):
        nc = self.nc
        with nc.Block() as block:
            decos = {'pe': block.tensor, 'dve': block.vector, 'act': block.scalar,
                     'pool': block.gpsimd, 'sp': block.sync}
            for e in self.ENG:
                ops = self.ops[e]

                def body(engine, ops=ops):
                    for waits, fn, s, inc in ops:
                        for (ws, wv) in waits:
                            engine.wait_ge(self.sem[ws], wv)
                        if fn is not None:
                            fn(engine).then_inc(self.sem[s], inc)
                decos[e](body)


def build_nc(dbg=False, stop=None):
    nc = bass.Bass("TRN2", target_bir_lowering=False)
    di = {}

    def inp(name, shape):
        di[name] = nc.dram_tensor(name, list(shape), F32, kind="ExternalInput").ap()
        return di[name]
    L = DEPTH
    xin = inp('xin', (NTOK, D))
    cT = inp('cT', (128, 8, 2))
    ident_d = inp('ident', (128, 128))
    w_mod = inp('w_mod', (L, D, 6 * D)); b_mod = inp('b_mod', (L, 6 * D))
    w_in = inp('w_in', (L, D, D_IN))
    a_re = inp('s5_a_re', (L, 2, 32, 64)); a_im = inp('s5_a_im', (L, 2, 32, 64)); log_dt = inp('s5_log_dt', (L, 2, 32))
    b_re = inp('s5_b_re', (L, 2, 32, 64, 16)); b_im = inp('s5_b_im', (L, 2, 32, 64, 16))
    c_re = inp('s5_c_re', (L, 2, 32, 16, 64)); c_im = inp('s5_c_im', (L, 2, 32, 16, 64))
    s5_d = inp('s5_d', (L, 512)); w_glu = inp('w_glu', (L, 512, 512)); b_glu = inp('b_glu', (L, 512))
    w_sc = inp('w_sc', (L, 3, 256)); w_dw = inp('w_dw', (L, 31, 256)); b_dw = inp('b_dw', (L, 256))
    ln_cf_g = inp('ln_cf_g', (L, 256)); ln_cf_b = inp('ln_cf_b', (L, 256))
    w_o = inp('w_o', (L, D, D)); ln1_g = inp('ln1_g', (L, D)); ln1_b = inp('ln1_b', (L, D))
    w_rg = inp('w_rg', (L, D, 4)); b_rg = inp('b_rg', (L, 4)); w_rexp = inp('w_rexp', (L, D, 16)); b_rexp = inp('b_rexp', (L, 16))
    w_gate = inp('w_gate', (L, NE, D, 256)); w_up = inp('w_up', (L, NE, D, 256)); w_down = inp('w_down', (L, NE, 256, D))
    ln2_g = inp('ln2_g', (L, D)); ln2_b = inp('ln2_b', (L, D))
    out = nc.dram_tensor('out', [NLAT, D], F32, kind="ExternalOutput").ap()
    xs = nc.dram_tensor('xs', [NTOK, D], F32).ap()
    modv = nc.dram_tensor('modv', [L, 2, 6 * D], F32).ap()
    dbg_out = None
    if dbg:
        dbg_out = nc.dram_tensor('dbg', [3, NTOK, 9216], F32, kind="ExternalOutput").ap()

    with ExitStack() as st:
        S = Sched(nc, st)
        S.debug = dbg
        ARENA_COLS = 51000
        arena = st.enter_context(nc.sbuf_tensor("arena", [128, ARENA_COLS], F32))[:]
        PS = [st.enter_context(nc.psum_tensor(f"ps{i}", [128, 512], F32)) for i in range(8)]
        PS = [p_[:] for p_ in PS]
        bump = [0]
        limit = [ARENA_COLS]
        YCAT_COLS = 8 * NTOK // 2

        def alloc(cols, dt=F32, shape=None):
            c32 = cols if dt == F32 else (cols + 1) // 2
            c32 = (c32 + 7) // 8 * 8
            o = bump[0]
            bump[0] += c32
            assert bump[0] <= limit[0], (bump[0], limit[0])
            v = arena[:, o:o + c32]
            if dt != F32:
                v = v.bitcast(dt)
            v = v[:, 0:cols]
            return v

        def r3(v, b):
            return v.rearrange("p (a b) -> p a b", b=b)

        def r4(v, b, c):
            return v.rearrange("p (a b c) -> p a b c", b=b, c=c)

        IDENT = alloc(128)
        EPS = alloc(8)
        S.dma('sp', IDENT, ident_d, writes=['IDENT'])
        S.op('pool', lambda e: e.memset(EPS, LN_EPS), writes=['EPS'])
        CT = alloc(16)
        SCT = alloc(16)
        persist_mark = bump[0]

        def TT(eng, out_, a, b, op, reads, writes):
            S.op(eng, lambda e: e.tensor_tensor(out=out_, in0=a, in1=b, op=op), reads, writes)

        def TS(eng, out_, a, s1, s2, op0, op1, reads, writes):
            if op1 is None:
                S.op(eng, lambda e: e.tensor_scalar(out=out_, in0=a, scalar1=s1, scalar2=None, op0=op0), reads, writes)
            else:
                S.op(eng, lambda e: e.tensor_scalar(out=out_, in0=a, scalar1=s1, scalar2=s2, op0=op0, op1=op1), reads, writes)

        def STT(out_, a, sc, b, op0, op1, reads, writes):
            S.op('dve', lambda e: e.scalar_tensor_tensor(out=out_, in0=a, scalar=sc, in1=b, op0=op0, op1=op1), reads, writes)

        def ACT(out_, in_, func, reads, writes, scale=None, bias=None, accum_out=None):
            kw = {}
            if scale is not None:
                kw['scale'] = scale
            if bias is not None:
                kw['bias'] = bias
            if accum_out is not None:
                kw['accum_out'] = accum_out
            S.op('act', lambda e: e.activation(out=out_, in_=in_, func=func, **kw), reads, writes)

        def CP(eng, out_, in_, reads, writes):
            if eng == 'act':
                S.op('act', lambda e: e.copy(out=out_, in_=in_), reads, writes)
            else:
                S.op(eng, lambda e: e.tensor_copy(out=out_, in_=in_), reads, writes)

        def MM(out_, lhsT, rhs, start, stop, reads, writes):
            S.op('pe', lambda e: e.matmul(out_, lhsT=lhsT, rhs=rhs, start=start, stop=stop), reads, writes)

        def TR(out_, in_, reads, writes):
            n = in_.shape[0]
            S.op('pe', lambda e: e.transpose(out=out_, in_=in_, identity=IDENT[0:n, 0:n]), list(reads) + ['IDENT'], writes)

        def OP(eng, meth, reads, writes, **kw):
            S.op(eng, lambda e: getattr(e, meth)(**kw), reads, writes)

        def MEMSET(eng, ap, val, writes):
            S.op(eng, lambda e: e.memset(ap, val), (), writes)

        S.dma('sp', xs[0:1152], xin[0:1152], writes=['xs'])
        S.dma('pool', xs[1152:2304], xin[1152:2304], writes=['xs2'])
        S.dma('sp', CT, cT.rearrange("k a r -> k (a r)"), writes=['CT'])
        ACT(SCT, CT, AF.Silu, ['CT'], ['SCT'])
        SCT3 = r3(SCT, 2)
        def emit_mod(l_, WM, MODSB, BM, sfx):
            for half in range(2):
                S.dma('pool', BM[0:2, :], b_mod[l_:l_ + 1, half * 3072:(half + 1) * 3072].partition_broadcast(2), writes=['BM' + sfx])
                for kc in range(8):
                    wb = WM[kc % 2]
                    S.dma(('sp', 'act')[kc % 2], wb, w_mod[l_, kc * 128:(kc + 1) * 128, half * 3072:(half + 1) * 3072],
                          writes=[f'WM{kc % 2}' + sfx])
                    for n in range(6):
                        MM(PS[n][0:2, :], SCT3[:, kc, :], wb[:, n * 512:(n + 1) * 512], kc == 0, kc == 7,
                           ['SCT', f'WM{kc % 2}' + sfx], [f'ps{n}'])
                for n in range(6):
                    CP('act', MODSB[0:2, n * 512:(n + 1) * 512], PS[n][0:2, :], [f'ps{n}'], ['MODSB' + sfx])
                TT('pool', MODSB[0:2, :], MODSB[0:2, :], BM[0:2, :], ALU.add, ['MODSB' + sfx, 'BM' + sfx], ['MODSB' + sfx])
                S.dma('sp', modv[l_, :, half * 3072:(half + 1) * 3072], MODSB[0:2, :], reads=['MODSB' + sfx], writes=['modv'])

        WM = [alloc(3072), alloc(3072)]
        MODSB = alloc(3072)
        BM = alloc(3072)
        emit_mod(0, WM, MODSB, BM, '')
        S.barrier()

        for l in range(L):
            if stop == 'p0':
                break
            last = (l == L - 1)
            bump[0] = persist_mark

            def load_bc(dst, src_row, key):
                S.dma('sp', dst, src_row.partition_broadcast(128), reads=['modv'], writes=[key])

            def modtile(j, key):
                ts = []
                for kind in range(2):
                    t = alloc(1024)
                    load_bc(t, modv[l, kind:kind + 1, j * 1024:(j + 1) * 1024], f'{key}{kind}')
                    ts.append(t)
                return ts

            def kind_of(ti):
                return 1 if ti < 2 else 0

            YCAT = arena[:, ARENA_COLS - YCAT_COLS:ARENA_COLS].bitcast(BF16); YCAT3 = r3(YCAT, NTOK)
            lay_mark = bump[0]
            limit[0] = ARENA_COLS

            UT = alloc(4 * NTOK, BF16); UT3 = r3(UT, NTOK)
            YACC = alloc(4 * NTOK); YACC3 = r3(YACC, NTOK)
            s5_mark = bump[0]
            SH1 = modtile(0, 'SH1'); SC1 = modtile(1, 'SC1')
            for kind in range(2):
                TS('dve', SC1[kind], SC1[kind], 1.0, None, ALU.add, None, [f'SC1{kind}'], [f'SC1{kind}'])
            MODC = [alloc(16), alloc(16)]
            for kind in range(2):
                S.dma('sp', MODC[kind][:, 0:8], modv[l, kind, 0:1024].rearrange("(kc k) -> k kc", k=128), reads=['modv'], writes=['MODC'],
                      allow_slow_non_contiguous=True)
                S.dma('sp', MODC[kind][:, 8:16], modv[l, kind, 1024:2048].rearrange("(kc k) -> k kc", k=128), reads=['modv'], writes=['MODC'],
                      allow_slow_non_contiguous=True)
                TS('dve', MODC[kind][:, 8:16], MODC[kind][:, 8:16], 1.0, None, ALU.add, None, ['MODC'], ['MODC'])
            if stop == 'b1a':
                break
            WST = alloc(8 * 512)
            WST3 = r3(WST, 512)
            WINU = alloc(8 * 512, BF16); WINU3 = r3(WINU, 512)
            S.dma('sp', WST3, w_in[l, :, 0:512].rearrange("(kc k) f -> k kc f", k=128), writes=['WST'])
            CP('act', WINU, WST, ['WST'], ['WINU'])
            DSK = alloc(4)
            S.dma('sp', DSK, s5_d[l].rearrange("(c p) -> p c", p=128), writes=['DSK'], allow_slow_non_contiguous=True)
            if stop == 'b1b':
                break
            XT = [alloc(1024), alloc(1024)]
            HTs3 = [r3(alloc(8 * 512, BF16), 512), r3(alloc(8 * 512, BF16), 512)]

            def make_hT(tok0, ntok, sh, sc, shk, sck, hpar=0):
                HT3 = HTs3[hpar]
                for i in range(ntok // 128):
                    ti = (tok0 // 128) + i
                    kd = kind_of(ti)
                    xt = XT[ti % 2]
                    xk = f'XT{ti % 2}'
                    S.dma('sp', xt, xs[ti * 128:(ti + 1) * 128, :], reads=['xs', 'xs2'], writes=[xk])
                    for hh in range(2):
                        pb = 6 + hh
                        for k4 in range(4):
                            kc = hh * 4 + k4
                            TR(PS[pb][:, k4 * 128:(k4 + 1) * 128], xt[:, kc * 128:(kc + 1) * 128], [xk], [f'ps{pb}'])
                        for k4 in range(4):
                            kc = hh * 4 + k4
                            ACT(HT3[:, kc, i * 128:(i + 1) * 128], PS[pb][:, k4 * 128:(k4 + 1) * 128], AF.Identity,
                                [f'ps{pb}', 'MODC'], [f'HT{hpar}'], scale=MODC[kd][:, 8 + kc:9 + kc], bias=MODC[kd][:, kc:kc + 1])

            make_hT(BLOCKS[0][0], BLOCKS[0][1], SH1, SC1, 'SH1', 'SC1', 0)
            for bix, (tok0, ntok) in enumerate(BLOCKS):
                hpar = bix % 2
                HT3 = HTs3[hpar]
                if bix + 1 < len(BLOCKS):
                    make_hT(BLOCKS[bix + 1][0], BLOCKS[bix + 1][1], SH1, SC1, 'SH1', 'SC1', (bix + 1) % 2)
                if stop == 'b1c':
                    break
                for fc in range(4):
                    pb = fc % 2
                    for kc in range(8):
                        MM(PS[pb][:, 0:ntok], WINU3[:, kc, fc * 128:(fc + 1) * 128], HT3[:, kc, 0:ntok], kc == 0, kc == 7,
                           ['WINU', f'HT{hpar}'], [f'ps{pb}'])
                    CP('act', UT3[:, fc, tok0:tok0 + ntok], PS[pb][:, 0:ntok], [f'ps{pb}'], ['UT'])
                    if stop == 'b1d':
                        continue
                    ACT(YACC3[:, fc, tok0:tok0 + ntok], PS[pb][:, 0:ntok], AF.Copy, [f'ps{pb}', 'DSK'], ['YACC'], scale=DSK[:, fc:fc + 1])
                if stop in ('b1d', 'b1e'):
                    break
            S.barrier()
            bump[0] = s5_mark
            if stop in ('b1', 'b1c', 'b1d', 'b1e'):
                break

            def T32():
                return alloc(32)
            NAT = alloc(128)
            NAT2 = alloc(8)
            ARE = T32(); AIM = T32(); DT = T32()

            def load_T(dst, src2d, key):
                S.dma('sp', NAT[0:32, :], src2d, writes=['NAT'])
                TR(PS[0][:, 0:32], NAT[0:32, :], ['NAT'], ['ps0'])
                CP('act', dst, PS[0][:, 0:32], ['ps0'], [key])
            load_T(ARE, a_re[l].rearrange("d (pr g2) p -> (d pr) (g2 p)", g2=2), 'ARE')
            load_T(AIM, a_im[l].rearrange("d (pr g2) p -> (d pr) (g2 p)", g2=2), 'AIM')
            S.dma('sp', NAT2[0:32, 0:2], log_dt[l].rearrange("d (pr g2) -> (d pr) g2", g2=2), writes=['NAT2'])
            CP('dve', r3(NAT[0:32, :], 64), NAT2[0:32, 0:2].unsqueeze(2).to_broadcast([32, 2, 64]), ['NAT2', 'NAT'], ['NAT'])
            TR(PS[0][:, 0:32], NAT[0:32, :], ['NAT'], ['ps0'])
            ACT(DT, PS[0][:, 0:32], AF.Exp, ['ps0'], ['DT'])
            RHO = T32(); TH = T32(); KF = T32(); KI = alloc(32).bitcast(I32); TMP = T32(); TMP2 = T32()
            TT('dve', RHO, ARE, DT, ALU.mult, ['ARE', 'DT'], ['RHO'])
            TT('dve', TH, AIM, DT, ALU.mult, ['AIM', 'DT'], ['TH'])
            TS('dve', TMP, TH, 1.0 / TWO_PI, None, ALU.mult, None, ['TH'], ['TMP'])
            CP('dve', KI, TMP, ['TMP'], ['KI'])
            CP('dve', KF, KI, ['KI'], ['KF'])
            STT(TMP, KF, -C1, TH, ALU.mult, ALU.add, ['KF', 'TH'], ['TMP'])
            STT(TMP2, KF, -C2, TMP, ALU.mult, ALU.add, ['KF', 'TMP'], ['TMP2'])
            SHh = T32(); CHh = T32(); HPI = alloc(8)
            MEMSET('pool', HPI, math.pi / 2, ['HPI'])
            ACT(SHh, TMP2, AF.Sin, ['TMP2'], ['SHh'], scale=0.5)
            ACT(CHh, TMP2, AF.Sin, ['TMP2', 'HPI'], ['CHh'], scale=-0.5, bias=HPI[:, 0:1])
            C1T = T32(); S1T = T32(); R1 = T32()
            TT('dve', TMP, CHh, CHh, ALU.mult, ['CHh'], ['TMP'])
            TT('dve', TMP2, SHh, SHh, ALU.mult, ['SHh'], ['TMP2'])
            TT('dve', C1T, TMP, TMP2, ALU.subtract, ['TMP', 'TMP2'], ['C1T'])
            TT('dve', TMP, SHh, CHh, ALU.mult, ['SHh', 'CHh'], ['TMP'])
            TS('dve', S1T, TMP, 2.0, None, ALU.mult, None, ['TMP'], ['S1T'])
            ACT(R1, RHO, AF.Exp, ['RHO'], ['R1'])
            ER = alloc(16 * 32); EI = alloc(16 * 32); ER3 = r3(ER, 32); EI3 = r3(EI, 32)
            MEMSET('pool', ER3[:, 0, :], 1.0, ['ER'])
            MEMSET('pool', EI3[:, 0, :], 0.0, ['EI'])
            for s in range(15):
                TT('dve', TMP, ER3[:, s, :], C1T, ALU.mult, ['ER', 'C1T'], ['TMP'])
                TT('dve', TMP2, EI3[:, s, :], S1T, ALU.mult, ['EI', 'S1T'], ['TMP2'])
                TT('dve', ER3[:, s + 1, :], TMP, TMP2, ALU.subtract, ['TMP', 'TMP2'], ['ER'])
                TT('dve', TMP, ER3[:, s, :], S1T, ALU.mult, ['ER', 'S1T'], ['TMP'])
                TT('dve', TMP2, EI3[:, s, :], C1T, ALU.mult, ['EI', 'C1T'], ['TMP2'])
                TT('dve', EI3[:, s + 1, :], TMP, TMP2, ALU.add, ['TMP', 'TMP2'], ['EI'])
            RP = alloc(8 * 32); RP3 = r3(RP, 32)
            for k in range(1, 9):
                ACT(RP3[:, k - 1, :], RHO, AF.Exp, ['RHO'], ['RP'], scale=float(k))
            QR = T32(); QI = T32(); NR = T32(); NI = T32(); DEN = T32()
            TT('dve', TMP, R1, C1T, ALU.mult, ['R1', 'C1T'], ['TMP'])
            TS('dve', NR, TMP, -1.0, None, ALU.add, None, ['TMP'], ['NR'])
            TT('dve', NI, R1, S1T, ALU.mult, ['R1', 'S1T'], ['NI'])
            TT('dve', TMP, ARE, ARE, ALU.mult, ['ARE'], ['TMP'])
            TT('dve', TMP2, AIM, AIM, ALU.mult, ['AIM'], ['TMP2'])
            TT('dve', DEN, TMP, TMP2, ALU.add, ['TMP', 'TMP2'], ['DEN'])
            OP('dve', 'reciprocal', ['DEN'], ['DEN'], out=DEN, in_=DEN)
            TT('dve', TMP, NR, ARE, ALU.mult, ['NR', 'ARE'], ['TMP'])
            TT('dve', TMP2, NI, AIM, ALU.mult, ['NI', 'AIM'], ['TMP2'])
            TT('dve', TMP, TMP, TMP2, ALU.add, ['TMP', 'TMP2'], ['TMP'])
            TT('dve', QR, TMP, DEN, ALU.mult, ['TMP', 'DEN'], ['QR'])
            TT('dve', TMP, NI, ARE, ALU.mult, ['NI', 'ARE'], ['TMP'])
            TT('dve', TMP2, NR, AIM, ALU.mult, ['NR', 'AIM'], ['TMP2'])
            TT('dve', TMP, TMP, TMP2, ALU.subtract, ['TMP', 'TMP2'], ['TMP'])
            TT('dve', QI, TMP, DEN, ALU.mult, ['TMP', 'DEN'], ['QI'])
            FR = alloc(256); FI = alloc(256); ELR = alloc(256); ELI = alloc(256); KR = alloc(256); KIm = alloc(256)
            FR3 = r3(FR, 8); FI3 = r3(FI, 8); ELR3 = r3(ELR, 8); ELI3 = r3(ELI, 8); KR3 = r3(KR, 8); KI3 = r3(KIm, 8)
            for s in range(8):
                TT('dve', TMP, ER3[:, s, :], QR, ALU.mult, ['ER', 'QR'], ['TMP'])
                TT('dve', TMP2, EI3[:, s, :], QI, ALU.mult, ['EI', 'QI'], ['TMP2'])
                TT('dve', FR3[:, :, s], TMP, TMP2, ALU.add, ['TMP', 'TMP2'], ['FR'])
                TT('dve', TMP, ER3[:, s, :], QI, ALU.mult, ['ER', 'QI'], ['TMP'])
                TT('dve', TMP2, EI3[:, s, :], QR, ALU.mult, ['EI', 'QR'], ['TMP2'])
                TT('dve', FI3[:, :, s], TMP, TMP2, ALU.subtract, ['TMP', 'TMP2'], ['FI'])
                CP('pool', ELR3[:, :, s], ER3[:, s, :], ['ER'], ['ELR'])
                CP('pool', ELI3[:, :, s], EI3[:, s, :], ['EI'], ['ELI'])
                TT('dve', KR3[:, :, s], ER3[:, s + 8, :], RP3[:, s, :], ALU.mult, ['ER', 'RP'], ['KR'])
                TT('dve', KI3[:, :, s], EI3[:, s + 8, :], RP3[:, s, :], ALU.mult, ['EI', 'RP'], ['KI_'])
            LRR = alloc(64); LII = alloc(64); LRR3 = r3(LRR, 2); LII3 = r3(LII, 2)
            for c in range(2):
                TT('dve', LRR3[:, :, c], ER3[:, 8, :], RP3[:, 7, :], ALU.mult, ['ER', 'RP'], ['LRR'])
                TT('dve', LII3[:, :, c], EI3[:, 8, :], RP3[:, 7, :], ALU.mult, ['EI', 'RP'], ['LII'])
            BRE = alloc(1024); BIM = alloc(1024); CTR = alloc(1024); CTI = alloc(1024)
            BRE3 = r3(BRE, 32); BIM3 = r3(BIM, 32); CTR3 = r3(CTR, 32); CTI3 = r3(CTI, 32)
            CN = alloc(32 * 128); CN3 = r3(CN, 128)
            for (dst3, src, key) in ((BRE3, b_re, 'BRE'), (BIM3, b_im, 'BIM')):
                MEMSET('pool', dst3, 0.0, [key])
                v = src[l].rearrange("d (pr g2) p n -> g2 p (d pr) n", g2=2)
                for g2 in range(2):
                    S.dma('sp', dst3[g2 * 64:(g2 + 1) * 64, :, g2 * 16:(g2 + 1) * 16], v[g2], writes=[key])
            for (dst3, src, key) in ((CTR3, c_re, 'CTR'), (CTI3, c_im, 'CTI')):
                MEMSET('pool', CN3[0:32], 0.0, ['CN'])
                v = src[l].rearrange("d (pr g2) n p -> g2 n (d pr) p", g2=2)
                for g2 in range(2):
                    S.dma('sp', CN3[g2 * 16:(g2 + 1) * 16, :, g2 * 64:(g2 + 1) * 64], v[g2], writes=['CN'])
                for ub in range(2):
                    for uu in range(16):
                        u = ub * 16 + uu
                        TR(PS[ub][:, uu * 32:(uu + 1) * 32], CN3[0:32, u, :], ['CN'], [f'ps{ub}'])
                    CP('act', dst3[:, ub * 16:(ub + 1) * 16, :], r3(PS[ub], 32), [f'ps{ub}'], [key])
            if stop == 'tab':
                break
            XZ = alloc(32 * 289 * 2, BF16); XZ4 = r4(XZ, 289, 2)
            MEMSET('pool', XZ, 0.0, ['XZ'])
            WB = [alloc(8 * 128), alloc(8 * 128)]
            T1 = alloc(256); T2 = alloc(256)
            LBm = {}
            LCm = {}
            for d in range(2):
                for ri in range(2):
                    LBm[d, ri] = alloc(8 * 128, BF16)
                    LCm[d, ri] = alloc(8 * 128, BF16)
            DEC = [alloc(512), alloc(512)]

            def bc_s(tab3, u):
                return tab3[:, u, :].unsqueeze(2).to_broadcast([128, 8, 32])

            def bc_m(mat3, u):
                return mat3[:, u, :].unsqueeze(1).to_broadcast([128, 8, 32])

            def cplx_outer(eng, out_re, out_im, Ar, Ai, Br, Bi, u, keys_r, neg_im, okeys):
                t1 = r3(T1, 32); t2 = r3(T2, 32)
                TT(eng, t1, bc_s(Ar, u), bc_m(Br, u), ALU.mult, keys_r, ['T1'])
                TT(eng, t2, bc_s(Ai, u), bc_m(Bi, u), ALU.mult, keys_r, ['T2'])
                TT(eng, out_re, t1, t2, ALU.subtract, ['T1', 'T2'], [okeys[0]])
                TT(eng, t1, bc_s(Ar, u), bc_m(Bi, u), ALU.mult, keys_r, ['T1'])
                TT(eng, t2, bc_s(Ai, u), bc_m(Br, u), ALU.mult, keys_r, ['T2'])
                if neg_im:
                    S.op('dve', lambda e: e.scalar_tensor_tensor(out=out_im, in0=t1, scalar=-1.0, in1=t2, op0=ALU.mult, op1=ALU.subtract),
                         ['T1', 'T2'], [okeys[1]])
                else:
                    TT(eng, out_im, t1, t2, ALU.add, ['T1', 'T2'], [okeys[1]])

            tabkeys = ['FR', 'FI', 'ELR', 'ELI', 'KR', 'KI_', 'BRE', 'BIM', 'CTR', 'CTI']

            def gen_unit(d, pair, carry):
                u = d * 16 + pair
                q = pair % 4
                win = slice(32 * q, 32 * q + 32)
                if not carry:
                    WBr3 = r3(WB[0], 128); WBi3 = r3(WB[1], 128)
                    cplx_outer('dve', WBr3[:, :, win], WBi3[:, :, win], FR3, FI3, BRE3, BIM3, u, tabkeys, False, ['WB0', 'WB1'])
                    for ri in range(2):
                        w3 = r3(WB[ri], 128)
                        for hb in range(2):
                            pb = 6 + hb
                            for s4 in range(4):
                                s = hb * 4 + s4
                                TR(PS[pb][:, s4 * 128:(s4 + 1) * 128], w3[:, s, :], [f'WB{ri}'], [f'ps{pb}'])
                            CP('act', LBm[d, ri][:, hb * 512:(hb + 1) * 512], PS[pb], [f'ps{pb}'], [f'LB{d}{ri}'])
                    lc_r = r3(LCm[d, 0], 128); lc_i = r3(LCm[d, 1], 128)
                    cplx_outer('dve', lc_r[:, :, win], lc_i[:, :, win], ELR3, ELI3, CTR3, CTI3, u, tabkeys, True, [f'LC{d}0', f'LC{d}1'])
                else:
                    lc_r = r3(LCm[d, 0], 128); lc_i = r3(LCm[d, 1], 128)
                    cplx_outer('dve', lc_r[:, :, win], lc_i[:, :, win], KR3, KI3, CTR3, CTI3, u, tabkeys, True, [f'LC{d}0', f'LC{d}1'])

            def zero_windows(carry):
                if not carry:
                    MEMSET('pool', WB[0], 0.0, ['WB0'])
                    MEMSET('pool', WB[1], 0.0, ['WB1'])
                for d in range(2):
                    for ri in range(2):
                        MEMSET('pool', LCm[d, ri], 0.0, [f'LC{d}{ri}'])

            def slots(d, bi, plus):
                tok0, ntok = BLOCKS[bi]
                nch = ntok // 8
                if d == 0:
                    jj0 = tok0 // 8
                    return slice(jj0 + plus, jj0 + plus + nch)
                if bi == 0:
                    hi = 31 + plus
                else:
                    cl0 = (tok0 - NCTX) // 8
                    hi = 287 - cl0 + plus
                lo = hi - nch
                return slice(hi, lo if lo >= 0 else None, -1)

            order = [(q, pair) for q in range(4) for pair in range(q, 16, 4)]
            def gen_B(d, pair):
                u = d * 16 + pair
                q = pair % 4
                win = slice(32 * q, 32 * q + 32)
                WBr3 = r3(WB[0], 128); WBi3 = r3(WB[1], 128)
                cplx_outer('dve', WBr3[:, :, win], WBi3[:, :, win], FR3, FI3, BRE3, BIM3, u, tabkeys, False, ['WB0', 'WB1'])
                for ri in range(2):
                    w3 = r3(WB[ri], 128)
                    for hb in range(2):
                        pb = 6 + hb
                        for s4 in range(4):
                            s_ = hb * 4 + s4
                            TR(PS[pb][:, s4 * 128:(s4 + 1) * 128], w3[:, s_, :], [f'WB{ri}'], [f'ps{pb}'])
                        CP('act', LBm[d, ri][:, hb * 512:(hb + 1) * 512], PS[pb], [f'ps{pb}'], [f'LB{d}{ri}'])
                dk = f'DEC{d}'
                CP('pool', r3(DEC[d], 8), R1[:, u:u + 1].unsqueeze(2).to_broadcast([128, 64, 8]), ['R1'], [dk])
                MEMSET('pool', r3(DEC[d], 8)[:, :, 0], 0.0, [dk])

            def gen_C(d, pair, carry, lcset, lckey):
                u = d * 16 + pair
                q = pair % 4
                win = slice(32 * q, 32 * q + 32)
                lc_r = r3(lcset[d, 0], 128); lc_i = r3(lcset[d, 1], 128)
                if carry:
                    cplx_outer('dve', lc_r[:, :, win], lc_i[:, :, win], KR3, KI3, CTR3, CTI3, u, tabkeys, True, [f'{lckey}{d}0', f'{lckey}{d}1'])
                else:
                    cplx_outer('dve', lc_r[:, :, win], lc_i[:, :, win], ELR3, ELI3, CTR3, CTI3, u, tabkeys, True, [f'{lckey}{d}0', f'{lckey}{d}1'])

            def emit_B(pair, bi, par):
                cc = pair // 4
                tok0, ntok = BLOCKS[bi]
                for d in range(2):
                    u = d * 16 + pair
                    for ri in range(2):
                        pb = 2 * d + ri
                        lb3 = r3(LBm[d, ri], 128)
                        for s_ in range(8):
                            tau = s_ if d == 0 else 7 - s_
                            MM(r3(PS[pb][:, 0:ntok], 8)[:, :, s_], lb3[:, s_, :],
                               r3(UT3[:, cc, tok0:tok0 + ntok], 8)[:, :, tau], True, True,
                               [f'LB{d}{ri}', 'UT'], [f'ps{pb}'])
                        bp = BPB[par, d, ri]; bk = f'BP{par}{d}{ri}'
                        CP('act', bp[:, 0:ntok], PS[pb][:, 0:ntok], [f'ps{pb}'], [bk])
                        g = GB[par, d, ri]; gk = f'G{par}{d}{ri}'
                        OP('dve', 'tensor_tensor_scan', [f'DEC{d}', bk], [gk],
                           out=g[:, 0:ntok], data0=DEC[d][:, 0:ntok], data1=bp[:, 0:ntok], initial=0.0,
                           op0=ALU.mult, op1=ALU.add)
                        CP('pool', XZ4[:, u, slots(d, bi, 1), ri], r3(g[:, 0:ntok], 8)[:, :, 7], [gk], ['XZ'])

            def emit_C(pair, bi, par):
                cc = pair // 4
                tok0, ntok = BLOCKS[bi]
                pby = 4 + par
                for d in range(2):
                    for ri in range(2):
                        lc3 = r3(LCm[d, ri], 128)
                        g = GB[par, d, ri]; gk = f'G{par}{d}{ri}'
                        for s_ in range(8):
                            tau = s_ if d == 0 else 7 - s_
                            first = (d == 0 and ri == 0 and s_ == 0)
                            lastmm = (d == 1 and ri == 1 and s_ == 7)
                            MM(r3(PS[pby][:, 0:ntok], 8)[:, :, tau], lc3[:, s_, :], r3(g[:, 0:ntok], 8)[:, :, s_],
                               first, lastmm, [f'LC{d}{ri}', gk], [f'ps{pby}'])
                TT('dve', YACC3[:, cc, tok0:tok0 + ntok], YACC3[:, cc, tok0:tok0 + ntok], PS[pby][:, 0:ntok], ALU.add,
                   ['YACC', f'ps{pby}'], ['YACC'])

            BPB = {}
            GB = {}
            s5blk0 = bump[0]
            for par in range(2):
                for d in range(2):
                    for ri in range(2):
                        BPB[par, d, ri] = alloc(512)
                        GB[par, d, ri] = alloc(512, BF16)
            MODSB1 = arena[:, s5blk0:s5blk0 + 3072]
            BM1 = arena[:, s5blk0 + 3072:s5blk0 + 6144]
            assert bump[0] - s5blk0 >= 6144
            curqB = -1; curqC = -1
            prev = None
            cnt_items = 0
            for (q, pair) in order:
                if q != curqB:
                    MEMSET('pool', WB[0], 0.0, ['WB0'])
                    MEMSET('pool', WB[1], 0.0, ['WB1'])
                    curqB = q
                for d in range(2):
                    gen_B(d, pair)
                for bi in range(len(BLOCKS)):
                    par = cnt_items % 2
                    cnt_items += 1
                    emit_B(pair, bi, par)
                    if prev is not None:
                        emit_C(*prev)
                    if bi == 0:
                        if q != curqC:
                            for d in range(2):
                                for ri in range(2):
                                    MEMSET('pool', LCm[d, ri], 0.0, [f'LC{d}{ri}'])
                            curqC = q
                        for d in range(2):
                            gen_C(d, pair, False, LCm, 'LC')
                    prev = (pair, bi, par)
            emit_C(*prev)
            if stop == 'pass1':
                break
            if l == 0:
                S.barrier()
                emit_mod(1, [UT.bitcast(F32)[:, 0:3072], CN[:, 0:3072]], MODSB1, BM1, 'L1')
            ZP = [alloc(64), alloc(64)]
            CAB = alloc(128)
            LL4 = alloc(128)
            CAB4 = r4(CAB, 2, 2); LL4v = r4(LL4, 2, 2)
            CP('pool', LL4v[:, :, 0, :], LRR3, ['LRR'], ['LL4'])
            CP('pool', LL4v[:, :, 1, :], LII3, ['LII'], ['LL4'])
            MEMSET('pool', ZP[0], 0.0, ['ZP0r', 'ZP0i'])
            CH_E = 'dve'
            for k in range(NCH):
                zp = ZP[k % 2]; zn = ZP[(k + 1) % 2]
                zpk = f'ZP{k % 2}'; znk = f'ZP{(k + 1) % 2}'
                zn3 = r3(zn, 2); zp3 = r3(zp, 2)
                xk = XZ4[:, :, k + 1, :]
                TT(CH_E, CAB4, zp3.unsqueeze(2).to_broadcast([128, 32, 2, 2]), LL4v, ALU.mult, [zpk + 'r', zpk + 'i', 'LL4'], ['CAB'])
                TT(CH_E, CAB4[:, :, 0, :], CAB4[:, :, 0, :], xk, ALU.add, ['CAB', 'XZ'], ['CAB'])
                TT(CH_E, zn3[:, :, 0], CAB4[:, :, 0, 0], CAB4[:, :, 1, 1], ALU.subtract, ['CAB'], [znk + 'r'])
                TT(CH_E, zn3[:, :, 1], CAB4[:, :, 0, 1], CAB4[:, :, 1, 0], ALU.add, ['CAB'], [znk + 'i'])
                CP('pool', xk, zn3, [znk + 'r', znk + 'i'], ['XZ' + str(k)])
            if stop == 'chain':
                break
            S.barrier()
            LCsets = [LCm, LBm]
            LCkeys = ['LC', 'LB']
            setq = [-1, -1]

            def gen_pair2(idx):
                q, pair = order[idx]
                pp = idx % 2
                if setq[pp] != q:
                    for d in range(2):
                        for ri in range(2):
                            MEMSET('pool', LCsets[pp][d, ri], 0.0, [f'{LCkeys[pp]}{d}{ri}'])
                    setq[pp] = q
                for d in range(2):
                    gen_C(d, pair, True, LCsets[pp], LCkeys[pp])
            gen_pair2(0)
            it2 = 0
            for idx, (q, pair) in enumerate(order):
                cc = pair // 4
                pp = idx % 2
                if idx + 1 < len(order):
                    gen_pair2(idx + 1)
                for bi, (tok0, ntok) in enumerate(BLOCKS):
                    pby = it2 % 4
                    it2 += 1
                    for d in range(2):
                        u = d * 16 + pair
                        for ri in range(2):
                            lc3 = r3(LCsets[pp][d, ri], 128)
                            for s_ in range(8):
                                tau = s_ if d == 0 else 7 - s_
                                first = (d == 0 and ri == 0 and s_ == 0)
                                lastmm = (d == 1 and ri == 1 and s_ == 7)
                                MM(r3(PS[pby][:, 0:ntok], 8)[:, :, tau], lc3[:, s_, :], XZ4[:, u, slots(d, bi, 0), ri],
                                   first, lastmm, [f'{LCkeys[pp]}{d}{ri}', 'XZ'], [f'ps{pby}'])
                    TT('dve', YACC3[:, cc, tok0:tok0 + ntok], YACC3[:, cc, tok0:tok0 + ntok], PS[pby][:, 0:ntok], ALU.add,
                       ['YACC', f'ps{pby}'], ['YACC'])
            S.barrier()
            if dbg and l == 0:
                for c in range(4):
                    S.dma('sp', dbg_out[0, 0:128, c * NTOK:(c + 1) * NTOK], YACC3[:, c, :], reads=['YACC'], writes=['dbg0'])
                S.barrier()
                if stop == 's5':
                    break
            bump[0] = s5_mark
            limit[0] = ARENA_COLS - YCAT_COLS
            WG_ST = alloc(4 * 512); WG = alloc(4 * 512, BF16); WG3 = r3(WG, 512)
            S.dma('sp', r3(WG_ST, 512), w_glu[l].rearrange("(kc k) f -> k kc f", k=128), writes=['WGST'])
            CP('act', WG, WG_ST, ['WGST'], ['WG'])
            BG = alloc(4)
            S.dma('sp', BG, b_glu[l].rearrange("(c p) -> p c", p=128), writes=['BG'], allow_slow_non_contiguous=True)
            GT = alloc(4 * NTOK, BF16); GT3 = r3(GT, NTOK)
            SG = [alloc(512), alloc(512)]
            for c in range(4):
                ACT(YACC3[:, c, :], YACC3[:, c, :], AF.Gelu, ['YACC'], ['YACC'])
                CP('dve', GT3[:, c, :], YACC3[:, c, :], ['YACC'], ['GT'])
            for (tok0, ntok) in BLOCKS:
                for oc in range(4):
                    pb = oc % 2
                    for kc in range(4):
                        MM(PS[pb][:, 0:ntok], WG3[:, kc, oc * 128:(oc + 1) * 128], GT3[:, kc, tok0:tok0 + ntok], kc == 0, kc == 3,
                           ['WG', 'GT'], [f'ps{pb}'])
                    ACT(SG[pb][:, 0:ntok], PS[pb][:, 0:ntok], AF.Sigmoid, [f'ps{pb}', 'BG'], [f'SG{pb}'], bias=BG[:, oc:oc + 1])
                    TT('dve', YCAT3[:, oc, tok0:tok0 + ntok], YACC3[:, oc, tok0:tok0 + ntok], SG[pb][:, 0:ntok], ALU.mult,
                       ['YACC', f'SG{pb}'], ['YCAT'])
            S.barrier()

            if stop == 'gate':
                break
            bump[0] = lay_mark
            SH1 = modtile(0, 'SH1'); SC1 = modtile(1, 'SC1')
            for kind in range(2):
                TS('dve', SC1[kind], SC1[kind], 1.0, None, ALU.add, None, [f'SC1{kind}'], [f'SC1{kind}'])
            MODC = [alloc(16), alloc(16)]
            for kind in range(2):
                S.dma('sp', MODC[kind][:, 0:8], modv[l, kind, 0:1024].rearrange("(kc k) -> k kc", k=128), reads=['modv'], writes=['MODC'],
                      allow_slow_non_contiguous=True)
                S.dma('sp', MODC[kind][:, 8:16], modv[l, kind, 1024:2048].rearrange("(kc k) -> k kc", k=128), reads=['modv'], writes=['MODC'],
                      allow_slow_non_contiguous=True)
                TS('dve', MODC[kind][:, 8:16], MODC[kind][:, 8:16], 1.0, None, ALU.add, None, ['MODC'], ['MODC'])
            GLU = alloc(2 * NTOK); GLU3 = r3(GLU, NTOK)
            NF2 = 1280
            WST = alloc(8 * 640); WST3 = r3(WST, 640)
            WINR = alloc(8 * NF2, BF16); WINR3 = r3(WINR, NF2)
            for hf in range(2):
                S.dma('sp', WST3, w_in[l, :, 512 + hf * 640:512 + (hf + 1) * 640].rearrange("(kc k) f -> k kc f", k=128), writes=['WST'])
                CP('act', WINR3[:, :, hf * 640:(hf + 1) * 640], WST3, ['WST'], ['WINR'])
            WSC = alloc(8)
            WSC3 = r3(WSC[:, 0:6], 3)
            for c_ in range(2):
                S.dma('sp', WSC3[:, c_, :], w_sc[l][:, c_ * 128:(c_ + 1) * 128].rearrange("k p -> p k"), writes=['WSC'], allow_slow_non_contiguous=True)
            XT = [alloc(1024), alloc(1024)]
            HTs3 = [r3(alloc(8 * 512, BF16), 512), r3(alloc(8 * 512, BF16), 512)]
            BGT = alloc(512); CGT = alloc(512); CV = alloc(512); ACC = alloc(512); SIG = alloc(512)
            make_hT(BLOCKS[0][0], BLOCKS[0][1], SH1, SC1, 'SH1', 'SC1', 0)
            for bix, (tok0, ntok) in enumerate(BLOCKS):
                hpar = bix % 2
                HT3 = HTs3[hpar]
                if bix + 1 < len(BLOCKS):
                    make_hT(BLOCKS[bix + 1][0], BLOCKS[bix + 1][1], SH1, SC1, 'SH1', 'SC1', (bix + 1) % 2)
                W = 64 if tok0 >= NCTX else 256
                nr = ntok // W

                def zmm(fc, pb):
                    f0 = fc * 128 - 512
                    for kc in range(8):
                        MM(PS[pb][:, 0:ntok], WINR3[:, kc, f0:f0 + 128], HT3[:, kc, 0:ntok], kc == 0, kc == 7,
                           ['WINR', f'HT{hpar}'], [f'ps{pb}'])
                for j in range(2):
                    zmm(4 + j, 0)
                    CP('act', BGT[:, 0:ntok], PS[0][:, 0:ntok], ['ps0'], ['BGT'])
                    zmm(6 + j, 1)
                    CP('act', CGT[:, 0:ntok], PS[1][:, 0:ntok], ['ps1'], ['CGT'])
                    zmm(8 + j, 2)
                    TT('dve', CV[:, 0:ntok], CGT[:, 0:ntok], PS[2][:, 0:ntok], ALU.mult, ['CGT', 'ps2'], ['CV'])
                    cv3 = r3(CV[:, 0:ntok], W); ac3 = r3(ACC[:, 0:ntok], W)
                    TS('dve', ACC[:, 0:ntok], CV[:, 0:ntok], WSC3[:, j, 1:2], None, ALU.mult, None, ['CV', 'WSC'], ['ACC'])
                    STT(ac3[:, :, 1:W], cv3[:, :, 0:W - 1], WSC3[:, j, 0:1], ac3[:, :, 1:W], ALU.mult, ALU.add, ['CV', 'WSC', 'ACC'], ['ACC'])
                    STT(ac3[:, :, 0:W - 1], cv3[:, :, 1:W], WSC3[:, j, 2:3], ac3[:, :, 0:W - 1], ALU.mult, ALU.add, ['CV', 'WSC', 'ACC'], ['ACC'])
                    TT('dve', YCAT3[:, 4 + j, tok0:tok0 + ntok], ACC[:, 0:ntok], BGT[:, 0:ntok], ALU.mult, ['ACC', 'BGT'], ['YCAT'])
                    zmm(12 + j, 3)
                    ACT(SIG[:, 0:ntok], PS[3][:, 0:ntok], AF.Sigmoid, ['ps3'], ['SIG'])
                    zmm(10 + j, 4)
                    TT('dve', GLU3[:, j, tok0:tok0 + ntok], SIG[:, 0:ntok], PS[4][:, 0:ntok], ALU.mult, ['SIG', 'ps4'], ['GLU'])
            if stop == 'b2':
                S.barrier()
                break
            WDW = alloc(64)
            WDW3 = r3(WDW[:, 0:62], 31)
            for c_ in range(2):
                S.dma('sp', WDW3[:, c_, :], w_dw[l][:, c_ * 128:(c_ + 1) * 128].rearrange("k p -> p k"), writes=['WDW'], allow_slow_non_contiguous=True)
            CFV = alloc(8)
            CFV3 = r3(CFV[:, 0:6], 2)
            for i_, src in enumerate((b_dw, ln_cf_g, ln_cf_b)):
                S.dma('sp', CFV3[:, i_, :], src[l].rearrange("(c p) -> p c", p=128), writes=['CFV'], allow_slow_non_contiguous=True)
            ONES = alloc(128)
            MEMSET('pool', ONES, 1.0 / 256.0, ['ONES'])
            TC = alloc(2 * NTOK); TC3 = r3(TC, NTOK)
            SQ = alloc(2 * 512); SQ3 = r3(SQ, 512)
            S.barrier()
            GLB = WST[:, 0:NTOK].bitcast(BF16); GLB3 = r3(GLB, NTOK)
            DG3 = r3(WINR[:, 0:62 * 128], 128)
            for j in range(2):
                CP('act', GLB3[:, j, :], GLU3[:, j, :], ['GLU'], ['GLB'])
                for k in range(31):
                    TS('dve', DG3[:, j * 31 + k, :], IDENT, WDW3[:, j, k:k + 1], None, ALU.mult, None, ['IDENT', 'WDW'], ['DG'])
            cbank = 0
            taporder = [15] + [k for k in range(31) if k != 15]
            for j in range(2):
                pb = 2 + cbank % 4; cbank += 1
                for idx, k in enumerate(taporder):
                    dlt = k - 15
                    lo = max(0, -dlt); hi = min(NCTX, NCTX - dlt)
                    MM(PS[pb][:, lo:hi], DG3[:, j * 31 + k, :], GLB3[:, j, lo + dlt:hi + dlt], idx == 0, idx == 30,
                       ['DG', 'GLB'], [f'ps{pb}'])
                ACT(TC3[:, j, 0:NCTX], PS[pb][:, 0:NCTX], AF.Identity, [f'ps{pb}', 'CFV'], ['TC'], bias=CFV3[:, 0, j:j + 1])
                for b in range(4):
                    pb = 2 + cbank % 4; cbank += 1
                    taps = []
                    for k in taporder:
                        dlt = k - 15
                        lo = max(8 * b, -dlt); hi = min(8 * b + 8, 32 - dlt)
                        if hi > lo:
                            taps.append((k, dlt, lo, hi))
                    for idx, (k, dlt, lo, hi) in enumerate(taps):
                        MM(PS[pb][:, (lo - 8 * b) * 64:(hi - 8 * b) * 64], DG3[:, j * 31 + k, :],
                           GLB3[:, j, NCTX + (lo + dlt) * 64:NCTX + (hi + dlt) * 64], idx == 0, idx == len(taps) - 1,
                           ['DG', 'GLB'], [f'ps{pb}'])
                    ACT(TC3[:, j, NCTX + b * 512:NCTX + (b + 1) * 512], PS[pb], AF.Identity, [f'ps{pb}', 'CFV'], ['TC'],
                        bias=CFV3[:, 0, j:j + 1])
            MEAN = alloc(512); RSTD = alloc(512); TN = alloc(512)
            for (tok0, ntok) in BLOCKS:
                for j in range(2):
                    ACT(SQ3[:, j, 0:ntok], TC3[:, j, tok0:tok0 + ntok], AF.Square, ['TC'], ['SQ'])
                for j in range(2):
                    MM(PS[0][:, 0:ntok], ONES, TC3[:, j, tok0:tok0 + ntok], j == 0, j == 1, ['ONES', 'TC'], ['ps0'])
                for j in range(2):
                    MM(PS[1][:, 0:ntok], ONES, SQ3[:, j, 0:ntok], j == 0, j == 1, ['ONES', 'SQ'], ['ps1'])
                CP('act', MEAN[:, 0:ntok], PS[0][:, 0:ntok], ['ps0'], ['MEAN'])
                TT('dve', RSTD[:, 0:ntok], MEAN[:, 0:ntok], MEAN[:, 0:ntok], ALU.mult, ['MEAN'], ['RSTD'])
                TT('dve', RSTD[:, 0:ntok], PS[1][:, 0:ntok], RSTD[:, 0:ntok], ALU.subtract, ['ps1', 'RSTD'], ['RSTD'])
                ACT(RSTD[:, 0:ntok], RSTD[:, 0:ntok], AF.Sqrt, ['RSTD', 'EPS'], ['RSTD'], bias=EPS[:, 0:1])
                OP('dve', 'reciprocal', ['RSTD'], ['RSTD'], out=RSTD[:, 0:ntok], in_=RSTD[:, 0:ntok])
                for j in range(2):
                    TT('dve', TN[:, 0:ntok], TC3[:, j, tok0:tok0 + ntok], MEAN[:, 0:ntok], ALU.subtract, ['TC', 'MEAN'], ['TN'])
                    TT('dve', TN[:, 0:ntok], TN[:, 0:ntok], RSTD[:, 0:ntok], ALU.mult, ['TN', 'RSTD'], ['TN'])
                    ACT(YCAT3[:, 6 + j, tok0:tok0 + ntok], TN[:, 0:ntok], AF.Silu, ['TN', 'CFV'], ['YCAT'],
                        scale=CFV3[:, 1, j:j + 1], bias=CFV3[:, 2, j:j + 1])
            S.barrier()

            if stop == 'c':
                break
            bump[0] = lay_mark
            G1 = modtile(2, 'G1')
            LG = alloc(1024); LB_ = alloc(1024)
            load_bc(LG, ln1_g[l:l + 1, :], 'LG'); load_bc(LB_, ln1_b[l:l + 1, :], 'LB_')
            WO_ST = alloc(8 * 512); WO = alloc(8 * 1024, BF16); WO3 = r3(WO, 1024)
            for hf in range(2):
                S.dma('sp', r3(WO_ST, 512), w_o[l, :, hf * 512:(hf + 1) * 512].rearrange("(kc k) f -> k kc f", k=128), writes=['WOST'])
                CP('act', WO3[:, :, hf * 512:(hf + 1) * 512], r3(WO_ST, 512), ['WOST'], ['WO'])
            XT = [alloc(1024), alloc(1024)]
            RT = [alloc(1024), alloc(1024)]
            ST6 = alloc(16); MV = alloc(8)

            def layer_norm_tile(rt, rk, lg, lb, gkeys):
                OP('dve', 'bn_stats', [rk], ['ST6'], out=ST6[:, 0:6], in_=rt[:, 0:512])
                OP('dve', 'bn_stats', [rk], ['ST6'], out=ST6[:, 6:12], in_=rt[:, 512:1024])
                OP('dve', 'bn_aggr', ['ST6'], ['MV'], out=MV[:, 0:2], in_=ST6[:, 0:12])
                ACT(MV[:, 2:3], MV[:, 1:2], AF.Sqrt, ['MV', 'EPS'], ['MV'], bias=EPS[:, 0:1])
                OP('dve', 'reciprocal', ['MV'], ['MV'], out=MV[:, 3:4], in_=MV[:, 2:3])
                STT(MV[:, 4:5], MV[:, 0:1], -1.0, MV[:, 3:4], ALU.mult, ALU.mult, ['MV'], ['MV'])
                ACT(rt, rt, AF.Identity, [rk, 'MV'], [rk], scale=MV[:, 3:4], bias=MV[:, 4:5])
                TT('dve', rt, rt, lg, ALU.mult, [rk] + gkeys, [rk])
                TT('pool', rt, rt, lb, ALU.add, [rk] + gkeys, [rk])

            tiles_E = range(18) if not last else range(2, 18)
            for ti in tiles_E:
                kd = kind_of(ti)
                xt = XT[ti % 2]; xk = f'XT{ti % 2}'
                rt = RT[ti % 2]; rk = f'RT{ti % 2}'
                S.dma('sp', xt, xs[ti * 128:(ti + 1) * 128, :], reads=['xs', 'xs2'], writes=[xk])
                for hf in range(2):
                    pb = (ti % 2) * 2 + hf
                    for kc in range(8):
                        MM(PS[pb], YCAT3[:, kc, ti * 128:(ti + 1) * 128], WO3[:, kc, hf * 512:(hf + 1) * 512], kc == 0, kc == 7,
                           ['YCAT', 'WO'], [f'ps{pb}'])
                    TT('dve', rt[:, hf * 512:(hf + 1) * 512], PS[pb], G1[kd][:, hf * 512:(hf + 1) * 512], ALU.mult,
                       [f'ps{pb}', f'G1{kd}'], [rk])
                STT(rt, xt, DN_ALPHA, rt, ALU.mult, ALU.add, [xk, rk], [rk])
                layer_norm_tile(rt, rk, LG, LB_, ['LG', 'LB_'])
                S.dma('pool', xs[ti * 128:(ti + 1) * 128, :], rt, reads=[rk], writes=[f'xs_t{ti}'])
            S.barrier()
            if dbg and l == 0:
                S.dma('sp', dbg_out[1, :, 0:D], xs, reads=[], writes=['dbg1'])
                S.barrier()
                if stop == 'ln1':
                    break

            bump[0] = persist_mark
            limit[0] = ARENA_COLS
            SH2 = modtile(3, 'SH2'); SC2 = modtile(4, 'SC2')
            for kind in range(2):
                TS('dve', SC2[kind], SC2[kind], 1.0, None, ALU.add, None, [f'SC2{kind}'], [f'SC2{kind}'])
            tiles_F = list(range(18)) if not last else list(range(2, 18))
            blocks_F = BLOCKS if not last else BLOCKS[1:]
            H2T = alloc(8 * NTOK, BF16); H2T3 = r3(H2T, NTOK)
            RW = alloc(18 * 16); RW3 = r3(RW, 16)
            WR = alloc(8 * 20); WR3 = r3(WR, 20)
            S.dma('sp', WR3[:, :, 0:4], w_rg[l].rearrange("(kc k) f -> k kc f", k=128), writes=['WR'])
            S.dma('sp', WR3[:, :, 4:20], w_rexp[l].rearrange("(kc k) f -> k kc f", k=128), writes=['WR'])
            BR = alloc(24)
            S.dma('sp', BR[:, 0:4], b_rg[l:l + 1, :].partition_broadcast(128), writes=['BR'])
            S.dma('sp', BR[:, 4:20], b_rexp[l:l + 1, :].partition_broadcast(128), writes=['BR'])
            RWT = alloc(NTOK, BF16)
            SEL = alloc(16 * 128, BF16); SEL3 = r3(SEL, 128)
            XTB = alloc(2048)
            XT = [XTB[:, 0:1024], XTB[:, 1024:2048]]
            SELF3 = r3(XTB, 128)
            CP('pool', SELF3[0:32], IDENT[0:32, 0:16].unsqueeze(2).to_broadcast([32, 16, 128]), ['IDENT'], ['XT0', 'XT1'])
            TT('pool', SELF3[0:32], SELF3[0:32], IDENT[0:32, 16:32].unsqueeze(2).to_broadcast([32, 16, 128]), ALU.add, ['IDENT', 'XT0', 'XT1'], ['XT0', 'XT1'])
            CP('pool', SEL3[0:32], SELF3[0:32], ['XT0', 'XT1'], ['SEL'])
            H32 = alloc(1024); H32_3 = r3(H32, 128)
            SM = alloc(64)
            LGT = SM[:, 0:20]; MG = SM[:, 20:24]; PEN = SM[:, 24:28]; SC_ = SM[:, 28:40]; EG = SM[:, 40:44]
            EM = alloc(16); EM2 = alloc(16); MK1 = alloc(16); MK2 = alloc(16)
            def f1_stageA(ti):
                kd = kind_of(ti)
                xt = XT[ti % 2]; xk = f'XT{ti % 2}'
                S.dma('sp', xt, xs[ti * 128:(ti + 1) * 128, :], reads=[f'xs_t{ti}'], writes=[xk])
                TT('dve', xt, xt, SC2[kd], ALU.mult, [xk, f'SC2{kd}'], [xk])
                TT('pool', xt, xt, SH2[kd], ALU.add, [xk, f'SH2{kd}'], [xk])

            def f1_stageA2(ti):
                xt = XT[ti % 2]; xk = f'XT{ti % 2}'
                for hh in range(2):
                    pb = 6 + hh
                    for k4 in range(4):
                        kc = hh * 4 + k4
                        TR(PS[pb][:, k4 * 128:(k4 + 1) * 128], xt[:, kc * 128:(kc + 1) * 128], [xk], [f'ps{pb}'])
                    CP('act', H2T3[:, hh * 4:(hh + 1) * 4, ti * 128:(ti + 1) * 128], r3(PS[pb], 128), [f'ps{pb}'], ['H2T'])
                    CP('act', H32_3[:, hh * 4:(hh + 1) * 4, :], r3(PS[pb], 128), [f'ps{pb}'], ['H32'])
                for kc in range(8):
                    MM(PS[5][:, 0:20], H32_3[:, kc, :], WR3[:, kc, :], kc == 0, kc == 7, ['H32', 'WR'], ['ps5'])

            NT = 18
            LGA = alloc(NT * 20); LGA3 = r3(LGA, 20)
            MEMSET('pool', LGA, 0.0, ['LGA'])

            def f1_stageB(ti):
                TT('dve', LGA3[:, ti, :], PS[5][:, 0:20], BR[:, 0:20], ALU.add, ['ps5', 'BR'], ['LGA'])

            f1_stageA(tiles_F[0])
            f1_stageA2(tiles_F[0])
            for ix, ti in enumerate(tiles_F):
                if ix + 1 < len(tiles_F):
                    f1_stageA(tiles_F[ix + 1])
                f1_stageB(ti)
                if ix + 1 < len(tiles_F):
                    f1_stageA2(tiles_F[ix + 1])
            def bc2(v, n):
                return v.unsqueeze(2).to_broadcast([128, NT, n])
            GMX = alloc(NT); GSUM = alloc(NT); GW = alloc(NT); M1 = alloc(NT); M2 = alloc(NT); DL = alloc(NT); EX = alloc(NT)
            P1 = alloc(NT); P2 = alloc(NT)
            MGA = alloc(NT * 4); D4 = alloc(NT * 4); PENA = alloc(NT * 4)
            EMA = alloc(NT * 16); EM2A = alloc(NT * 16); MK1A = alloc(NT * 16); MK2A = alloc(NT * 16)
            RW2 = alloc(NT * 32); RW2_3 = r3(RW2, 32)
            RWH = alloc(NT * 16, BF16)
            G4 = LGA3[:, :, 0:4]
            OP('dve', 'tensor_reduce', ['LGA'], ['GMX'], out=GMX, in_=G4, axis=AX.X, op=ALU.max)
            TT('dve', r3(MGA, 4), G4, bc2(GMX, 4), ALU.is_ge, ['LGA', 'GMX'], ['MGA'])
            TT('dve', r3(D4, 4), G4, bc2(GMX, 4), ALU.subtract, ['LGA', 'GMX'], ['D4'])
            ACT(D4, D4, AF.Exp, ['D4'], ['D4'])
            OP('dve', 'tensor_reduce', ['D4'], ['GSUM'], out=GSUM, in_=r3(D4, 4), axis=AX.X, op=ALU.add)
            OP('dve', 'reciprocal', ['GSUM'], ['GW'], out=GW, in_=GSUM)
            TS('dve', PENA, MGA, -1.0, 1e30, ALU.add, ALU.mult, ['MGA'], ['PENA'])
            TT('dve', r4(EMA, 4, 4), LGA3[:, :, 4:20].rearrange("p t (g j) -> p t g j", j=4),
               r3(PENA, 4).unsqueeze(3).to_broadcast([128, NT, 4, 4]), ALU.add, ['LGA', 'PENA'], ['EMA'])
            OP('dve', 'tensor_reduce', ['EMA'], ['M1'], out=M1, in_=r3(EMA, 16), axis=AX.X, op=ALU.max)
            TT('dve', r3(MK1A, 16), r3(EMA, 16), bc2(M1, 16), ALU.is_ge, ['EMA', 'M1'], ['MK1A'])
            STT(EM2A, MK1A, -1e30, EMA, ALU.mult, ALU.add, ['MK1A', 'EMA'], ['EM2A'])
            OP('dve', 'tensor_reduce', ['EM2A'], ['M2'], out=M2, in_=r3(EM2A, 16), axis=AX.X, op=ALU.max)
            TT('dve', r3(MK2A, 16), r3(EM2A, 16), bc2(M2, 16), ALU.is_ge, ['EM2A', 'M2'], ['MK2A'])
            TT('dve', DL, M2, M1, ALU.subtract, ['M1', 'M2'], ['DL'])
            ACT(EX, DL, AF.Exp, ['DL'], ['EX'])
            TS('dve', P1, EX, 1.0, None, ALU.add, None, ['EX'], ['P1'])
            OP('dve', 'reciprocal', ['P1'], ['P1'], out=P1, in_=P1)
            TT('dve', P2, EX, P1, ALU.mult, ['EX', 'P1'], ['P2'])
            TT('dve', P1, P1, GW, ALU.mult, ['P1', 'GW'], ['P1'])
            TT('dve', P2, P2, GW, ALU.mult, ['P2', 'GW'], ['P2'])
            TT('dve', r3(MK1A, 16), r3(MK1A, 16), bc2(P1, 16), ALU.mult, ['MK1A', 'P1'], ['MK1A'])
            TT('dve', r3(MK2A, 16), r3(MK2A, 16), bc2(P2, 16), ALU.mult, ['MK2A', 'P2'], ['MK2A'])
            TT('dve', MK1A, MK1A, MK2A, ALU.add, ['MK1A', 'MK2A'], ['MK1A'])
            CP('dve', RWH, MK1A, ['MK1A'], ['RWH'])
            CP('dve', RW2_3[:, :, 0:16], r3(RWH, 16), ['RWH'], ['RW2'])
            TT('dve', RW2_3[:, :, 16:32], r3(MK1A, 16), RW2_3[:, :, 0:16], ALU.subtract, ['MK1A', 'RW2'], ['RW2'])
            for ti in tiles_F:
                TR(PS[4][0:32, 0:128], RW2_3[:, ti, :], ['RW2'], ['ps4'])
                CP('act', RWT[0:32, ti * 128:(ti + 1) * 128], PS[4][0:32, 0:128], ['ps4'], ['RWT'])
            if stop in ('f1', 'f1a', 'f1b', 'f1c', 'f1d', 'f1a0', 'f1a1'):
                S.barrier()
                break
            S.barrier()
            FACC = alloc(18 * 1024); FACC3 = r3(FACC, 1024)
            EST = [alloc(2048), alloc(2048)]
            WGa_ = [alloc(2048, BF16), SC2[0].bitcast(BF16)]
            WUp_ = [alloc(2048, BF16), SC2[1].bitcast(BF16)]
            WDn_ = [alloc(2048, BF16), H32.bitcast(BF16)]

            def load_expert(e_):
                wp = e_ % 2
                S.dma('sp', r3(EST[0], 256), w_gate[l, e_].rearrange("(kc k) f -> k kc f", k=128), writes=['EST0'])
                CP('pool', WGa_[wp], EST[0], ['EST0'], [f'WGa{wp}'])
                S.dma('act', r3(EST[1], 256), w_up[l, e_].rearrange("(kc k) f -> k kc f", k=128), writes=['EST1'])
                CP('pool', WUp_[wp], EST[1], ['EST1'], [f'WUp{wp}'])
                S.dma('sp', r3(EST[0], 1024), w_down[l, e_].rearrange("(fc f) d -> f fc d", f=128), writes=['EST0'])
                CP('pool', WDn_[wp], EST[0], ['EST0'], [f'WDn{wp}'])
            SIL = [alloc(512), alloc(512)]
            ACTT = alloc(2 * 512, BF16); ACTT3 = r3(ACTT, 512)
            ACTTs3 = [ACTT3, r3(alloc(2 * 512, BF16), 512)]

            def exp_U(e_, bi, par):
                wp = e_ % 2
                WGa3 = r3(WGa_[wp], 256); WUp3 = r3(WUp_[wp], 256)
                tok0, ntok = blocks_F[bi]
                at3 = ACTTs3[par]
                for fc in range(2):
                    for kc in range(8):
                        MM(PS[fc][:, 0:ntok], WGa3[:, kc, fc * 128:(fc + 1) * 128], H2T3[:, kc, tok0:tok0 + ntok], kc == 0, kc == 7,
                           [f'WGa{wp}', 'H2T'], [f'ps{fc}'])
                    for kc in range(8):
                        MM(PS[2 + fc][:, 0:ntok], WUp3[:, kc, fc * 128:(fc + 1) * 128], H2T3[:, kc, tok0:tok0 + ntok], kc == 0, kc == 7,
                           [f'WUp{wp}', 'H2T'], [f'ps{2 + fc}'])
                    if fc == 0:
                        MM(PS[4][:, 0:ntok], SEL3[0:32, e_, :], RWT[0:32, tok0:tok0 + ntok], True, True, ['SEL', 'RWT'], ['ps4'])
                    ACT(SIL[fc][:, 0:ntok], PS[fc][:, 0:ntok], AF.Silu, [f'ps{fc}'], [f'SIL{fc}'])
                    TT('dve', SIL[fc][:, 0:ntok], SIL[fc][:, 0:ntok], PS[2 + fc][:, 0:ntok], ALU.mult,
                       [f'SIL{fc}', f'ps{2 + fc}'], [f'SIL{fc}'])
                    TT('dve', at3[:, fc, 0:ntok], SIL[fc][:, 0:ntok], PS[4][:, 0:ntok], ALU.mult,
                       [f'SIL{fc}', 'ps4'], [f'ACTT{par}{fc}'])

            dcount = [0]

            def exp_D(e_, bi, par):
                wp = e_ % 2
                WDn3 = r3(WDn_[wp], 1024)
                tok0, ntok = blocks_F[bi]
                at3 = ACTTs3[par]
                for i in range(ntok // 128):
                    ti = tok0 // 128 + i
                    for hf in range(2):
                        pb = 5 + dcount[0] % 3
                        dcount[0] += 1
                        for fc in range(2):
                            MM(PS[pb], at3[:, fc, i * 128:(i + 1) * 128], WDn3[:, fc, hf * 512:(hf + 1) * 512], fc == 0, fc == 1,
                               [f'ACTT{par}{fc}', f'WDn{wp}'], [f'ps{pb}'])
                        fk = f'FACC{ti}'
                        if e_ == 0:
                            CP('act', FACC3[:, ti, hf * 512:(hf + 1) * 512], PS[pb], [f'ps{pb}'], [fk])
                        else:
                            TT('dve', FACC3[:, ti, hf * 512:(hf + 1) * 512], FACC3[:, ti, hf * 512:(hf + 1) * 512], PS[pb], ALU.add,
                               [f'ps{pb}', fk], [fk])

            items_x = [(e_, bi) for e_ in range(NE) for bi in range(len(blocks_F))]
            load_expert(0)
            prev_x = None
            for ix, (e_, bi) in enumerate(items_x):
                exp_U(e_, bi, ix % 2)
                if prev_x is not None:
                    exp_D(*prev_x)
                if bi == 0 and e_ + 1 < NE:
                    load_expert(e_ + 1)
                prev_x = (e_, bi, ix % 2)
            exp_D(*prev_x)
            S.barrier()
            if stop == 'f2':
                break
            G2 = SH2
            for kind in range(2):
                load_bc(G2[kind], modv[l, kind:kind + 1, 5 * 1024:6 * 1024], f'G2{kind}')
            LG2 = EST[0][:, 0:1024]; LB2 = EST[0][:, 1024:2048]
            load_bc(LG2, ln2_g[l:l + 1, :], 'LG2'); load_bc(LB2, ln2_b[l:l + 1, :], 'LB2')
            ST6 = alloc(16); MV = alloc(8)
            for ti in tiles_F:
                kd = kind_of(ti)
                xt = XT[ti % 2]; xk = f'XT{ti % 2}'
                S.dma('sp', xt, xs[ti * 128:(ti + 1) * 128, :], reads=[f'xs_t{ti}'], writes=[xk])
                fk = f'FACC{ti}'
                ft = FACC3[:, ti, :]
                TT('pool', ft, ft, G2[kd], ALU.mult, [fk, f'G2{kd}'], [fk])
                STT(ft, xt, DN_ALPHA, ft, ALU.mult, ALU.add, [xk, fk], [fk])
                if stop == 'l0dbg':
                    S.dma('sp', dbg_out[2, 0:128, 0:1024], ft, reads=[fk], writes=['dbgx'])
                    S.dma('sp', dbg_out[2, 128:256, 0:1024], xt, reads=[xk], writes=['dbgx2'])
                    S.dma('sp', dbg_out[2, 256:384, 0:288], RW, reads=['RW'], writes=['dbgx3'])
                    break
                layer_norm_tile(ft, fk, LG2, LB2, ['LG2', 'LB2'])
                if last:
                    S.dma('pool', out[(ti - 2) * 128:(ti - 1) * 128, :], ft, reads=[fk], writes=[f'out{ti}'])
                else:
                    S.dma('pool', xs[ti * 128:(ti + 1) * 128, :], ft, reads=[fk], writes=['xs'])
            S.barrier()
            if dbg and l == 0:
                S.dma('sp', dbg_out[2, :, 0:D], xs, reads=[], writes=['dbg2'])
                S.barrier()
                if stop in ('l0', 'l0dbg'):
                    break
        S.barrier()
        S.emit()
        print("n ops", S.nops, {k: len(v) for k, v in S.ops.items()})
    return nc


_NC_CACHE = {}


def make_in_maps(inputs):
    ident = np.eye(128, dtype=np.float32)
    x = np.asarray(inputs['x'], np.float32)
    c = np.asarray(inputs['c'], np.float32)
    ctx = np.asarray(inputs['ctx'], np.float32)
    c_ctx = np.asarray(inputs['c_ctx'], np.float32)
    wnames = ['w_mod', 'b_mod', 'w_in', 's5_a_re', 's5_a_im', 's5_log_dt', 's5_b_re', 's5_b_im', 's5_c_re', 's5_c_im',
              's5_d', 'w_glu', 'b_glu', 'w_sc', 'w_dw', 'b_dw', 'ln_cf_g', 'ln_cf_b', 'w_o', 'ln1_g', 'ln1_b',
              'w_rg', 'b_rg', 'w_rexp', 'b_rexp', 'w_gate', 'w_up', 'w_down', 'ln2_g', 'ln2_b']
    shared = {n: np.ascontiguousarray(np.asarray(inputs[n], np.float32)) for n in wnames}
    maps = []
    for b in range(x.shape[0]):
        m = dict(shared)
        m['xin'] = np.ascontiguousarray(np.concatenate([ctx[b], x[b]], axis=0))
        cv = np.stack([c[b], c_ctx], axis=0)
        m['cT'] = np.ascontiguousarray(cv.reshape(2, 8, 128).transpose(2, 1, 0))
        m['ident'] = ident
        maps.append(m)
    return maps


def kernel(**inputs):
    if 'nc' not in _NC_CACHE:
        _NC_CACHE['nc'] = build_nc()
    nc = _NC_CACHE['nc']
    maps = make_in_maps(inputs)
    res = run_bass_kernel_spmd(nc, maps, core_ids=list(range(8)))
    return np.stack([np.asarray(r['out'], np.float32) for r in res.results], axis=0)
```
